# Optimizing a Trainium2 kernel written in Bass

```python
import math
import jax
import jax.numpy as jnp
from jax import lax
import numpy as np

D_MODEL = 2048
BATCH = 4
SEQ = 8192
DEPTH = 2

GRID_W = 64
CTX_LEN = 256
BRANCH_W = 512
N_BRANCH = 3
SSM_GROUP = 16
SSM_GROUPS = BRANCH_W // SSM_GROUP
SSM_STATE = 64
SSM_DT_MIN = 1e-3
SSM_DT_MAX = 1e-1
NA_HEADS = 8
NA_HEAD_DIM = BRANCH_W // NA_HEADS
NA_WIN_ROWS = 8
NA_WIN_COLS = 16
HY_ORDER = 2
HY_SHORT = 3
HY_BANDS = 16
HY_EMB = 1 + 2 * HY_BANDS
HY_FILTER_W = 64
HY_MIN_DECAY = math.log(1e-2) / 1.5
HY_MAX_DECAY = math.log(1e-2) / 0.3
N_EXPERTS = 16
EXPERT_FF = 2048
EC_CAPACITY = 2
IN_WIDTH = BRANCH_W + 3 * BRANCH_W + (HY_ORDER + 1) * BRANCH_W
NORM_EPS = 1e-6

kernel_name = 'hybrid_s5_natten_hyena_ec_dit_block'


def _rmsnorm(x, g):
    xf = x.astype(jnp.float32)
    y = xf * lax.rsqrt(jnp.mean(xf * xf, axis=-1, keepdims=True) + NORM_EPS)
    return (y * g.astype(jnp.float32)).astype(x.dtype)


def _modulate(x, shift, scale):
    return x * (1.0 + scale) + shift


def _cmul(ar, ai, br, bi):
    return ar * br - ai * bi, ar * bi + ai * br


def _s5_combine(e1, e2):
    a1r, a1i, b1r, b1i = e1
    a2r, a2i, b2r, b2i = e2
    ar, ai = _cmul(a2r, a2i, a1r, a1i)
    br, bi = _cmul(a2r, a2i, b1r, b1i)
    return ar, ai, br + b2r, bi + b2i


def _s5_discretize(lam_re, lam_im, log_step, b_re, b_im):
    lam_re = lam_re.astype(jnp.float32)
    lam_im = lam_im.astype(jnp.float32)
    step = jnp.exp(log_step.astype(jnp.float32))[:, None]
    mag = jnp.exp(lam_re * step)
    a_re = mag * jnp.cos(lam_im * step)
    a_im = mag * jnp.sin(lam_im * step)
    den = lam_re * lam_re + lam_im * lam_im
    num_re = a_re - 1.0
    coef_re = (num_re * lam_re + a_im * lam_im) / den
    coef_im = (a_im * lam_re - num_re * lam_im) / den
    bb_re, bb_im = _cmul(coef_re[..., None], coef_im[..., None],
                         b_re.astype(jnp.float32), b_im.astype(jnp.float32))
    return a_re, a_im, bb_re, bb_im


def _s5_scan(a_re, a_im, bb_re, bb_im, u, h0_re, h0_im, reverse):
    bu_re = jnp.einsum('gpk,blgk->blgp', bb_re, u)
    bu_im = jnp.einsum('gpk,blgk->blgp', bb_im, u)
    first = -1 if reverse else 0
    s_re, s_im = _cmul(a_re, a_im, h0_re, h0_im)
    bu_re = bu_re.at[:, first].add(s_re)
    bu_im = bu_im.at[:, first].add(s_im)
    length = u.shape[1]
    a_re_t = jnp.broadcast_to(a_re, (1, length) + a_re.shape)
    a_im_t = jnp.broadcast_to(a_im, (1, length) + a_im.shape)
    _, _, h_re, h_im = lax.associative_scan(_s5_combine, (a_re_t, a_im_t, bu_re, bu_im),
                                            reverse=reverse, axis=1)
    return h_re, h_im


def _s5_readout(c_re, c_im, h_re, h_im):
    return (jnp.einsum('gkp,blgp->blgk', c_re.astype(jnp.float32), h_re)
            - jnp.einsum('gkp,blgp->blgk', c_im.astype(jnp.float32), h_im))


def _s5_output(y, u, p, dtype):
    b, length = u.shape[0], u.shape[1]
    y = (y + p['ssm_d'].astype(jnp.float32).reshape(SSM_GROUPS, SSM_GROUP) * u).reshape(b, length, BRANCH_W)
    z = jax.nn.gelu(y)
    return (z * jax.nn.sigmoid(z @ p['ssm_w_glu'].astype(jnp.float32))).astype(dtype)


def _s5_branch(u_ctx, u_lat, p, last):
    b = u_lat.shape[0]
    uc = u_ctx.astype(jnp.float32).reshape(b, u_ctx.shape[1], SSM_GROUPS, SSM_GROUP)
    ul = u_lat.astype(jnp.float32).reshape(b, u_lat.shape[1], SSM_GROUPS, SSM_GROUP)
    zero = jnp.zeros((b, SSM_GROUPS, SSM_STATE), jnp.float32)
    ys_lat, ys_ctx = [], []
    for d in range(2):
        rev = d == 1
        fin = 0 if rev else -1
        a_re, a_im, bb_re, bb_im = _s5_discretize(p['ssm_lam_re'][d], p['ssm_lam_im'][d], p['ssm_log_step'][d],
                                                  p['ssm_b_re'][d], p['ssm_b_im'][d])
        hc_re, hc_im = _s5_scan(a_re, a_im, bb_re, bb_im, uc, zero, zero, rev)
        hl_re, hl_im = _s5_scan(a_re, a_im, bb_re, bb_im, ul, hc_re[:, fin], hc_im[:, fin], rev)
        ys_lat.append(_s5_readout(p['ssm_c_re'][d], p['ssm_c_im'][d], hl_re, hl_im))
        if not last:
            ys_ctx.append(_s5_readout(p['ssm_c_re'][d], p['ssm_c_im'][d], hc_re, hc_im))
    out_lat = _s5_output(ys_lat[0] + ys_lat[1], ul, p, u_lat.dtype)
    out_ctx = None if last else _s5_output(ys_ctx[0] + ys_ctx[1], uc, p, u_ctx.dtype)
    return out_lat, out_ctx


def _na_heads(t):
    return t.reshape(t.shape[0], t.shape[1], NA_HEADS, NA_HEAD_DIM)


def _na_context(q, k, v):
    s = jnp.einsum('bqhd,bkhd->bhqk', q, k).astype(jnp.float32) * NA_HEAD_DIM ** -0.5
    pr = jax.nn.softmax(s, axis=-1).astype(v.dtype)
    o = jnp.einsum('bhqk,bkhd->bqhd', pr, v)
    return o.reshape(o.shape[0], o.shape[1], BRANCH_W)


def _na_latent(q, k, v, kc, vc, rpb):
    b, length, nh, dh = q.shape
    rows = length // GRID_W
    kr = min(NA_WIN_ROWS, rows)
    kw = NA_WIN_COLS
    qg = q.reshape(b, rows, GRID_W, nh, dh)
    kg = k.reshape(b, rows, GRID_W, nh, dh)
    vg = v.reshape(b, rows, GRID_W, nh, dh)
    col = np.arange(GRID_W)
    col_idx = np.clip(col - kw // 2, 0, GRID_W - kw)[:, None] + np.arange(kw)[None, :]
    dc = col_idx - col[:, None] + (NA_WIN_COLS - 1)
    rpb = rpb.astype(jnp.float32)
    scale = NA_HEAD_DIM ** -0.5

    def one_row(r):
        start = jnp.clip(r - kr // 2, 0, rows - kr)
        k_rows = lax.dynamic_slice_in_dim(kg, start, kr, axis=1)
        v_rows = lax.dynamic_slice_in_dim(vg, start, kr, axis=1)
        k_win = k_rows[:, :, col_idx]
        v_win = v_rows[:, :, col_idx]
        q_row = lax.dynamic_index_in_dim(qg, r, axis=1, keepdims=False)
        dr = start + jnp.arange(kr) - r + (NA_WIN_ROWS - 1)
        bias = jnp.transpose(rpb[:, dr][:, :, dc], (0, 2, 1, 3))
        s_win = jnp.einsum('bchd,bjcwhd->bhcjw', q_row, k_win).astype(jnp.float32) * scale + bias
        s_ctx = jnp.einsum('bchd,bkhd->bhck', q_row, kc).astype(jnp.float32) * scale
        s = jnp.concatenate([s_win.reshape(b, nh, GRID_W, kr * kw), s_ctx], axis=-1)
        pr = jax.nn.softmax(s, axis=-1).astype(v.dtype)
        p_win = pr[..., :kr * kw].reshape(b, nh, GRID_W, kr, kw)
        p_ctx = pr[..., kr * kw:]
        return (jnp.einsum('bhcjw,bjcwhd->bchd', p_win, v_win)
                + jnp.einsum('bhck,bkhd->bchd', p_ctx, vc))

    out = lax.map(one_row, jnp.arange(rows))
    return jnp.transpose(out, (1, 0, 2, 3, 4)).reshape(b, length, BRANCH_W)


def _hyena_spectrum(length, p):
    f32 = jnp.float32
    t = jnp.linspace(0.0, 1.0, length, dtype=f32)[:, None]
    freqs = jnp.linspace(1e-4, HY_BANDS - 1, HY_BANDS, dtype=f32)
    ang = (2.0 * math.pi / length) * jnp.arange(length, dtype=f32)[:, None] * freqs[None, :]
    z = jnp.concatenate([t, jnp.cos(ang), -jnp.sin(ang)], axis=-1)
    fr = p['hy_freq'].astype(f32)
    h = jnp.sin(fr * (z @ p['hy_w1'].astype(f32) + p['hy_b1'].astype(f32)))
    h = jnp.sin(fr * (h @ p['hy_w2'].astype(f32) + p['hy_b2'].astype(f32)))
    h = jnp.sin(fr * (h @ p['hy_w3'].astype(f32) + p['hy_b3'].astype(f32)))
    h = (h @ p['hy_w4'].astype(f32)).reshape(length, HY_ORDER, 2, BRANCH_W)
    decay = jnp.exp(-t * jnp.abs(jnp.linspace(HY_MIN_DECAY, HY_MAX_DECAY, BRANCH_W, dtype=f32)))
    h = h * decay[:, None, None, :]
    filt = jnp.concatenate([h[:, :, 0], jnp.zeros((1, HY_ORDER, BRANCH_W), f32), h[:0:-1, :, 1]], axis=0)
    filt = filt / jnp.sum(jnp.abs(filt), axis=0, keepdims=True)
    return jnp.fft.rfft(filt, axis=0)


def _hyena_branch(u, p):
    length = u.shape[1]
    half = HY_SHORT // 2
    z = lax.conv_general_dilated(u, p['hy_conv_w'][:, None, :].astype(u.dtype), (1,), ((half, half),),
                                 dimension_numbers=('NWC', 'WIO', 'NWC'),
                                 feature_group_count=u.shape[-1]) + p['hy_conv_b'].astype(u.dtype)
    parts = jnp.split(z, HY_ORDER + 1, axis=-1)
    spec = _hyena_spectrum(length, p)
    y = parts[-1].astype(jnp.float32)
    for n in range(HY_ORDER):
        conv = jnp.fft.irfft(jnp.fft.rfft(y, n=2 * length, axis=1) * spec[None, :, n],
                             n=2 * length, axis=1)[:, :length]
        y = parts[n].astype(jnp.float32) * (conv + y * p['hy_bias'][n].astype(jnp.float32))
    return y.astype(u.dtype)


def _merge(h, branches, p):
    gates = jnp.split(jax.nn.sigmoid(h @ p['w_gate'] + p['b_gate']), N_BRANCH, axis=-1)
    mixed = gates[0] * (branches[0] @ p['w_branch'][0])
    for i in range(1, N_BRANCH):
        mixed = mixed + gates[i] * (branches[i] @ p['w_branch'][i])
    return mixed @ p['w_out']


def _ec_moe(h, p):
    b, length, _ = h.shape
    cap = max(1, EC_CAPACITY * length // N_EXPERTS)
    aff = jax.nn.softmax((h @ p['router']).astype(jnp.float32), axis=-1)
    gate, idx = lax.top_k(jnp.swapaxes(aff, 1, 2), cap)
    bidx = jnp.arange(b)[:, None, None]
    xs = h[bidx, idx]
    a = jnp.einsum('becd,edf->becf', xs, p['exp_w1'])
    g = jnp.einsum('becd,edf->becf', xs, p['exp_w3'])
    y = jnp.einsum('becf,efd->becd', jax.nn.silu(a) * g, p['exp_w2'])
    y = y * gate[..., None].astype(y.dtype)
    return jnp.zeros_like(h).at[bidx, idx].add(y)


def _layer(x, xc, c, c_ctx, p, last):
    w = BRANCH_W
    mod = (jax.nn.silu(c) @ p['w_ada'] + p['b_ada'])[:, None, :]
    mod_c = (jax.nn.silu(c_ctx) @ p['w_ada'] + p['b_ada'])[None, None, :]
    sh1, sc1, g1, sh2, sc2, g2 = jnp.split(mod, 6, axis=-1)
    sh1c, sc1c, g1c, sh2c, sc2c, g2c = jnp.split(mod_c, 6, axis=-1)

    h = _modulate(_rmsnorm(x, p['norm1']), sh1, sc1)
    hc = _modulate(_rmsnorm(xc, p['norm1']), sh1c, sc1c)
    proj = h @ p['w_in']
    projc = hc @ p['w_in']

    ssm_l, ssm_c = _s5_branch(projc[..., :w], proj[..., :w], p, last)

    ql, kl, vl = [_na_heads(t) for t in jnp.split(proj[..., w:4 * w], 3, axis=-1)]
    qc, kc, vc = [_na_heads(t) for t in jnp.split(projc[..., w:4 * w], 3, axis=-1)]
    kc = _rmsnorm(kc, p['na_k_gain'])
    na_l = _na_latent(_rmsnorm(ql, p['na_q_gain']), _rmsnorm(kl, p['na_k_gain']), vl, kc, vc, p['na_rpb'])

    hy_l = _hyena_branch(proj[..., 4 * w:], p)

    x = x + g1 * _merge(h, (ssm_l, na_l, hy_l), p)
    x = x + g2 * _ec_moe(_modulate(_rmsnorm(x, p['norm2']), sh2, sc2), p)
    if last:
        return x, None
    na_c = _na_context(_rmsnorm(qc, p['na_q_gain']), kc, vc)
    hy_c = _hyena_branch(projc[..., 4 * w:], p)
    xc = xc + g1c * _merge(hc, (ssm_c, na_c, hy_c), p)
    xc = xc + g2c * _ec_moe(_modulate(_rmsnorm(xc, p['norm2']), sh2c, sc2c), p)
    return x, xc


def setup_inputs(seed: int = 0) -> dict:
    key = jax.random.key(seed)
    ks = iter(jax.random.split(key, 48))
    f32 = jnp.float32
    D, W, G, P, K, L_ = D_MODEL, BRANCH_W, SSM_GROUPS, SSM_STATE, SSM_GROUP, DEPTH

    def nrm(shape, scale):
        return jax.random.normal(next(ks), shape, f32) * scale

    inp = {}
    inp['x'] = nrm((BATCH, SEQ, D), 1.0)
    inp['c'] = nrm((BATCH, D), 1.0)
    inp['ctx'] = nrm((BATCH, CTX_LEN, D), 1.0)
    inp['c_ctx'] = nrm((D,), 1.0)
    inp['w_ada'] = nrm((L_, D, 6 * D), D ** -0.5)
    inp['b_ada'] = nrm((L_, 6 * D), 0.02)
    inp['norm1'] = 1.0 + nrm((L_, D), 0.02)
    inp['norm2'] = 1.0 + nrm((L_, D), 0.02)
    inp['w_in'] = nrm((L_, D, IN_WIDTH), D ** -0.5)
    inp['ssm_lam_re'] = -0.5 + nrm((L_, 2, G, P), 0.01)
    inp['ssm_lam_im'] = math.pi * jnp.arange(P, dtype=f32) + nrm((L_, 2, G, P), 0.01)
    inp['ssm_log_step'] = jax.random.uniform(next(ks), (L_, 2, G), f32, math.log(SSM_DT_MIN), math.log(SSM_DT_MAX))
    inp['ssm_b_re'] = nrm((L_, 2, G, P, K), (2 * K) ** -0.5)
    inp['ssm_b_im'] = nrm((L_, 2, G, P, K), (2 * K) ** -0.5)
    inp['ssm_c_re'] = nrm((L_, 2, G, K, P), (2 * P) ** -0.5)
    inp['ssm_c_im'] = nrm((L_, 2, G, K, P), (2 * P) ** -0.5)
    inp['ssm_d'] = nrm((L_, W), 1.0)
    inp['ssm_w_glu'] = nrm((L_, W, W), W ** -0.5)
    inp['na_q_gain'] = 1.0 + nrm((L_, NA_HEAD_DIM), 0.02)
    inp['na_k_gain'] = 1.0 + nrm((L_, NA_HEAD_DIM), 0.02)
    inp['na_rpb'] = nrm((L_, NA_HEADS, 2 * NA_WIN_ROWS - 1, 2 * NA_WIN_COLS - 1), 0.1)
    inp['hy_conv_w'] = nrm((L_, HY_SHORT, (HY_ORDER + 1) * W), HY_SHORT ** -0.5)
    inp['hy_conv_b'] = nrm((L_, (HY_ORDER + 1) * W), 0.02)
    inp['hy_w1'] = nrm((L_, HY_EMB, HY_FILTER_W), HY_EMB ** -0.5)
    inp['hy_b1'] = nrm((L_, HY_FILTER_W), 0.1)
    inp['hy_w2'] = nrm((L_, HY_FILTER_W, HY_FILTER_W), HY_FILTER_W ** -0.5)
    inp['hy_b2'] = nrm((L_, HY_FILTER_W), 0.1)
    inp['hy_w3'] = nrm((L_, HY_FILTER_W, HY_FILTER_W), HY_FILTER_W ** -0.5)
    inp['hy_b3'] = nrm((L_, HY_FILTER_W), 0.1)
    inp['hy_w4'] = nrm((L_, HY_FILTER_W, HY_ORDER * 2 * W), HY_FILTER_W ** -0.5)
    inp['hy_freq'] = 1.0 + nrm((L_, HY_FILTER_W), 0.01)
    inp['hy_bias'] = nrm((L_, HY_ORDER, W), 0.5)
    inp['w_gate'] = nrm((L_, D, N_BRANCH * D), D ** -0.5)
    inp['b_gate'] = nrm((L_, N_BRANCH * D), 0.02)
    inp['w_branch'] = nrm((L_, N_BRANCH, W, D), W ** -0.5)
    inp['w_out'] = nrm((L_, D, D), D ** -0.5)
    inp['router'] = nrm((L_, D, N_EXPERTS), D ** -0.5)
    inp['exp_w1'] = nrm((L_, N_EXPERTS, D, EXPERT_FF), D ** -0.5)
    inp['exp_w3'] = nrm((L_, N_EXPERTS, D, EXPERT_FF), D ** -0.5)
    inp['exp_w2'] = nrm((L_, N_EXPERTS, EXPERT_FF, D), EXPERT_FF ** -0.5)
    return inp


def reference(x, c, ctx, c_ctx, w_ada, b_ada, norm1, norm2, w_in,
              ssm_lam_re, ssm_lam_im, ssm_log_step, ssm_b_re, ssm_b_im, ssm_c_re, ssm_c_im, ssm_d, ssm_w_glu,
              na_q_gain, na_k_gain, na_rpb,
              hy_conv_w, hy_conv_b, hy_w1, hy_b1, hy_w2, hy_b2, hy_w3, hy_b3, hy_w4, hy_freq, hy_bias,
              w_gate, b_gate, w_branch, w_out,
              router, exp_w1, exp_w3, exp_w2):
    xc = ctx
    for l in range(DEPTH):
        p = {
            'w_ada': w_ada[l], 'b_ada': b_ada[l], 'norm1': norm1[l], 'norm2': norm2[l], 'w_in': w_in[l],
            'ssm_lam_re': ssm_lam_re[l], 'ssm_lam_im': ssm_lam_im[l], 'ssm_log_step': ssm_log_step[l],
            'ssm_b_re': ssm_b_re[l], 'ssm_b_im': ssm_b_im[l], 'ssm_c_re': ssm_c_re[l], 'ssm_c_im': ssm_c_im[l],
            'ssm_d': ssm_d[l], 'ssm_w_glu': ssm_w_glu[l],
            'na_q_gain': na_q_gain[l], 'na_k_gain': na_k_gain[l], 'na_rpb': na_rpb[l],
            'hy_conv_w': hy_conv_w[l], 'hy_conv_b': hy_conv_b[l], 'hy_w1': hy_w1[l], 'hy_b1': hy_b1[l],
            'hy_w2': hy_w2[l], 'hy_b2': hy_b2[l], 'hy_w3': hy_w3[l], 'hy_b3': hy_b3[l], 'hy_w4': hy_w4[l],
            'hy_freq': hy_freq[l], 'hy_bias': hy_bias[l],
            'w_gate': w_gate[l], 'b_gate': b_gate[l], 'w_branch': w_branch[l], 'w_out': w_out[l],
            'router': router[l], 'exp_w1': exp_w1[l], 'exp_w3': exp_w3[l], 'exp_w2': exp_w2[l],
        }
        x, xc = _layer(x, xc, c, c_ctx, p, l == DEPTH - 1)
    return x
```

```python
import numpy as np
import concourse.bass as bass
import concourse.mybir as mybir
from concourse.bass_utils import run_bass_kernel_spmd
from contextlib import ExitStack

F32 = mybir.dt.float32
BF16 = mybir.dt.bfloat16
U32 = mybir.dt.uint32
I32 = mybir.dt.int32
ALU = mybir.AluOpType
AF = mybir.ActivationFunctionType
AX = mybir.AxisListType

D = 2048
L = 8192
CT = 256
AT = L + CT
NT = AT // 128
KT = D // 128
W = 512
INW = 3584
DEPTH = 2
EPS = 1e-6
PI = float(np.pi)


class Buf:
    __slots__ = ("t", "last_w", "readers", "name", "space")

    def __init__(self, t, name="", space="sb"):
        self.t = t
        self.last_w = None
        self.readers = []
        self.name = name
        self.space = space

    def __getitem__(self, k):
        return self.t[k]


class _LV:
    def __init__(self, views):
        self.views = views

    def __getitem__(self, k):
        if isinstance(k, tuple):
            v = self.views[k[0]]
            return v[k[1:]] if len(k) > 1 else v
        return self.views[k]


class LayerView(Buf):
    __slots__ = ()

    def __init__(self, views, name):
        Buf.__init__(self, _LV(views), name, "dram")


class FW:
    ENG = ("pe", "act", "dve", "pool", "sp")

    def __init__(self, nc, es):
        self.nc = nc
        self.es = es
        self.root_es = es
        self.eng = {"pe": nc.tensor, "act": nc.scalar, "dve": nc.vector, "pool": nc.gpsimd, "sp": nc.sync}
        self.esem = {k: es.enter_context(nc.semaphore("es_" + k)) for k in self.ENG}
        self.ecnt = {k: 0 for k in self.ENG}
        self.dslots = []
        self.dcnt = []
        self.kmap = {}
        self.seen = {k: {} for k in self.ENG}
        self.n_inst = 0
        self.bufs = []
        self.uniq = 0

    def _reg(self, b):
        self.bufs.append(b)
        return b

    def sb(self, name, shape, dt=F32):
        self.uniq += 1
        name = "%s_%d" % (name, self.uniq)
        return self._reg(Buf(self.es.enter_context(self.nc.sbuf_tensor(name, list(shape), dt)), name, "sb"))

    def ps(self, name, shape, dt=F32):
        return self._reg(Buf(self.es.enter_context(self.nc.psum_tensor(name, list(shape), dt)), name, "ps"))

    def dram(self, name, shape, dt=F32, kind="Internal"):
        return self._reg(Buf(self.nc.dram_tensor(name, list(shape), dt, kind=kind).ap(), name, "dram"))

    def _slot(self, key):
        if key not in self.kmap:
            i = len(self.kmap)
            if i >= len(self.dslots):
                self.dslots.append(self.root_es.enter_context(self.nc.semaphore("ds%d" % i)))
                self.dcnt.append(0)
            self.kmap[key] = i
        return self.kmap[key]

    def _wait(self, e, ev):
        if ev is None:
            return
        kind, key, val = ev
        if kind == "d":
            val = self.dcnt[key]
            sem = self.dslots[key]
        else:
            sem = self.esem[key]
        k = (kind, key)
        if self.seen[e].get(k, 0) >= val:
            return
        self.seen[e][k] = val
        self.eng[e].wait_ge(sem, val)

    def _deps(self, e, reads, writes):
        for r in reads:
            self._wait(e, r.last_w)
        for w in writes:
            self._wait(e, w.last_w)
            for ev in w.readers:
                self._wait(e, ev)

    def _commit(self, ev, reads, writes):
        for r in reads:
            r.readers.append(ev)
            if len(r.readers) > 48:
                d = {}
                for x in r.readers:
                    k = (x[0], x[1])
                    if k not in d or d[k][2] < x[2]:
                        d[k] = x
                r.readers = list(d.values())
        for w in writes:
            w.last_w = ev
            w.readers = []

    def op(self, e, fn, reads=(), writes=()):
        self._deps(e, reads, writes)
        ins = fn(self.eng[e])
        self.ecnt[e] += 1
        ins.then_inc(self.esem[e], 1)
        ev = ("e", e, self.ecnt[e])
        self._commit(ev, reads, writes)
        self.n_inst += 1
        return ev

    def _stream(self, reads, writes, key):
        for b in list(writes) + list(reads):
            if b.space == "sb":
                return self._slot("b_" + b.name)
        if key is None:
            self.uniq += 1
            key = "u%d" % self.uniq
        return self._slot("k_" + key)

    def _issued(self, slot, ins, inc, reads, writes):
        self.dcnt[slot] += inc
        ins.then_inc(self.dslots[slot], inc)
        ev = ("d", slot, self.dcnt[slot])
        self._commit(ev, reads, writes)
        self.n_inst += 1
        return ev

    def dma(self, q, out, in_, reads=(), writes=(), key=None, **kw):
        slot = self._stream(reads, writes, key)
        self._deps(q, reads, writes)
        ins = self.eng[q].dma_start(out=out, in_=in_, **kw)
        return self._issued(slot, ins, 16, reads, writes)

    def idma(self, out, in_, out_offset=None, in_offset=None, reads=(), writes=(), key=None, **kw):
        slot = self._stream([r for r in reads if r.name != "idxT"], writes, None)
        self._deps("pool", reads, writes)
        ins = self.nc.gpsimd.indirect_dma_start(out=out, out_offset=out_offset, in_=in_, in_offset=in_offset, **kw)
        return self._issued(slot, ins, 16, reads, writes)

    def allgather(self, out_buf, out_ap, in_buf, in_ap):
        slot = self._stream([], [], None)
        self._deps("pool", [in_buf], [out_buf])
        ins = self.nc.gpsimd.collective_compute("AllGather", ALU.bypass, replica_groups=[list(range(8))], ins=[in_ap], outs=[out_ap])
        return self._issued(slot, ins, 1, [in_buf], [out_buf])

    def barrier(self):
        for e in self.ENG:
            for o in self.ENG:
                if o != e and self.ecnt[o] > 0:
                    self._wait(e, ("e", o, self.ecnt[o]))
            for slot in range(len(self.dslots)):
                if self.dcnt[slot] > 0:
                    self._wait(e, ("d", slot, self.dcnt[slot]))
        for b in self.bufs:
            b.last_w = None
            b.readers = []
        self.kmap = {}

    def finish(self, bufs):
        for slot in range(len(self.dslots)):
            if self.dcnt[slot] > 0:
                self._wait("sp", ("d", slot, self.dcnt[slot]))


def dap(buf, offset, pattern):
    return bass.AP(tensor=buf.t.tensor, offset=offset, ap=[list(p) for p in pattern])


class K:
    def __init__(self, nc, es, stop_after=None, dbg=None, gather=True, test_br=False, wdepth=DEPTH):
        self.nc = nc
        self.gather = gather
        self.test_br = test_br
        self.dumps_on = test_br
        self.fw = FW(nc, es)
        self.stop_after = stop_after
        self.dbg = dbg or []
        fw = self.fw
        shapes = {
            "x": [L, D], "ctx": [CT, D], "cvec": [2, D],
            "w_ada": [DEPTH, D, 6 * D], "b_ada": [DEPTH, 6 * D], "norm1": [DEPTH, D], "norm2": [DEPTH, D],
            "w_in": [DEPTH, D, INW], "w_gate": [DEPTH, D, 3 * D], "b_gate": [DEPTH, 3 * D],
            "w_branch": [DEPTH, 3, W, D], "w_out": [DEPTH, D, D], "router": [DEPTH, D, 16],
            "exp_w1": [DEPTH, 16, D, D], "exp_w3": [DEPTH, 16, D, D], "exp_w2": [DEPTH, 16, D, D],
            "ssm_lam_re": [DEPTH, 2, 32, 64], "ssm_lam_im": [DEPTH, 2, 32, 64], "ssm_log_step": [DEPTH, 2, 32],
            "ssm_b_re": [DEPTH, 2, 32, 64, 16], "ssm_b_im": [DEPTH, 2, 32, 64, 16],
            "ssm_c_re": [DEPTH, 2, 32, 16, 64], "ssm_c_im": [DEPTH, 2, 32, 16, 64],
            "ssm_d": [DEPTH, 512], "ssm_w_glu": [DEPTH, 512, 512],
            "na_q_gain": [DEPTH, 64], "na_k_gain": [DEPTH, 64], "na_tab": [DEPTH, 8, 128, 8, 4, 64],
            "hy_conv_w": [DEPTH, 3, 1536], "hy_conv_b": [DEPTH, 1536], "hy_w1": [DEPTH, 33, 64], "hy_b1": [DEPTH, 64],
            "hy_w2": [DEPTH, 64, 64], "hy_b2": [DEPTH, 64], "hy_w3": [DEPTH, 64, 64], "hy_b3": [DEPTH, 64],
            "hy_w4": [DEPTH, 64, 2048], "hy_freq": [DEPTH, 64], "hy_bias": [DEPTH, 2, 512],
            "hyc_F": [128, 3, 128], "hyc_T": [128, 2, 128], "hyc_zL": [33, L], "hyc_zc": [33, CT], "hyc_dL": [W, L], "hyc_dc": [W, CT],
        }

        BIG = set(BIGW)
        gather_mode = self.gather

        class Lazy(dict):
            def __missing__(s_, n):
                shp = shapes[n]
                if n in BIG and gather_mode:
                    R = int(np.prod(shp[1:-1])); C = shp[-1]
                    ext = fw.dram(n + "_sh", [DEPTH, R // 8, C], F32, kind="ExternalInput")
                    views = []
                    for l in range(DEPTH):
                        shl = fw.dram(n + "_shi%d" % l, [R // 8, C], F32)
                        fl = fw.dram(n + "_full%d" % l, [R, C], F32)
                        for r0 in range(0, R // 8, 128):
                            r1 = min(r0 + 128, R // 8)
                            fw.dma("sp" if (r0 // 128) % 2 else "act", shl.t[r0:r1, :], ext.t[l, r0:r1, :], reads=[ext], writes=[shl], key="shcp_%s%d" % (n, l))
                        fw.allgather(fl, fl.t, shl, shl.t)
                        views.append(fl.t.rearrange("(a r) c -> a r c", a=shp[1]) if len(shp) == 4 else fl.t)
                    b = LayerView(views, n)
                    fw._reg(b)
                    s_[n] = b
                    return b
                if n in BIG:
                    shp = [wdepth] + list(shp[1:])
                b = fw.dram(n, shp, F32, kind="ExternalInput")
                s_[n] = b
                return b
        d = Lazy()
        self.d = d
        if self.gather:
            for n in BIGW:
                d[n]
            fw.barrier()
        s = {}
        s["modD"] = fw.dram("modD", [2, 6 * D])
        s["hT"] = fw.dram("hT", [128, KT, AT])
        s["projT"] = fw.dram("projT", [INW, AT])
        s["vtok"] = fw.dram("vtok", [AT, W])
        s["brT"] = fw.dram("brT", [3 * W, AT])
        s["s5z"] = fw.dram("s5z", [W, AT])
        s["qnT"] = fw.dram("qnT", [W, AT]); s["knT"] = fw.dram("knT", [W, AT])
        s["hzT"] = fw.dram("hzT", [3 * W, AT]); s["hfT"] = fw.dram("hfT", [2048, L]); s["hfTc"] = fw.dram("hfTc", [2048, CT])
        s["hnrm"] = fw.dram("hnrm", [2, 2, W])
        s["hspec"] = fw.dram("hspec", [2, 128, 128, 2, 512]); s["hspecc"] = fw.dram("hspecc", [2, 128, 128, 2, 512])
        s["xa"] = fw.dram("xa", [AT, D])
        s["h2"] = fw.dram("h2", [AT, D])
        self.s = s
        self.out = fw.dram("out", [L, D], kind="ExternalOutput")
        self.dbg_out = {}
        self.ident = fw.sb("ident", [128, 128])
        io = fw.sb("iota_i", [128, 128], I32)
        fw.op("pool", lambda e: e.iota(io[:], pattern=[[1, 128]], base=0, channel_multiplier=-1), writes=[io])
        fw.op("dve", lambda e: e.tensor_single_scalar(out=self.ident[:], in_=io[:], scalar=0, op=ALU.is_equal), reads=[io], writes=[self.ident])
        self.pb = [fw.ps("pb%d" % i, [128, 512]) for i in range(8)]
        self.pbi = 0

    def dump(self, name, buf, ap):
        if not self.dumps_on:
            return
        o = self.fw.dram("dbg_" + name, list(ap.shape), kind="ExternalOutput")
        self.fw.dma("sp", o.t, ap, reads=[buf], writes=[o])
        self.dbg_out[name] = o

    def bank(self):
        b = self.pb[self.pbi % 8]
        self.pbi += 1
        return b

    def p0_mod(self, l, es):
        fw, d, s = self.fw, self.d, self.s
        cv = fw.sb("cv", [2, D]); sil2 = fw.sb("silc2", [128, KT, 2])
        wa = [fw.sb("wada%d" % i, [128, KT, 512]) for i in range(2)]
        bb = fw.sb("bada", [2, 512]); mo = [fw.sb("modo%d" % r, [1, 512]) for r in range(2)]
        fw.dma("sp", cv[:], d["cvec"].t, reads=[d["cvec"]], writes=[cv], key="small")
        pbt = self.bank()
        for kt in range(KT):
            fw.op("pe", lambda e, kt=kt: e.transpose(out=pbt[:, kt * 2:(kt + 1) * 2], in_=cv[0:2, kt * 128:(kt + 1) * 128], identity=self.ident[0:2, 0:2]),
                  reads=[cv, self.ident], writes=[pbt])
        fw.op("act", lambda e: e.activation(out=sil2[:].rearrange("p k r -> p (k r)"), in_=pbt[:, 0:32], func=AF.Silu), reads=[pbt], writes=[sil2])
        for j in range(24):
            wb = wa[j % 2]
            src = d["w_ada"].t[l, :, j * 512:(j + 1) * 512].rearrange("(kt p) n -> p kt n", p=128)
            fw.dma("sp" if j % 2 == 0 else "act", wb[:], src, reads=[d["w_ada"]], writes=[wb], key="wada%d" % (j % 2))
            fw.dma("pool", bb[:], dap(d["b_ada"], l * 6 * D + j * 512, [[0, 2], [1, 512]]), reads=[d["b_ada"]], writes=[bb], key="small")
            for r in range(2):
                pb = self.bank()
                for kt in range(KT):
                    fw.op("pe", lambda e, kt=kt: e.matmul(pb[0:1, :], sil2[:, kt, r:r + 1], wb[:, kt, :], start=(kt == 0), stop=(kt == KT - 1)),
                          reads=[sil2, wb], writes=[pb])
                fw.op("dve", lambda e: e.tensor_tensor(out=mo[r][:], in0=pb[0:1, :], in1=bb[0:1, :], op=ALU.add), reads=[pb, bb], writes=[mo[r]])
                fw.dma("sp", s["modD"].t[r:r + 1, j * 512:(j + 1) * 512], mo[r][:], reads=[mo[r]], writes=[s["modD"]], key="modst")

    def load_bcast(self, q, dst, src_buf, offset, n, key=None, ap=None):
        ap = dst[:] if ap is None else ap
        self.fw.dma(q, ap, dap(src_buf, offset, [[0, ap.shape[0]], [1, n]]), reads=[src_buf], writes=[dst], key=key)

    def p1_proj(self, l, es, xin):
        fw, d, s = self.fw, self.d, self.s
        A = [fw.sb("A1_%d" % r, [128, D]) for r in range(2)]
        Bv = [fw.sb("B1_%d" % r, [128, D]) for r in range(2)]
        tmp = fw.sb("p1tmp", [128, D])
        self.load_bcast("sp", tmp, d["norm1"], l * D, D, "small")
        for r in range(2):
            self.load_bcast("act", A[r], s["modD"], r * 6 * D + 1 * D, D, "small")
            self.load_bcast("pool", Bv[r], s["modD"], r * 6 * D + 0 * D, D, "small")
            fw.op("dve", lambda e, r=r: e.scalar_tensor_tensor(out=A[r][:], in0=A[r][:], scalar=1.0, in1=tmp[:], op0=ALU.add, op1=ALU.mult),
                  reads=[A[r], tmp], writes=[A[r]])
        xt = [fw.sb("xt%d" % i, [128, D]) for i in range(2)]
        ht = [fw.sb("ht%d" % i, [128, D]) for i in range(2)]
        st = fw.sb("p1st", [128, 4])
        hTb = [fw.sb("hTb%d" % i, [128, KT, 512]) for i in range(1)]
        wch = [fw.sb("wch%d" % i, [128, KT, 512]) for i in range(2)]
        stg = [fw.sb("stg%d" % i, [128, 512]) for i in range(3)]
        nblk = 17
        wi = 0
        si = 0
        for blk in range(nblk):
            ntile = 4 if blk < 16 else 2
            ntok = ntile * 128
            hb = hTb[0]
            r = 0 if blk < 16 else 1
            for ti in range(ntile):
                tt = blk * 4 + ti
                xb = xt[tt % 2]; hh = ht[tt % 2]
                if isinstance(xin, tuple):
                    src = xin[0].t[tt * 128:(tt + 1) * 128, :] if tt < 64 else xin[1].t[(tt - 64) * 128:(tt - 63) * 128, :]
                    srcb = xin[0] if tt < 64 else xin[1]
                else:
                    src = xin.t[tt * 128:(tt + 1) * 128, :]; srcb = xin
                fw.dma("sp", xb[:], src, reads=[srcb], writes=[xb], key="xld%d" % (tt % 2))
                fw.op("act", lambda e: e.activation(out=hh[:], in_=xb[:], func=AF.Square, accum_out=st[:, 0:1]), reads=[xb], writes=[hh, st])
                fw.op("act", lambda e: e.activation(out=st[:, 1:2], in_=st[:, 0:1], func=AF.Sqrt, scale=1.0 / D, bias=EPS), reads=[st], writes=[st])
                fw.op("dve", lambda e: e.reciprocal(out=st[:, 2:3], in_=st[:, 1:2]), reads=[st], writes=[st])
                fw.op("dve", lambda e: e.scalar_tensor_tensor(out=hh[:], in0=xb[:], scalar=st[:, 2:3], in1=A[r][:], op0=ALU.mult, op1=ALU.mult),
                      reads=[xb, st, A[r]], writes=[hh])
                fw.op("pool", lambda e: e.tensor_tensor(out=hh[:], in0=hh[:], in1=Bv[r][:], op=ALU.add), reads=[hh, Bv[r]], writes=[hh])
                for q4 in range(4):
                    pb = self.bank()
                    for j in range(4):
                        kt = q4 * 4 + j
                        fw.op("pe", lambda e, kt=kt, j=j: e.transpose(out=pb[:, j * 128:(j + 1) * 128], in_=hh[:, kt * 128:(kt + 1) * 128], identity=self.ident[:]),
                              reads=[hh, self.ident], writes=[pb])
                    eng = "act" if q4 % 2 == 0 else "dve"
                    if eng == "act":
                        fw.op("act", lambda e: e.activation(out=hb[:, q4 * 4:(q4 + 1) * 4, ti * 128:(ti + 1) * 128],
                                                            in_=pb[:].rearrange("p (j t) -> p j t", j=4), func=AF.Copy), reads=[pb], writes=[hb])
                    else:
                        fw.op("dve", lambda e: e.tensor_copy(out=hb[:, q4 * 4:(q4 + 1) * 4, ti * 128:(ti + 1) * 128],
                                                             in_=pb[:].rearrange("p (j t) -> p j t", j=4)), reads=[pb], writes=[hb])
            for q in range(4):
                fw.dma("sp" if q % 2 else "act", s["hT"].t[:, q * 4:(q + 1) * 4, blk * 512: blk * 512 + ntok], hb[:, q * 4:(q + 1) * 4, 0:ntok], reads=[hb], writes=[s["hT"]], key="hTst")
            if blk == 0 and l == 0:
                self.dump("hb_sb", hb, hb[:, 0, 0:128])
                self.dump("hT_dr", s["hT"], s["hT"].t[:, 0, 0:128])
            for cc in range(7):
                wb = wch[wi % 2]; wi += 1
                src = d["w_in"].t[l, :, cc * 512:(cc + 1) * 512].rearrange("(kt p) n -> p kt n", p=128)
                fw.dma("act" if wi % 2 else "sp", wb[:], src, reads=[d["w_in"]], writes=[wb], key="wch%d" % (wi % 2))
                if cc == 3:
                    for ti in range(ntile):
                        pb = self.bank()
                        for kt in range(KT):
                            fw.op("pe", lambda e, kt=kt: e.matmul(pb[:], hb[:, kt, ti * 128:(ti + 1) * 128], wb[:, kt, :], start=(kt == 0), stop=(kt == KT - 1)),
                                  reads=[hb, wb], writes=[pb])
                        sg = stg[si % 3]; si += 1
                        fw.op("act", lambda e: e.activation(out=sg[:], in_=pb[:], func=AF.Copy), reads=[pb], writes=[sg])
                        t0 = blk * 512 + ti * 128
                        fw.dma("pool", s["vtok"].t[t0:t0 + 128, :], sg[:], reads=[sg], writes=[s["vtok"]], key="pst")
                    continue
                for ct in range(4):
                    pb = self.bank()
                    for kt in range(KT):
                        fw.op("pe", lambda e, kt=kt: e.matmul(pb[:, 0:ntok], wb[:, kt, ct * 128:(ct + 1) * 128], hb[:, kt, 0:ntok], start=(kt == 0), stop=(kt == KT - 1)),
                              reads=[hb, wb], writes=[pb])
                    sg = stg[si % 3]; si += 1
                    if si % 2:
                        fw.op("act", lambda e: e.activation(out=sg[:, 0:ntok], in_=pb[:, 0:ntok], func=AF.Copy), reads=[pb], writes=[sg])
                    else:
                        fw.op("dve", lambda e: e.tensor_copy(out=sg[:, 0:ntok], in_=pb[:, 0:ntok]), reads=[pb], writes=[sg])
                    c0 = cc * 512 + ct * 128
                    fw.dma("pool", s["projT"].t[c0:c0 + 128, blk * 512: blk * 512 + ntok], sg[:, 0:ntok], reads=[sg], writes=[s["projT"]], key="pst")

    def sin_reduced(self, out, ang, tmp_i, tmp_f, bufs_r, bufs_w):
        fw = self.fw
        fw.op("dve", lambda e: e.tensor_scalar(out=tmp_i, in0=ang, scalar1=1.0 / (2 * PI), scalar2=None, op0=ALU.mult), reads=bufs_r, writes=bufs_w)
        fw.op("dve", lambda e: e.tensor_copy(out=tmp_f, in_=tmp_i), reads=bufs_w, writes=bufs_w)
        fw.op("dve", lambda e: e.scalar_tensor_tensor(out=ang, in0=tmp_f, scalar=-2 * PI, in1=ang, op0=ALU.mult, op1=ALU.add), reads=bufs_w + bufs_r, writes=bufs_r)
        fw.op("dve", lambda e: e.tensor_scalar(out=ang, in0=ang, scalar1=PI, scalar2=-PI, op0=ALU.min, op1=ALU.max), reads=bufs_r, writes=bufs_r)
        fw.op("act", lambda e: e.activation(out=out, in_=ang, func=AF.Sin), reads=bufs_r, writes=bufs_w)

    def p2a_s5(self, l, es):
        fw, d, s = self.fw, self.d, self.s
        T = AT
        NG = 32
        ld = fw.sb("s5_ld", [32, 3, 128])
        fw.dma("sp", ld[:, 0, :], d["ssm_lam_re"].t[l].rearrange("d (gp g2) p -> (d gp) (g2 p)", g2=2), reads=[d["ssm_lam_re"]], writes=[ld])
        fw.dma("sp", ld[:, 1, :], d["ssm_lam_im"].t[l].rearrange("d (gp g2) p -> (d gp) (g2 p)", g2=2), reads=[d["ssm_lam_im"]], writes=[ld])
        ls = fw.sb("s5_ls", [32, 2])
        fw.dma("sp", ls[:], d["ssm_log_step"].t[l].rearrange("d (gp g2) -> (d gp) g2", g2=2), reads=[d["ssm_log_step"]], writes=[ls])
        fw.op("dve", lambda e: e.tensor_copy(out=ld[:, 2, :].rearrange("a (g p) -> a g p", g=2), in_=ls[:].unsqueeze(2).to_broadcast([32, 2, 64])), reads=[ls], writes=[ld])
        par = fw.sb("s5_par", [128, 24, 32])
        P_ = lambda i: par[:, i, :]
        for i in range(3):
            pb = self.bank()
            fw.op("pe", lambda e, i=i: e.transpose(out=pb[:, 0:32], in_=ld[:, i, :], identity=self.ident[0:32, 0:32]), reads=[ld, self.ident], writes=[pb])
            fw.op("dve", lambda e, i=i: e.tensor_copy(out=P_(i), in_=pb[:, 0:32]), reads=[pb], writes=[par])
        LR, LI, STEP, MAG, ANG, ARE, AIM, TMP, DEN, NRE, CRE, CIM, T2, ANG2 = range(14)
        pi_ = fw.sb("s5_pari", [128, 32], I32)
        R, Wp = [par], [par, pi_]
        op = lambda eng, fn: fw.op(eng, fn, reads=[par, pi_], writes=[par, pi_])
        op("act", lambda e: e.activation(out=P_(STEP), in_=P_(2), func=AF.Exp))
        op("dve", lambda e: e.tensor_tensor(out=P_(MAG), in0=P_(LR), in1=P_(STEP), op=ALU.mult))
        op("act", lambda e: e.activation(out=P_(MAG), in_=P_(MAG), func=AF.Exp))
        op("dve", lambda e: e.tensor_tensor(out=P_(ANG), in0=P_(LI), in1=P_(STEP), op=ALU.mult))
        op("dve", lambda e: e.tensor_scalar(out=P_(ANG2), in0=P_(ANG), scalar1=PI / 2, scalar2=None, op0=ALU.add))
        self.sin_reduced(P_(AIM), P_(ANG), pi_[:], P_(TMP), [par], [par, pi_])
        self.sin_reduced(P_(ARE), P_(ANG2), pi_[:], P_(TMP), [par], [par, pi_])
        op("dve", lambda e: e.tensor_tensor(out=P_(ARE), in0=P_(ARE), in1=P_(MAG), op=ALU.mult))
        op("dve", lambda e: e.tensor_tensor(out=P_(AIM), in0=P_(AIM), in1=P_(MAG), op=ALU.mult))
        op("dve", lambda e: e.tensor_tensor(out=P_(DEN), in0=P_(LR), in1=P_(LR), op=ALU.mult))
        op("dve", lambda e: e.tensor_tensor(out=P_(TMP), in0=P_(LI), in1=P_(LI), op=ALU.mult))
        op("dve", lambda e: e.tensor_tensor(out=P_(DEN), in0=P_(DEN), in1=P_(TMP), op=ALU.add))
        op("dve", lambda e: e.reciprocal(out=P_(DEN), in_=P_(DEN)))
        op("dve", lambda e: e.tensor_scalar(out=P_(NRE), in0=P_(ARE), scalar1=-1.0, scalar2=None, op0=ALU.add))
        op("dve", lambda e: e.tensor_tensor(out=P_(CRE), in0=P_(NRE), in1=P_(LR), op=ALU.mult))
        op("dve", lambda e: e.tensor_tensor(out=P_(TMP), in0=P_(AIM), in1=P_(LI), op=ALU.mult))
        op("dve", lambda e: e.tensor_tensor(out=P_(CRE), in0=P_(CRE), in1=P_(TMP), op=ALU.add))
        op("dve", lambda e: e.tensor_tensor(out=P_(CRE), in0=P_(CRE), in1=P_(DEN), op=ALU.mult))
        op("dve", lambda e: e.tensor_tensor(out=P_(CIM), in0=P_(AIM), in1=P_(LR), op=ALU.mult))
        op("dve", lambda e: e.tensor_tensor(out=P_(TMP), in0=P_(NRE), in1=P_(LI), op=ALU.mult))
        op("dve", lambda e: e.tensor_tensor(out=P_(CIM), in0=P_(CIM), in1=P_(TMP), op=ALU.subtract))
        op("dve", lambda e: e.tensor_tensor(out=P_(CIM), in0=P_(CIM), in1=P_(DEN), op=ALU.mult))
        NS = 14
        pw = fw.sb("s5_pw", [128, NS, 3, 32])
        fw.op("dve", lambda e: e.tensor_copy(out=pw[:, 0, 0, :], in_=P_(ARE)), reads=[par], writes=[pw])
        fw.op("dve", lambda e: e.tensor_copy(out=pw[:, 0, 1, :], in_=P_(AIM)), reads=[par], writes=[pw])
        for k in range(1, NS):
            fw.op("dve", lambda e, k=k: e.tensor_tensor(out=P_(TMP), in0=pw[:, k - 1, 0, :], in1=pw[:, k - 1, 0, :], op=ALU.mult), reads=[pw, par], writes=[par])
            fw.op("dve", lambda e, k=k: e.tensor_tensor(out=P_(T2), in0=pw[:, k - 1, 1, :], in1=pw[:, k - 1, 1, :], op=ALU.mult), reads=[pw, par], writes=[par])
            fw.op("dve", lambda e, k=k: e.tensor_tensor(out=pw[:, k, 0, :], in0=P_(TMP), in1=P_(T2), op=ALU.subtract), reads=[pw, par], writes=[pw])
            fw.op("dve", lambda e, k=k: e.tensor_tensor(out=P_(TMP), in0=pw[:, k - 1, 0, :], in1=pw[:, k - 1, 1, :], op=ALU.mult), reads=[pw, par], writes=[par])
            fw.op("dve", lambda e, k=k: e.tensor_scalar(out=pw[:, k, 1, :], in0=P_(TMP), scalar1=2.0, scalar2=None, op0=ALU.mult), reads=[pw, par], writes=[pw])
        fw.op("dve", lambda e: e.tensor_scalar(out=pw[:, :, 2, :], in0=pw[:, :, 1, :], scalar1=-1.0, scalar2=None, op0=ALU.mult), reads=[pw], writes=[pw])
        braw = fw.sb("s5_braw", [128, 2, 32, 16]); bb = fw.sb("s5_bb", [128, 2, 32, 16]); btmp = fw.sb("s5_btmp", [128, 32, 16])
        for ri, nm in enumerate(("ssm_b_re", "ssm_b_im")):
            fw.dma("sp" if ri == 0 else "act", braw[:, ri, :, :], d[nm].t[l].rearrange("d (gp g2) p k -> (g2 p) (d gp) k", g2=2), reads=[d[nm]], writes=[braw])
        cre_b = P_(CRE).unsqueeze(2).to_broadcast([128, 32, 16]); cim_b = P_(CIM).unsqueeze(2).to_broadcast([128, 32, 16])
        fw.op("dve", lambda e: e.tensor_tensor(out=bb[:, 0, :, :], in0=braw[:, 0, :, :], in1=cre_b, op=ALU.mult), reads=[braw, par], writes=[bb])
        fw.op("dve", lambda e: e.tensor_tensor(out=btmp[:], in0=braw[:, 1, :, :], in1=cim_b, op=ALU.mult), reads=[braw, par], writes=[btmp])
        fw.op("dve", lambda e: e.tensor_tensor(out=bb[:, 0, :, :], in0=bb[:, 0, :, :], in1=btmp[:], op=ALU.subtract), reads=[bb, btmp], writes=[bb])
        fw.op("dve", lambda e: e.tensor_tensor(out=bb[:, 1, :, :], in0=braw[:, 1, :, :], in1=cre_b, op=ALU.mult), reads=[braw, par], writes=[bb])
        fw.op("dve", lambda e: e.tensor_tensor(out=btmp[:], in0=braw[:, 0, :, :], in1=cim_b, op=ALU.mult), reads=[braw, par, bb], writes=[btmp])
        fw.op("dve", lambda e: e.tensor_tensor(out=bb[:, 1, :, :], in0=bb[:, 1, :, :], in1=btmp[:], op=ALU.add), reads=[bb, btmp], writes=[bb])
        cpd = fw.sb("s5_cpd", [32, 2, 128])
        fw.op("pool", lambda e: e.memset(cpd[:], 0.0), writes=[cpd])
        A = [fw.sb("s5_A%d" % i, [128, T]) for i in range(2)]
        Bq = [fw.sb("s5_B%d" % i, [128, T]) for i in range(2)]
        yacc = fw.sb("s5_yacc", [128, T])
        uch = [fw.sb("s5_u%d" % i, [128, 512]) for i in range(2)]
        wB = fw.sb("s5_wB", [128, 2, 128]); lB = fw.sb("s5_lB", [128, 2, 128]); lC = fw.sb("s5_lC", [128, 2, 128])
        dcol = fw.sb("s5_dcol", [128, 4])
        drow = fw.sb("s5_drow", [4, 128])
        fw.dma("sp", drow[:], d["ssm_d"].t[l].rearrange("(c p) -> c p", p=128), reads=[d["ssm_d"]], writes=[drow])
        pbd = self.bank()
        fw.op("pe", lambda e: e.transpose(out=pbd[:, 0:4], in_=drow[:], identity=self.ident[0:4, 0:4]), reads=[drow, self.ident], writes=[pbd])
        fw.op("dve", lambda e: e.tensor_copy(out=dcol[:], in_=pbd[:, 0:4]), reads=[pbd], writes=[dcol])
        zst = [fw.sb("s5_z%d" % i, [128, 512]) for i in range(2)]
        chunks = [(i * 512, 512) for i in range(16)] + [(L, CT)]
        ui = 0
        for ct in range(4):
            first = True
            for dr in range(2):
                for g4 in range(4):
                    gp = ct * 4 + g4
                    gi = dr * 16 + gp
                    fw.op("pool", lambda e: e.memset(wB[:], 0.0), writes=[wB])
                    for ri in range(2):
                        for g2 in range(2):
                            c0 = 32 * g4 + 16 * g2
                            fw.op("dve", lambda e, ri=ri, g2=g2, c0=c0: e.tensor_copy(out=wB[64 * g2:64 * g2 + 64, ri, c0:c0 + 16], in_=bb[64 * g2:64 * g2 + 64, ri, gi, :]),
                                  reads=[bb], writes=[wB])
                    for ri in range(2):
                        pb = self.bank()
                        fw.op("pe", lambda e, ri=ri: e.transpose(out=pb[:, 0:128], in_=wB[:, ri, :], identity=self.ident[:]), reads=[wB, self.ident], writes=[pb])
                        fw.op("act", lambda e, ri=ri: e.activation(out=lB[:, ri, :], in_=pb[:, 0:128], func=AF.Copy), reads=[pb], writes=[lB])
                    fw.op("pool", lambda e: e.memset(lC[:], 0.0), writes=[lC])
                    for ri, nm in enumerate(("ssm_c_re", "ssm_c_im")):
                        for g2 in range(2):
                            fw.dma("sp" if g2 == 0 else "act", cpd[16 * g2:16 * g2 + 16, ri, 64 * g2:64 * g2 + 64], d[nm].t[l, dr, 2 * gp + g2, :, :], reads=[d[nm]], writes=[cpd])
                    for ri in range(2):
                        pb = self.bank()
                        fw.op("pe", lambda e, ri=ri: e.transpose(out=pb[:, 0:32], in_=cpd[:, ri, :], identity=self.ident[0:32, 0:32]), reads=[cpd, self.ident], writes=[pb])
                        fw.op("act", lambda e, ri=ri: e.activation(out=lC[:, ri, 32 * g4:32 * g4 + 32], in_=pb[:, 0:32], func=AF.Copy, scale=(1.0 if ri == 0 else -1.0)),
                              reads=[pb], writes=[lC])
                    for (c0, n) in chunks:
                        ub = uch[ui % 2]; ui += 1
                        fw.dma("sp" if ui % 2 else "act", ub[:, 0:n], s["projT"].t[ct * 128:(ct + 1) * 128, c0:c0 + n], reads=[s["projT"]], writes=[ub])
                        if dr == 0:
                            q0 = c0 + CT if c0 < L else 0
                        else:
                            q0 = c0
                        for ri in range(2):
                            pb = self.bank()
                            fw.op("pe", lambda e, ri=ri: e.matmul(pb[:, 0:n], lB[:, ri, :], ub[:, 0:n], start=True, stop=True), reads=[lB, ub], writes=[pb])
                            fw.op("act", lambda e, ri=ri: e.activation(out=A[ri][:, q0:q0 + n], in_=pb[:, 0:n], func=AF.Copy), reads=[pb], writes=[A[ri]])
                    src, dst = A, Bq
                    for k in range(NS):
                        sft = 1 << k
                        ar = pw[:, k, 0, gi:gi + 1]; ai = pw[:, k, 1, gi:gi + 1]; nai = pw[:, k, 2, gi:gi + 1]
                        if dr == 0:
                            o_sl = slice(sft, T); i_sl = slice(0, T - sft); h_sl = slice(0, sft)
                        else:
                            o_sl = slice(0, T - sft); i_sl = slice(sft, T); h_sl = slice(T - sft, T)
                        for ri in range(2):
                            fw.op("act", lambda e, ri=ri: e.activation(out=dst[ri][:, h_sl], in_=src[ri][:, h_sl], func=AF.Copy), reads=[src[ri]], writes=[dst[ri]])
                        fw.op("dve", lambda e: e.scalar_tensor_tensor(out=dst[0][:, o_sl], in0=src[0][:, i_sl], scalar=ar, in1=src[0][:, o_sl], op0=ALU.mult, op1=ALU.add),
                              reads=[src[0], pw], writes=[dst[0]])
                        fw.op("dve", lambda e: e.scalar_tensor_tensor(out=dst[0][:, o_sl], in0=src[1][:, i_sl], scalar=nai, in1=dst[0][:, o_sl], op0=ALU.mult, op1=ALU.add),
                              reads=[src[1], pw, dst[0]], writes=[dst[0]])
                        fw.op("dve", lambda e: e.scalar_tensor_tensor(out=dst[1][:, o_sl], in0=src[0][:, i_sl], scalar=ai, in1=src[1][:, o_sl], op0=ALU.mult, op1=ALU.add),
                              reads=[src[0], src[1], pw], writes=[dst[1]])
                        fw.op("dve", lambda e: e.scalar_tensor_tensor(out=dst[1][:, o_sl], in0=src[1][:, i_sl], scalar=ar, in1=dst[1][:, o_sl], op0=ALU.mult, op1=ALU.add),
                              reads=[src[1], pw, dst[1]], writes=[dst[1]])
                        src, dst = dst, src
                    hfin = src
                    for (c0, n) in chunks:
                        if dr == 0:
                            q0 = c0 + CT if c0 < L else 0
                        else:
                            q0 = c0
                        pb = self.bank()
                        fw.op("pe", lambda e: e.matmul(pb[:, 0:n], lC[:, 0, :], hfin[0][:, q0:q0 + n], start=True, stop=False), reads=[lC, hfin[0]], writes=[pb])
                        fw.op("pe", lambda e: e.matmul(pb[:, 0:n], lC[:, 1, :], hfin[1][:, q0:q0 + n], start=False, stop=True), reads=[lC, hfin[1]], writes=[pb])
                        if first:
                            fw.op("act", lambda e: e.activation(out=yacc[:, c0:c0 + n], in_=pb[:, 0:n], func=AF.Copy), reads=[pb], writes=[yacc])
                        else:
                            fw.op("pool" if False else "dve", lambda e: e.tensor_tensor(out=yacc[:, c0:c0 + n], in0=pb[:, 0:n], in1=yacc[:, c0:c0 + n], op=ALU.add), reads=[pb, yacc], writes=[yacc])
                    first = False
            for ci, (c0, n) in enumerate(chunks):
                ub = uch[ui % 2]; ui += 1
                fw.dma("sp", ub[:, 0:n], s["projT"].t[ct * 128:(ct + 1) * 128, c0:c0 + n], reads=[s["projT"]], writes=[ub])
                zb = zst[ci % 2]
                fw.op("dve", lambda e: e.scalar_tensor_tensor(out=zb[:, 0:n], in0=ub[:, 0:n], scalar=dcol[:, ct:ct + 1], in1=yacc[:, c0:c0 + n], op0=ALU.mult, op1=ALU.add),
                      reads=[ub, dcol, yacc], writes=[zb])
                fw.op("act", lambda e: e.activation(out=zb[:, 0:n], in_=zb[:, 0:n], func=AF.Gelu_apprx_tanh), reads=[zb], writes=[zb])
                fw.dma("act", s["s5z"].t[ct * 128:(ct + 1) * 128, c0:c0 + n], zb[:, 0:n], reads=[zb], writes=[s["s5z"]])

    def p2a_glu(self, l, es):
        fw, d, s = self.fw, self.d, self.s
        wg = fw.sb("glu_w", [128, 4, 512])
        fw.dma("sp", wg[:], d["ssm_w_glu"].t[l].rearrange("(k p) n -> p k n", p=128), reads=[d["ssm_w_glu"]], writes=[wg])
        zz = [fw.sb("glu_z%d" % i, [128, 4, 512]) for i in range(2)]
        og = [fw.sb("glu_o%d" % i, [128, 512]) for i in range(2)]
        chunks = [(i * 512, 512) for i in range(16)] + [(L, CT)]
        oi = 0
        for ci, (c0, n) in enumerate(chunks):
            zb = zz[ci % 2]
            fw.dma("sp", zb[:, :, 0:n], s["s5z"].t[:, c0:c0 + n].rearrange("(k p) t -> p k t", p=128), reads=[s["s5z"]], writes=[zb])
            for m in range(4):
                pb = self.bank()
                for k in range(4):
                    fw.op("pe", lambda e, k=k: e.matmul(pb[:, 0:n], wg[:, k, m * 128:(m + 1) * 128], zb[:, k, 0:n], start=(k == 0), stop=(k == 3)), reads=[wg, zb], writes=[pb])
                ob = og[oi % 2]; oi += 1
                fw.op("act", lambda e: e.activation(out=ob[:, 0:n], in_=pb[:, 0:n], func=AF.Sigmoid), reads=[pb], writes=[ob])
                fw.op("dve", lambda e: e.tensor_tensor(out=ob[:, 0:n], in0=ob[:, 0:n], in1=zb[:, m, 0:n], op=ALU.mult), reads=[ob, zb], writes=[ob])
                fw.dma("act", s["brT"].t[m * 128:(m + 1) * 128, c0:c0 + n], ob[:, 0:n], reads=[ob], writes=[s["brT"]])

    def p2b_na_norm(self, l, es):
        fw, d, s = self.fw, self.d, self.s
        bones = fw.sb("na_bones", [128, 128])
        fw.op("pool", lambda e: e.memset(bones[:], 0.0), writes=[bones])
        fw.op("pool", lambda e: e.memset(bones[0:64, 0:64], 1.0), writes=[bones])
        fw.op("pool", lambda e: e.memset(bones[64:128, 64:128], 1.0), writes=[bones])
        grow = fw.sb("na_grow", [2, 128]); gcol = fw.sb("na_gcol", [128, 2])
        for qi, nm in enumerate(("na_q_gain", "na_k_gain")):
            for rep in range(2):
                fw.dma("sp", grow[qi:qi + 1, rep * 64:(rep + 1) * 64], d[nm].t[l:l + 1, :], reads=[d[nm]], writes=[grow])
        pb = self.bank()
        fw.op("pe", lambda e: e.transpose(out=pb[:, 0:2], in_=grow[:], identity=self.ident[0:2, 0:2]), reads=[grow, self.ident], writes=[pb])
        fw.op("dve", lambda e: e.tensor_copy(out=gcol[:], in_=pb[:, 0:2]), reads=[pb], writes=[gcol])
        fw.op("dve", lambda e: e.tensor_scalar(out=gcol[:, 0:1], in0=gcol[:, 0:1], scalar1=0.125, scalar2=None, op0=ALU.mult), reads=[gcol], writes=[gcol])
        xq = [fw.sb("na_x%d" % i, [128, 512]) for i in range(2)]
        sq = [fw.sb("na_sq%d" % i, [128, 512]) for i in range(2)]
        rs = [fw.sb("na_rs%d" % i, [128, 512]) for i in range(2)]
        chunks = [(i * 512, 512) for i in range(16)] + [(L, CT)]
        it = 0
        for qi, dst in enumerate(("qnT", "knT")):
            for ct in range(4):
                row0 = W + qi * W + ct * 128
                for (c0, n) in chunks:
                    xb = xq[it % 2]; sb_ = sq[it % 2]; rb = rs[it % 2]; it += 1
                    fw.dma("sp", xb[:, 0:n], s["projT"].t[row0:row0 + 128, c0:c0 + n], reads=[s["projT"]], writes=[xb])
                    fw.op("act", lambda e: e.activation(out=sb_[:, 0:n], in_=xb[:, 0:n], func=AF.Square), reads=[xb], writes=[sb_])
                    pb = self.bank()
                    fw.op("pe", lambda e: e.matmul(pb[:, 0:n], bones[:], sb_[:, 0:n], start=True, stop=True), reads=[bones, sb_], writes=[pb])
                    fw.op("act", lambda e: e.activation(out=rb[:, 0:n], in_=pb[:, 0:n], func=AF.Sqrt, scale=1.0 / 64, bias=EPS), reads=[pb], writes=[rb])
                    fw.op("dve", lambda e: e.reciprocal(out=rb[:, 0:n], in_=rb[:, 0:n]), reads=[rb], writes=[rb])
                    fw.op("dve", lambda e: e.scalar_tensor_tensor(out=rb[:, 0:n], in0=xb[:, 0:n], scalar=gcol[:, qi:qi + 1], in1=rb[:, 0:n], op0=ALU.mult, op1=ALU.mult),
                          reads=[xb, gcol, rb], writes=[rb])
                    fw.dma("act", s[dst].t[ct * 128:(ct + 1) * 128, c0:c0 + n], rb[:, 0:n], reads=[rb], writes=[s[dst]])

    def p2b_na(self, l, es):
        fw, d, s = self.fw, self.d, self.s
        ones = fw.sb("na_ones", [128, 64])
        fw.op("pool", lambda e: e.memset(ones[:], 1.0), writes=[ones])
        tab3 = fw.sb("na_tab3", [128, 8, 4, 64]); tabe = fw.sb("na_tabe", [128, 8, 4, 64])
        fw.dma("sp", tab3[:], d["na_tab"].t[l, 3], reads=[d["na_tab"]], writes=[tab3])
        Kc = fw.sb("na_Kc", [128, 4, CT]); Vc = fw.sb("na_Vc", [128, 2, W])
        fw.dma("sp", Kc[:], s["knT"].t[:, L:AT].rearrange("(hp p) t -> p hp t", p=128), reads=[s["knT"]], writes=[Kc])
        fw.dma("act", Vc[:], s["vtok"].t[L:AT, :].rearrange("(j p) c -> p j c", p=128), reads=[s["vtok"]], writes=[Vc])
        Kw = [fw.sb("na_Kw%d" % i, [128, 4, 512]) for i in range(2)]
        Vw = [fw.sb("na_Vw%d" % i, [128, 4, W]) for i in range(2)]
        Qr = [fw.sb("na_Qr%d" % i, [128, 4, 64]) for i in range(2)]
        Pm = [fw.sb("na_P%d" % i, [128, 6, 64]) for i in range(2)]
        orow = [fw.sb("na_o%d" % i, [64, 8, 64]) for i in range(2)]
        rden = [fw.sb("na_rd%d" % i, [64, 64]) for i in range(2)]
        hi = 0
        for r in range(128):
            start = min(max(r - 4, 0), 120)
            dr0 = start - r + 7
            kb = Kw[r % 2]; vb = Vw[r % 2]; qb = Qr[r % 2]; ob = orow[r % 2]
            fw.dma("sp", kb[:], s["knT"].t[:, 64 * start:64 * start + 512].rearrange("(hp p) t -> p hp t", p=128), reads=[s["knT"]], writes=[kb])
            fw.dma("act", vb[:], s["vtok"].t[64 * start:64 * start + 512, :].rearrange("(j p) c -> p j c", p=128), reads=[s["vtok"]], writes=[vb])
            fw.dma("sp", qb[:], s["qnT"].t[:, 64 * r:64 * r + 64].rearrange("(hp p) t -> p hp t", p=128), reads=[s["qnT"]], writes=[qb])
            if dr0 == 3:
                tab = tab3
            else:
                tab = tabe
                fw.dma("act", tabe[:], d["na_tab"].t[l, dr0], reads=[d["na_tab"]], writes=[tabe])
            for h in range(8):
                hp, b = h // 2, 64 * (h % 2)
                pm = Pm[hi % 2]; rd = rden[hi % 2]; hi += 1
                ps = self.bank()
                for j in range(4):
                    fw.op("pe", lambda e, j=j: e.matmul(ps[:, j * 64:(j + 1) * 64], kb[b:b + 64, hp, j * 128:(j + 1) * 128], qb[b:b + 64, hp, :], start=True, stop=True),
                          reads=[kb, qb], writes=[ps])
                for j in range(2):
                    fw.op("pe", lambda e, j=j: e.matmul(ps[:, 256 + j * 64:256 + (j + 1) * 64], Kc[b:b + 64, hp, j * 128:(j + 1) * 128], qb[b:b + 64, hp, :], start=True, stop=True),
                          reads=[Kc, qb], writes=[ps])
                fw.op("dve", lambda e: e.tensor_tensor(out=pm[:, 0:4, :], in0=ps[:, 0:256].rearrange("p (j q) -> p j q", j=4), in1=tab[:, h, :, :], op=ALU.add),
                      reads=[ps, tab], writes=[pm])
                fw.op("act", lambda e: e.activation(out=pm[:, 0:4, :], in_=pm[:, 0:4, :], func=AF.Exp), reads=[pm], writes=[pm])
                fw.op("act", lambda e: e.activation(out=pm[:, 4:6, :], in_=ps[:, 256:384].rearrange("p (j q) -> p j q", j=2), func=AF.Exp), reads=[ps], writes=[pm])
                po = self.bank(); pd = self.bank()
                for j in range(6):
                    vsrc = vb[:, j, 64 * h:64 * h + 64] if j < 4 else Vc[:, j - 4, 64 * h:64 * h + 64]
                    fw.op("pe", lambda e, j=j, vsrc=vsrc: e.matmul(po[0:64, 0:64], vsrc, pm[:, j, :], start=(j == 0), stop=(j == 5)), reads=[vb, Vc, pm], writes=[po])
                for j in range(6):
                    fw.op("pe", lambda e, j=j: e.matmul(pd[0:64, 0:64], ones[:], pm[:, j, :], start=(j == 0), stop=(j == 5)), reads=[ones, pm], writes=[pd])
                fw.op("dve", lambda e: e.reciprocal(out=rd[:], in_=pd[0:64, 0:64]), reads=[pd], writes=[rd])
                fw.op("dve", lambda e: e.tensor_tensor(out=ob[:, h, :], in0=po[0:64, 0:64], in1=rd[:], op=ALU.mult), reads=[po, rd], writes=[ob])
            fw.dma("sp", s["brT"].t[W:2 * W, 64 * r:64 * r + 64].rearrange("(h d) q -> d h q", d=64), ob[:], reads=[ob], writes=[s["brT"]])
        if l == 0:
            Qc = fw.sb("na_Qc", [128, 4, CT]); Pc = fw.sb("na_Pc", [128, 2, CT]); oc = fw.sb("na_oc", [64, 8, CT]); rdc = fw.sb("na_rdc", [64, CT])
            fw.dma("sp", Qc[:], s["qnT"].t[:, L:AT].rearrange("(hp p) t -> p hp t", p=128), reads=[s["qnT"]], writes=[Qc])
            for h in range(8):
                hp, b = h // 2, 64 * (h % 2)
                ps = self.bank()
                for j in range(2):
                    fw.op("pe", lambda e, j=j: e.matmul(ps[:, j * CT:(j + 1) * CT], Kc[b:b + 64, hp, j * 128:(j + 1) * 128], Qc[b:b + 64, hp, :], start=True, stop=True),
                          reads=[Kc, Qc], writes=[ps])
                fw.op("act", lambda e: e.activation(out=Pc[:], in_=ps[:].rearrange("p (j q) -> p j q", j=2), func=AF.Exp), reads=[ps], writes=[Pc])
                po = self.bank(); pd = self.bank()
                for j in range(2):
                    fw.op("pe", lambda e, j=j: e.matmul(po[0:64, 0:CT], Vc[:, j, 64 * h:64 * h + 64], Pc[:, j, :], start=(j == 0), stop=(j == 1)), reads=[Vc, Pc], writes=[po])
                for j in range(2):
                    fw.op("pe", lambda e, j=j: e.matmul(pd[0:64, 0:CT], ones[:], Pc[:, j, :], start=(j == 0), stop=(j == 1)), reads=[ones, Pc], writes=[pd])
                fw.op("dve", lambda e: e.reciprocal(out=rdc[:], in_=pd[0:64, 0:CT]), reads=[pd], writes=[rdc])
                fw.op("dve", lambda e: e.tensor_tensor(out=oc[:, h, :], in0=po[0:64, 0:CT], in1=rdc[:], op=ALU.mult), reads=[po, rdc], writes=[oc])
            fw.dma("sp", s["brT"].t[W:2 * W, L:AT].rearrange("(h d) q -> d h q", d=64), oc[:], reads=[oc], writes=[s["brT"]])

    def p2c_hy_short(self, l, es):
        fw, d, s = self.fw, self.d, self.s
        wrow = fw.sb("hs_wrow", [4, 1536]); wcol = fw.sb("hs_wcol", [128, 12, 4])
        fw.dma("sp", wrow[0:3, :], d["hy_conv_w"].t[l], reads=[d["hy_conv_w"]], writes=[wrow])
        fw.dma("sp", wrow[3:4, :], d["hy_conv_b"].t[l:l + 1, :], reads=[d["hy_conv_b"]], writes=[wrow])
        for ct in range(12):
            pb = self.bank()
            fw.op("pe", lambda e: e.transpose(out=pb[:, 0:4], in_=wrow[:, ct * 128:(ct + 1) * 128], identity=self.ident[0:4, 0:4]), reads=[wrow, self.ident], writes=[pb])
            fw.op("dve", lambda e: e.tensor_copy(out=wcol[:, ct, :], in_=pb[:, 0:4]), reads=[pb], writes=[wcol])
        ub = [fw.sb("hs_u%d" % i, [128, L]) for i in range(2)]
        zb = [fw.sb("hs_z%d" % i, [128, L]) for i in range(2)]
        it = 0
        for ct in range(12):
            for (c0, n) in ((0, L), (L, CT)):
                u = ub[it % 2]; z = zb[it % 2]; it += 1
                fw.dma("sp", u[:, 0:n], s["projT"].t[2048 + ct * 128:2048 + (ct + 1) * 128, c0:c0 + n], reads=[s["projT"]], writes=[u])
                fw.op("dve", lambda e: e.tensor_scalar(out=z[:, 0:n], in0=u[:, 0:n], scalar1=wcol[:, ct, 1:2], scalar2=wcol[:, ct, 3:4], op0=ALU.mult, op1=ALU.add),
                      reads=[u, wcol], writes=[z])
                fw.op("dve", lambda e: e.scalar_tensor_tensor(out=z[:, 1:n], in0=u[:, 0:n - 1], scalar=wcol[:, ct, 0:1], in1=z[:, 1:n], op0=ALU.mult, op1=ALU.add),
                      reads=[u, wcol, z], writes=[z])
                fw.op("dve", lambda e: e.scalar_tensor_tensor(out=z[:, 0:n - 1], in0=u[:, 1:n], scalar=wcol[:, ct, 2:3], in1=z[:, 0:n - 1], op0=ALU.mult, op1=ALU.add),
                      reads=[u, wcol, z], writes=[z])
                fw.dma("act", s["hzT"].t[ct * 128:(ct + 1) * 128, c0:c0 + n], z[:, 0:n], reads=[z], writes=[s["hzT"]])

    def p2c_hy_filt(self, l, es, seg):
        fw, d, s = self.fw, self.d, self.s
        Lx, zname, dname = (L, "hyc_zL", "hyc_dL") if seg == 0 else (CT, "hyc_zc", "hyc_dc")
        CH = min(512, Lx); nch = Lx // CH
        hfT = s["hfT"] if seg == 0 else s["hfTc"]
        w1 = fw.sb("hf_w1", [33, 64]); w2 = fw.sb("hf_w2", [64, 64]); w3 = fw.sb("hf_w3", [64, 64]); w4 = fw.sb("hf_w4", [64, 2048])
        fw.dma("sp", w1[:], d["hy_w1"].t[l], reads=[d["hy_w1"]], writes=[w1]); fw.dma("sp", w2[:], d["hy_w2"].t[l], reads=[d["hy_w2"]], writes=[w2])
        fw.dma("sp", w3[:], d["hy_w3"].t[l], reads=[d["hy_w3"]], writes=[w3]); fw.dma("act", w4[:], d["hy_w4"].t[l], reads=[d["hy_w4"]], writes=[w4])
        brow = fw.sb("hf_brow", [4, 64]); bcol = fw.sb("hf_bcol", [64, 4])
        for i, nm in enumerate(("hy_b1", "hy_b2", "hy_b3", "hy_freq")):
            fw.dma("sp", brow[i:i + 1, :], d[nm].t[l:l + 1, :], reads=[d[nm]], writes=[brow])
        pb = self.bank()
        fw.op("pe", lambda e: e.transpose(out=pb[0:64, 0:4], in_=brow[:], identity=self.ident[0:4, 0:4]), reads=[brow, self.ident], writes=[pb])
        fw.op("dve", lambda e: e.tensor_copy(out=bcol[:], in_=pb[0:64, 0:4]), reads=[pb], writes=[bcol])
        zp = fw.sb("hf_zp", [33, Lx])
        fw.dma("sp", zp[:], d[zname].t, reads=[d[zname]], writes=[zp])
        h3 = fw.sb("hf_h3", [64, Lx])
        ha = fw.sb("hf_ha", [64, CH]); hb_ = fw.sb("hf_hb", [64, CH]); ti = fw.sb("hf_ti", [64, CH], I32); tf = fw.sb("hf_tf", [64, CH])
        for ch in range(nch):
            c0 = ch * CH
            cur_in = zp[:, c0:c0 + CH]; cur_buf = zp
            for li, (wt, kdim) in enumerate(((w1, 33), (w2, 64), (w3, 64))):
                pb = self.bank()
                fw.op("pe", lambda e: e.matmul(pb[0:64, 0:CH], wt[0:kdim, :], cur_in, start=True, stop=True), reads=[wt, cur_buf], writes=[pb])
                fw.op("dve", lambda e: e.tensor_scalar(out=ha[:], in0=pb[0:64, 0:CH], scalar1=bcol[:, li:li + 1], scalar2=bcol[:, 3:4], op0=ALU.add, op1=ALU.mult),
                      reads=[pb, bcol], writes=[ha])
                outap = (hb_[:] if li < 2 else h3[:, c0:c0 + CH]); outbuf = hb_ if li < 2 else h3
                self.sin_reduced(outap, ha[:], ti[:], tf[:], [ha], [outbuf, ti, tf])
                cur_in = hb_[:]; cur_buf = hb_
        dec = [fw.sb("hf_dec%d" % i, [128, CH]) for i in range(2)]
        hf = [fw.sb("hf_hf%d" % i, [128, CH]) for i in range(2)]
        junk = fw.sb("hf_junk", [128, CH])
        acc = fw.sb("hf_acc", [128, 2 * nch + 2]); nr = fw.sb("hf_nr", [128, 2])
        it = 0
        for o in range(2):
            for cti in range(4):
                for dr in range(2):
                    m0 = o * 1024 + dr * 512 + cti * 128
                    for ch in range(nch):
                        c0 = ch * CH
                        db = dec[it % 2]; hb2 = hf[it % 2]; it += 1
                        fw.dma("sp", db[:], d[dname].t[cti * 128:(cti + 1) * 128, c0:c0 + CH], reads=[d[dname]], writes=[db])
                        pb = self.bank()
                        fw.op("pe", lambda e: e.matmul(pb[:, 0:CH], w4[:, m0:m0 + 128], h3[:, c0:c0 + CH], start=True, stop=True), reads=[w4, h3], writes=[pb])
                        fw.op("dve", lambda e: e.tensor_tensor(out=hb2[:], in0=pb[:, 0:CH], in1=db[:], op=ALU.mult), reads=[pb, db], writes=[hb2])
                        if dr == 1 and ch == 0:
                            fw.op("dve", lambda e: e.memset(hb2[:, 0:1], 0.0), reads=[hb2], writes=[hb2])
                        fw.op("act", lambda e: e.activation(out=junk[:], in_=hb2[:], func=AF.Abs, accum_out=acc[:, dr * nch + ch:dr * nch + ch + 1]), reads=[hb2], writes=[junk, acc])
                        fw.dma("act", hfT.t[m0:m0 + 128, c0:c0 + CH], hb2[:], reads=[hb2], writes=[hfT])
                fw.op("dve", lambda e: e.tensor_reduce(out=nr[:, 0:1], in_=acc[:, 0:2 * nch], axis=AX.X, op=ALU.add), reads=[acc], writes=[nr])
                fw.op("dve", lambda e: e.reciprocal(out=nr[:, 1:2], in_=nr[:, 0:1]), reads=[nr], writes=[nr])
                fw.dma("sp", s["hnrm"].t[seg, o, cti * 128:(cti + 1) * 128].rearrange("(p a) -> p a", a=1), nr[:, 1:2], reads=[nr], writes=[s["hnrm"]])

    def hy_setup(self):
        fw, d = self.fw, self.d
        F3 = fw.sb("hy_F3", [128, 3, 128]); T2 = fw.sb("hy_T2", [128, 2, 128])
        fw.dma("sp", F3[:], d["hyc_F"].t, reads=[d["hyc_F"]], writes=[F3]); fw.dma("act", T2[:], d["hyc_T"].t, reads=[d["hyc_T"]], writes=[T2])
        FF = fw.sb("hy_FF", [128, 256]); FiFr = fw.sb("hy_FiFr", [128, 256]); FrnFi = fw.sb("hy_FrnFi", [128, 256])
        cp = lambda dst, src: fw.op("dve", lambda e: e.tensor_copy(out=dst, in_=src), reads=[F3], writes=[FF, FiFr, FrnFi])
        cp(FF[:, 0:128], F3[:, 0, :]); cp(FF[:, 128:256], F3[:, 1, :])
        cp(FiFr[:, 0:128], F3[:, 1, :]); cp(FiFr[:, 128:256], F3[:, 0, :])
        cp(FrnFi[:, 0:128], F3[:, 0, :]); cp(FrnFi[:, 128:256], F3[:, 2, :])
        self.hy = dict(F3=F3, T2=T2, FF=FF, FiFr=FiFr, FrnFi=FrnFi)
        self.hy_tmp = [fw.sb("hy_tmp%d" % i, [128, 512]) for i in range(2)]
        self.hy_B = [fw.sb("hy_B%d" % i, [128, 2, 512]) for i in range(2)]
        self.hy_bi = 0

    def hy_cmul(self, out, a_re, a_im, a_bufs, b_re, b_im, b_bufs, conj=False):
        fw = self.fw
        tmp = self.hy_tmp[0]
        o_re, o_im = out[:, 0, :], out[:, 1, :]
        fw.op("dve", lambda e: e.tensor_tensor(out=o_re, in0=a_re, in1=b_re, op=ALU.mult), reads=a_bufs + b_bufs, writes=[out])
        fw.op("dve", lambda e: e.tensor_tensor(out=tmp[:], in0=a_im, in1=b_im, op=ALU.mult), reads=a_bufs + b_bufs, writes=[tmp])
        fw.op("dve", lambda e: e.tensor_tensor(out=o_re, in0=o_re, in1=tmp[:], op=(ALU.add if conj else ALU.subtract)), reads=[out, tmp], writes=[out])
        fw.op("dve", lambda e: e.tensor_tensor(out=o_im, in0=a_re, in1=b_im, op=ALU.mult), reads=a_bufs + b_bufs, writes=[out])
        fw.op("dve", lambda e: e.tensor_tensor(out=tmp[:], in0=a_im, in1=b_re, op=ALU.mult), reads=a_bufs + b_bufs + [tmp], writes=[tmp])
        if conj:
            fw.op("dve", lambda e: e.tensor_tensor(out=o_im, in0=tmp[:], in1=o_im, op=ALU.subtract), reads=[out, tmp], writes=[out])
        else:
            fw.op("dve", lambda e: e.tensor_tensor(out=o_im, in0=o_im, in1=tmp[:], op=ALU.add), reads=[out, tmp], writes=[out])

    def hy_fft(self, X, nrow):
        fw, H = self.fw, self.hy
        p1 = [self.bank(), self.bank()]
        for c in range(4):
            fw.op("pe", lambda e, c=c: e.matmul(p1[c // 2][:, (c % 2) * 256:(c % 2) * 256 + 256], X[0:nrow, c, :], H["FF"][0:nrow, :], start=True, stop=True),
                  reads=[X, H["FF"]], writes=[p1[c // 2]])
        Bt = self.hy_B[self.hy_bi % 2]; self.hy_bi += 1
        twr = H["T2"][:, 0, :].unsqueeze(1).to_broadcast([128, 2, 128]); twi = H["T2"][:, 1, :].unsqueeze(1).to_broadcast([128, 2, 128])
        for half in range(2):
            pv = p1[half][:].rearrange("p (c r k) -> p c r k", c=2, r=2)
            ov = Bt[:, :, half * 256:(half + 1) * 256].rearrange("p r (c k) -> p r c k", c=2)
            tmp = self.hy_tmp[0]
            tv = tmp[:, 0:256].rearrange("p (c k) -> p c k", c=2)
            a_re, a_im = pv[:, :, 0, :], pv[:, :, 1, :]
            fw.op("dve", lambda e: e.tensor_tensor(out=ov[:, 0], in0=a_re, in1=twr, op=ALU.mult), reads=[p1[half], H["T2"]], writes=[Bt])
            fw.op("dve", lambda e: e.tensor_tensor(out=tv, in0=a_im, in1=twi, op=ALU.mult), reads=[p1[half], H["T2"]], writes=[tmp])
            fw.op("dve", lambda e: e.tensor_tensor(out=ov[:, 0], in0=ov[:, 0], in1=tv, op=ALU.subtract), reads=[Bt, tmp], writes=[Bt])
            fw.op("dve", lambda e: e.tensor_tensor(out=ov[:, 1], in0=a_re, in1=twi, op=ALU.mult), reads=[p1[half], H["T2"]], writes=[Bt])
            fw.op("dve", lambda e: e.tensor_tensor(out=tv, in0=a_im, in1=twr, op=ALU.mult), reads=[p1[half], H["T2"], tmp], writes=[tmp])
            fw.op("dve", lambda e: e.tensor_tensor(out=ov[:, 1], in0=ov[:, 1], in1=tv, op=ALU.add), reads=[Bt, tmp], writes=[Bt])
        pr, pi = self.bank(), self.bank()
        Fr, Fi, nFi = H["F3"][:, 0, :], H["F3"][:, 1, :], H["F3"][:, 2, :]
        fw.op("pe", lambda e: e.matmul(pr[:], Fr, Bt[:, 0, :], start=True, stop=False), reads=[H["F3"], Bt], writes=[pr])
        fw.op("pe", lambda e: e.matmul(pr[:], nFi, Bt[:, 1, :], start=False, stop=True), reads=[H["F3"], Bt], writes=[pr])
        fw.op("pe", lambda e: e.matmul(pi[:], Fr, Bt[:, 1, :], start=True, stop=False), reads=[H["F3"], Bt], writes=[pi])
        fw.op("pe", lambda e: e.matmul(pi[:], Fi, Bt[:, 0, :], start=False, stop=True), reads=[H["F3"], Bt], writes=[pi])
        return pr, pi

    def hy_ifft(self, Yh, nrow):
        fw, H = self.fw, self.hy
        p1 = [self.bank(), self.bank()]
        for c in range(4):
            dst = p1[c // 2][:, (c % 2) * 256:(c % 2) * 256 + 256]
            fw.op("pe", lambda e, c=c: e.matmul(dst, Yh[:, 0, c * 128:(c + 1) * 128], H["FrnFi"][:], start=True, stop=False), reads=[Yh, H["FrnFi"]], writes=[p1[c // 2]])
            fw.op("pe", lambda e, c=c: e.matmul(dst, Yh[:, 1, c * 128:(c + 1) * 128], H["FiFr"][:], start=False, stop=True), reads=[Yh, H["FiFr"]], writes=[p1[c // 2]])
        Et = self.hy_B[self.hy_bi % 2]; self.hy_bi += 1
        twr = H["T2"][:, 0, :].unsqueeze(1).to_broadcast([128, 2, 128]); twi = H["T2"][:, 1, :].unsqueeze(1).to_broadcast([128, 2, 128])
        for half in range(2):
            pv = p1[half][:].rearrange("p (c r k) -> p c r k", c=2, r=2)
            ov = Et[:, :, half * 256:(half + 1) * 256].rearrange("p r (c k) -> p r c k", c=2)
            tmp = self.hy_tmp[0]
            tv = tmp[:, 0:256].rearrange("p (c k) -> p c k", c=2)
            a_re, a_im = pv[:, :, 0, :], pv[:, :, 1, :]
            fw.op("dve", lambda e: e.tensor_tensor(out=ov[:, 0], in0=a_re, in1=twr, op=ALU.mult), reads=[p1[half], H["T2"]], writes=[Et])
            fw.op("dve", lambda e: e.tensor_tensor(out=tv, in0=a_im, in1=twi, op=ALU.mult), reads=[p1[half], H["T2"]], writes=[tmp])
            fw.op("dve", lambda e: e.tensor_tensor(out=ov[:, 0], in0=ov[:, 0], in1=tv, op=ALU.add), reads=[Et, tmp], writes=[Et])
            fw.op("dve", lambda e: e.tensor_tensor(out=ov[:, 1], in0=a_im, in1=twr, op=ALU.mult), reads=[p1[half], H["T2"]], writes=[Et])
            fw.op("dve", lambda e: e.tensor_tensor(out=tv, in0=a_re, in1=twi, op=ALU.mult), reads=[p1[half], H["T2"], tmp], writes=[tmp])
            fw.op("dve", lambda e: e.tensor_tensor(out=ov[:, 1], in0=ov[:, 1], in1=tv, op=ALU.subtract), reads=[Et, tmp], writes=[Et])
        po = self.bank()
        Fr, Fi = H["F3"][:, 0, 0:nrow], H["F3"][:, 1, 0:nrow]
        fw.op("pe", lambda e: e.matmul(po[0:nrow, :], Fr, Et[:, 0, :], start=True, stop=False), reads=[H["F3"], Et], writes=[po])
        fw.op("pe", lambda e: e.matmul(po[0:nrow, :], Fi, Et[:, 1, :], start=False, stop=True), reads=[H["F3"], Et], writes=[po])
        return po

    def p2c_hy_spec(self, l, es, seg):
        fw, d, s = self.fw, self.d, self.s
        self.hy_setup()
        Lx, nrow = (L, 64) if seg == 0 else (CT, 2)
        hfT = s["hfT"] if seg == 0 else s["hfTc"]
        spec = s["hspec"] if seg == 0 else s["hspecc"]
        rn = fw.sb("hp_rn", [128, 2, 512])
        for o in range(2):
            self.load_bcast("sp", rn, s["hnrm"], (seg * 2 + o) * 512, 512, ap=rn[:, o, :])
        Xf = [fw.sb("hp_Xf%d" % i, [64, 4, 128]) for i in range(2)]; Xb = [fw.sb("hp_Xb%d" % i, [64, 4, 128]) for i in range(2)]
        Sf = [fw.sb("hp_Sf%d" % i, [128, 2, 512]) for i in range(2)]; Hs = [fw.sb("hp_H%d" % i, [128, 2, 512]) for i in range(2)]
        it = 0
        for o in range(2):
            for g in range(128):
                c0 = g * 4
                xf = Xf[it % 2]; xb = Xb[it % 2]; sf = Sf[it % 2]; hs = Hs[it % 2]; it += 1
                r0 = o * 1024 + c0
                fw.dma("sp", xf[0:nrow], hfT.t[r0:r0 + 4, :].rearrange("c (a b) -> a c b", b=128), reads=[hfT], writes=[xf])
                fw.dma("act", xb[0:nrow], hfT.t[r0 + 512:r0 + 516, :].rearrange("c (a b) -> a c b", b=128), reads=[hfT], writes=[xb])
                pr, pi = self.hy_fft(xf, nrow)
                fw.op("act", lambda e: e.activation(out=sf[:, 0, :], in_=pr[:], func=AF.Copy), reads=[pr], writes=[sf])
                fw.op("act", lambda e: e.activation(out=sf[:, 1, :], in_=pi[:], func=AF.Copy), reads=[pi], writes=[sf])
                pr2, pi2 = self.hy_fft(xb, nrow)
                rnb = rn[:, o, c0:c0 + 4].unsqueeze(2).to_broadcast([128, 4, 128])
                v4 = lambda ap: ap.rearrange("p (c k) -> p c k", c=4)
                fw.op("dve", lambda e: e.tensor_tensor(out=hs[:, 0, :], in0=pr2[:], in1=sf[:, 0, :], op=ALU.add), reads=[pr2, sf], writes=[hs])
                fw.op("dve", lambda e: e.tensor_tensor(out=hs[:, 1, :], in0=sf[:, 1, :], in1=pi2[:], op=ALU.subtract), reads=[pi2, sf], writes=[hs])
                fw.op("dve", lambda e: e.tensor_tensor(out=v4(hs[:, 0, :]), in0=v4(hs[:, 0, :]), in1=rnb, op=ALU.mult), reads=[hs, rn], writes=[hs])
                fw.op("dve", lambda e: e.tensor_tensor(out=v4(hs[:, 1, :]), in0=v4(hs[:, 1, :]), in1=rnb, op=ALU.mult), reads=[hs, rn], writes=[hs])
                fw.dma("sp", spec.t[o, g], hs[:], reads=[hs], writes=[spec])

    def p2c_hy_conv(self, l, es, seg):
        fw, d, s = self.fw, self.d, self.s
        self.hy_setup()
        Lx, nrow, col0 = (L, 64, 0) if seg == 0 else (CT, 2, L)
        spec = s["hspec"] if seg == 0 else s["hspecc"]
        bias = fw.sb("hc_bias", [64, 2, 512])
        for o in range(2):
            self.load_bcast("sp", bias, d["hy_bias"], (l * 2 + o) * 512, 512, ap=bias[:, o, :])
        Y0 = [fw.sb("hc_Y0_%d" % i, [64, 4, 128]) for i in range(2)]; P0 = [fw.sb("hc_P0_%d" % i, [64, 4, 128]) for i in range(2)]
        P1 = [fw.sb("hc_P1_%d" % i, [64, 4, 128]) for i in range(2)]; Y1 = [fw.sb("hc_Y1_%d" % i, [64, 4, 128]) for i in range(2)]
        Hs = [fw.sb("hc_H%d" % i, [128, 2, 512]) for i in range(4)]; Yh = [fw.sb("hc_Yh%d" % i, [128, 2, 512]) for i in range(2)]
        invn = 1.0 / 16384.0
        f3 = lambda ap: ap.rearrange("p c k -> p (c k)")
        for g in range(128):
            c0 = g * 4
            y0 = Y0[g % 2]; p0 = P0[g % 2]; p1 = P1[g % 2]; y1 = Y1[g % 2]
            ld = lambda q, dst, row: fw.dma(q, dst[0:nrow], s["hzT"].t[row:row + 4, col0:col0 + Lx].rearrange("c (a b) -> a c b", b=128), reads=[s["hzT"]], writes=[dst])
            ld("sp", y0, 1024 + c0); ld("act", p0, c0); ld("sp", p1, 512 + c0)
            cur = y0
            for o in range(2):
                hs = Hs[(2 * g + o) % 4]; yh = Yh[o]
                fw.dma("act", hs[:], spec.t[o, g], reads=[spec], writes=[hs])
                pr, pi = self.hy_fft(cur, nrow)
                self.hy_cmul(yh, pr[:], pi[:], [pr, pi], hs[:, 0, :], hs[:, 1, :], [hs])
                po = self.hy_ifft(yh, nrow)
                part = p0 if o == 0 else p1
                dst = y1
                bb_ = bias[0:nrow, o, c0:c0 + 4].unsqueeze(2).to_broadcast([nrow, 4, 128])
                tmpb = self.hy_tmp[1]
                tv = tmpb[0:nrow, :].rearrange("p (c k) -> p c k", c=4)
                fw.op("dve", lambda e: e.tensor_tensor(out=tv, in0=cur[0:nrow], in1=bb_, op=ALU.mult), reads=[cur, bias], writes=[tmpb])
                fw.op("dve", lambda e: e.scalar_tensor_tensor(out=tmpb[0:nrow, :], in0=po[0:nrow, :], scalar=invn, in1=tmpb[0:nrow, :], op0=ALU.mult, op1=ALU.add),
                      reads=[po, tmpb], writes=[tmpb])
                fw.op("dve", lambda e: e.tensor_tensor(out=f3(dst[0:nrow]), in0=tmpb[0:nrow, :], in1=f3(part[0:nrow]), op=ALU.mult), reads=[tmpb, part], writes=[dst])
                cur = y1
            fw.dma("sp", s["brT"].t[2 * W + c0:2 * W + c0 + 4, col0:col0 + Lx].rearrange("c (a b) -> a c b", b=128), y1[0:nrow], reads=[y1], writes=[s["brT"]])

    def p3_merge(self, l, es, xin):
        fw, d, s = self.fw, self.d, self.s
        TB = 256
        hTb = fw.sb("m_hTb", [128, KT, TB])
        brb = [fw.sb("m_brb%d" % i, [128, 4, TB]) for i in range(3)]
        wg = [fw.sb("m_wg%d" % i, [128, KT, 512]) for i in range(2)]
        wbr = [fw.sb("m_wbr%d" % i, [128, 4, 512]) for i in range(2)]
        bg = [fw.sb("m_bg%d" % i, [128, 512]) for i in range(2)]
        mixed = [fw.sb("m_mix%d" % i, [128, D]) for i in range(2)]
        gate = [fw.sb("m_gate%d" % i, [128, 512]) for i in range(2)]
        mT = fw.sb("m_mT", [128, KT, 128])
        g1 = [fw.sb("m_g1_%d" % r, [128, D]) for r in range(2)]
        xt = fw.sb("m_xt", [128, D]); ot = fw.sb("m_ot", [128, D])
        for r in range(2):
            self.load_bcast("sp", g1[r], s["modD"], r * 6 * D + 2 * D, D, "small")
        wi = 0
        nblk = AT // TB
        if l == 0:
            self.dump("hTd0", s["hT"], s["hT"].t[:, 0, 0:128])
            self.dump("hTd15", s["hT"], s["hT"].t[:, 15, 0:128])
            self.dump("hTd0b", s["hT"], s["hT"].t[:, 0, 640:768])
        for blk in range(nblk):
            t0 = blk * TB
            r = 0 if t0 < L else 1
            if r == 1 and l == DEPTH - 1:
                continue
            fw.dma("sp", hTb[:], s["hT"].t[:, :, t0:t0 + TB], reads=[s["hT"]], writes=[hTb], key="m_ld")
            for i in range(3):
                fw.dma("act", brb[i][:], s["brT"].t[i * W:(i + 1) * W, t0:t0 + TB].rearrange("(k p) t -> p k t", p=128),
                       reads=[s["brT"]], writes=[brb[i]], key="m_ld")
            for i in range(3):
                for cc in range(4):
                    wgb = wg[wi % 2]; wbb = wbr[wi % 2]; bgb = bg[wi % 2]; wi += 1
                    c0 = i * D + cc * 512
                    fw.dma("sp", wgb[:], d["w_gate"].t[l, :, c0:c0 + 512].rearrange("(kt p) n -> p kt n", p=128),
                           reads=[d["w_gate"]], writes=[wgb], key="m_wg%d" % (wi % 2))
                    fw.dma("act", wbb[:], d["w_branch"].t[l, i, :, cc * 512:(cc + 1) * 512].rearrange("(k p) n -> p k n", p=128),
                           reads=[d["w_branch"]], writes=[wbb], key="m_wb%d" % (wi % 2))
                    self.load_bcast("pool", bgb, d["b_gate"], l * 3 * D + c0, 512, "m_bg%d" % (wi % 2))
                    for ti in range(TB // 128):
                        pg = self.bank()
                        for kt in range(KT):
                            fw.op("pe", lambda e, kt=kt: e.matmul(pg[:], hTb[:, kt, ti * 128:(ti + 1) * 128], wgb[:, kt, :], start=(kt == 0), stop=(kt == KT - 1)),
                                  reads=[hTb, wgb], writes=[pg])
                        pbp = self.bank()
                        for k4 in range(4):
                            fw.op("pe", lambda e, k4=k4: e.matmul(pbp[:], brb[i][:, k4, ti * 128:(ti + 1) * 128], wbb[:, k4, :], start=(k4 == 0), stop=(k4 == 3)),
                                  reads=[brb[i], wbb], writes=[pbp])
                        gt = gate[ti % 2]
                        fw.op("dve", lambda e: e.tensor_tensor(out=gt[:], in0=pg[:], in1=bgb[:], op=ALU.add), reads=[pg, bgb], writes=[gt])
                        fw.op("act", lambda e: e.activation(out=gt[:], in_=gt[:], func=AF.Sigmoid), reads=[gt], writes=[gt])
                        if blk == 0 and ti == 0 and i == 0 and cc == 0 and l == 0:
                            self.dump("gate00", gt, gt[:])
                            self.dump("hTb0", hTb, hTb[:, 0, 0:128])
                            self.dump("hTb15", hTb, hTb[:, 15, 0:128])
                            self.dump("wgb0", wgb, wgb[:, 0, :])
                            self.dump("bgb", bgb, bgb[:])
                        mx = mixed[ti]
                        if i == 0:
                            fw.op("dve", lambda e: e.tensor_tensor(out=mx[:, cc * 512:(cc + 1) * 512], in0=pbp[:], in1=gt[:], op=ALU.mult),
                                  reads=[pbp, gt], writes=[mx])
                        else:
                            fw.op("dve", lambda e: e.tensor_tensor(out=gt[:], in0=pbp[:], in1=gt[:], op=ALU.mult), reads=[pbp, gt], writes=[gt])
                            fw.op("pool", lambda e: e.tensor_tensor(out=mx[:, cc * 512:(cc + 1) * 512], in0=mx[:, cc * 512:(cc + 1) * 512], in1=gt[:], op=ALU.add),
                                  reads=[mx, gt], writes=[mx])
            for ti in range(TB // 128):
                mx = mixed[ti]
                if blk == 0 and ti == 0 and l == 0:
                    self.dump("mixed0", mx, mx[:])
                for q4 in range(4):
                    pb = self.bank()
                    for j in range(4):
                        kt = q4 * 4 + j
                        fw.op("pe", lambda e, kt=kt, j=j: e.transpose(out=pb[:, j * 128:(j + 1) * 128], in_=mx[:, kt * 128:(kt + 1) * 128], identity=self.ident[:]),
                              reads=[mx, self.ident], writes=[pb])
                    fw.op("act", lambda e: e.activation(out=mT[:, q4 * 4:(q4 + 1) * 4, :], in_=pb[:].rearrange("p (j t) -> p j t", j=4), func=AF.Copy),
                          reads=[pb], writes=[mT])
                tt0 = t0 + ti * 128
                if isinstance(xin, tuple):
                    srcb = xin[0] if tt0 < L else xin[1]
                    src = srcb.t[tt0:tt0 + 128, :] if tt0 < L else srcb.t[tt0 - L:tt0 - L + 128, :]
                else:
                    srcb = xin; src = xin.t[tt0:tt0 + 128, :]
                fw.dma("sp", xt[:], src, reads=[srcb], writes=[xt], key="m_x")
                for cc in range(4):
                    wgb = wg[wi % 2]; wi += 1
                    fw.dma("sp" if cc % 2 else "act", wgb[:], d["w_out"].t[l, :, cc * 512:(cc + 1) * 512].rearrange("(kt p) n -> p kt n", p=128),
                           reads=[d["w_out"]], writes=[wgb], key="m_wg%d" % (wi % 2))
                    po = self.bank()
                    for kt in range(KT):
                        fw.op("pe", lambda e, kt=kt: e.matmul(po[:], mT[:, kt, :], wgb[:, kt, :], start=(kt == 0), stop=(kt == KT - 1)),
                              reads=[mT, wgb], writes=[po])
                    fw.op("dve", lambda e: e.tensor_tensor(out=ot[:, cc * 512:(cc + 1) * 512], in0=po[:], in1=g1[r][:, cc * 512:(cc + 1) * 512], op=ALU.mult),
                          reads=[po, g1[r]], writes=[ot])
                fw.op("pool", lambda e: e.tensor_tensor(out=ot[:], in0=ot[:], in1=xt[:], op=ALU.add), reads=[ot, xt], writes=[ot])
                fw.dma("sp", s["xa"].t[tt0:tt0 + 128, :], ot[:], reads=[ot], writes=[s["xa"]], key="m_st")

    def p4a_router(self, l, es):
        fw, d, s = self.fw, self.d, self.s
        A = [fw.sb("r_A%d" % r, [128, D]) for r in range(2)]
        Bv = [fw.sb("r_B%d" % r, [128, D]) for r in range(2)]
        tmp = fw.sb("r_tmp", [128, D])
        self.load_bcast("sp", tmp, d["norm2"], l * D, D, "small")
        for r in range(2):
            self.load_bcast("act", A[r], s["modD"], r * 6 * D + 4 * D, D, "small")
            self.load_bcast("pool", Bv[r], s["modD"], r * 6 * D + 3 * D, D, "small")
            fw.op("dve", lambda e, r=r: e.scalar_tensor_tensor(out=A[r][:], in0=A[r][:], scalar=1.0, in1=tmp[:], op0=ALU.add, op1=ALU.mult),
                  reads=[A[r], tmp], writes=[A[r]])
        rt = fw.sb("r_rt", [128, KT, 16])
        fw.dma("sp", rt[:], d["router"].t[l].rearrange("(kt p) n -> p kt n", p=128), reads=[d["router"]], writes=[rt], key="small")
        xt = [fw.sb("r_xt%d" % i, [128, D]) for i in range(2)]
        hh = [fw.sb("r_hh%d" % i, [128, D]) for i in range(2)]
        hT = fw.sb("r_hT", [128, KT, 128])
        st = fw.sb("r_st", [128, 8]); lg = fw.sb("r_lg", [128, 16]); ex = fw.sb("r_ex", [128, 16])
        affT = self.affT
        ntile = NT if l == 0 else 64
        for tt in range(ntile):
            r = 0 if tt < 64 else 1
            xb = xt[tt % 2]; h = hh[tt % 2]
            fw.dma("sp", xb[:], s["xa"].t[tt * 128:(tt + 1) * 128, :], reads=[s["xa"]], writes=[xb], key="r_x%d" % (tt % 2))
            fw.op("act", lambda e: e.activation(out=h[:], in_=xb[:], func=AF.Square, accum_out=st[:, 0:1]), reads=[xb], writes=[h, st])
            fw.op("act", lambda e: e.activation(out=st[:, 1:2], in_=st[:, 0:1], func=AF.Sqrt, scale=1.0 / D, bias=EPS), reads=[st], writes=[st])
            fw.op("dve", lambda e: e.reciprocal(out=st[:, 2:3], in_=st[:, 1:2]), reads=[st], writes=[st])
            fw.op("dve", lambda e: e.scalar_tensor_tensor(out=h[:], in0=xb[:], scalar=st[:, 2:3], in1=A[r][:], op0=ALU.mult, op1=ALU.mult),
                  reads=[xb, st, A[r]], writes=[h])
            fw.op("pool", lambda e: e.tensor_tensor(out=h[:], in0=h[:], in1=Bv[r][:], op=ALU.add), reads=[h, Bv[r]], writes=[h])
            fw.dma("act", s["h2"].t[tt * 128:(tt + 1) * 128, :], h[:], reads=[h], writes=[s["h2"]], key="r_st")
            for q4 in range(4):
                pb = self.bank()
                for j in range(4):
                    kt = q4 * 4 + j
                    fw.op("pe", lambda e, kt=kt, j=j: e.transpose(out=pb[:, j * 128:(j + 1) * 128], in_=h[:, kt * 128:(kt + 1) * 128], identity=self.ident[:]),
                          reads=[h, self.ident], writes=[pb])
                fw.op("act" if q4 % 2 else "dve",
                      (lambda e: e.activation(out=hT[:, q4 * 4:(q4 + 1) * 4, :], in_=pb[:].rearrange("p (j t) -> p j t", j=4), func=AF.Copy)) if q4 % 2 else
                      (lambda e: e.tensor_copy(out=hT[:, q4 * 4:(q4 + 1) * 4, :], in_=pb[:].rearrange("p (j t) -> p j t", j=4))),
                      reads=[pb], writes=[hT])
            pl = self.bank()
            for kt in range(KT):
                fw.op("pe", lambda e, kt=kt: e.matmul(pl[:, 0:16], hT[:, kt, :], rt[:, kt, :], start=(kt == 0), stop=(kt == KT - 1)), reads=[hT, rt], writes=[pl])
            fw.op("dve", lambda e: e.tensor_copy(out=lg[:], in_=pl[:, 0:16]), reads=[pl], writes=[lg])
            fw.op("dve", lambda e: e.tensor_reduce(out=st[:, 3:4], in_=lg[:], axis=AX.X, op=ALU.max), reads=[lg], writes=[st])
            fw.op("dve", lambda e: e.tensor_scalar(out=st[:, 4:5], in0=st[:, 3:4], scalar1=-1.0, scalar2=None, op0=ALU.mult), reads=[st], writes=[st])
            fw.op("act", lambda e: e.activation(out=ex[:], in_=lg[:], func=AF.Exp, bias=st[:, 4:5], scale=1.0, accum_out=st[:, 5:6]), reads=[lg, st], writes=[ex, st])
            fw.op("dve", lambda e: e.reciprocal(out=st[:, 6:7], in_=st[:, 5:6]), reads=[st], writes=[st])
            fw.op("dve", lambda e: e.tensor_scalar(out=ex[:], in0=ex[:], scalar1=st[:, 6:7], scalar2=None, op0=ALU.mult), reads=[ex, st], writes=[ex])
            pt = self.bank()
            fw.op("pe", lambda e: e.transpose(out=pt[0:16, 0:128], in_=ex[:], identity=self.ident[:]), reads=[ex, self.ident], writes=[pt])
            fw.op("act", lambda e: e.activation(out=affT[0:16, tt * 128:(tt + 1) * 128], in_=pt[0:16, 0:128], func=AF.Copy), reads=[pt], writes=[affT])

    def p4b_topk(self, l, es):
        fw = self.fw
        affT = self.affT
        vals, idxu = self.tk_vals, self.tk_idx
        segs = [(0, L, 1024, 0)] + ([(L, AT, 32, 1024)] if l == 0 else [])
        for (a, b, cap, off) in segs:
            for rd in range(cap // 8):
                o = off + rd * 8
                fw.op("dve", lambda e: e.max(out=vals[0:16, o:o + 8], in_=affT[0:16, a:b]), reads=[affT], writes=[vals])
                fw.op("dve", lambda e: e.max_index(out=idxu[0:16, o:o + 8], in_max=vals[0:16, o:o + 8], in_values=affT[0:16, a:b]), reads=[affT, vals], writes=[idxu])
                fw.op("dve", lambda e: e.match_replace(out=affT[0:16, a:b], in_to_replace=vals[0:16, o:o + 8], in_values=affT[0:16, a:b], imm_value=-1.0),
                      reads=[affT, vals], writes=[affT])
        nch = 9 if l == 0 else 8
        idxf = fw.sb("tk_idxf", [16, 1152])
        fw.op("dve", lambda e: e.tensor_copy(out=idxf[:], in_=idxu[:]), reads=[idxu], writes=[idxf])
        if l == 0:
            fw.op("dve", lambda e: e.tensor_scalar(out=idxf[:, 1024:1056], in0=idxf[:, 1024:1056], scalar1=float(L), scalar2=None, op0=ALU.add), reads=[idxf], writes=[idxf])
        for j in range(nch):
            n = 128 if j < 8 else 32
            pt = self.bank()
            fw.op("pe", lambda e: e.transpose(out=pt[0:n, 0:16], in_=idxf[0:16, j * 128:j * 128 + n], identity=self.ident[0:16, 0:16]), reads=[idxf, self.ident], writes=[pt])
            fw.op("dve", lambda e: e.tensor_copy(out=self.idxT[0:n, j, :], in_=pt[0:n, 0:16]), reads=[pt], writes=[self.idxT])
            pt2 = self.bank()
            fw.op("pe", lambda e: e.transpose(out=pt2[0:n, 0:16], in_=vals[0:16, j * 128:j * 128 + n], identity=self.ident[0:16, 0:16]), reads=[vals, self.ident], writes=[pt2])
            fw.op("act", lambda e: e.activation(out=self.gT[0:n, j, :], in_=pt2[0:n, 0:16], func=AF.Copy), reads=[pt2], writes=[self.gT])

    def p4c_experts(self, l, es):
        fw, d, s = self.fw, self.d, self.s
        xs = [fw.sb("e_xs%d" % i, [128, D]) for i in range(2)]
        xsT = fw.sb("e_xsT", [128, KT, 512])
        zT = fw.sb("e_zT", [128, KT, 512])
        w1c = [fw.sb("e_w1c%d" % i, [128, KT, 128]) for i in range(2)]
        w3c = [fw.sb("e_w3c%d" % i, [128, KT, 128]) for i in range(2)]
        ysc = [fw.sb("e_ysc%d" % i, [128, D]) for i in range(4)]
        sa = fw.sb("e_sa", [128, 512])
        g2 = [fw.sb("e_g2_%d" % r, [128, D]) for r in range(2)]
        for r in range(2):
            self.load_bcast("sp", g2[r], s["modD"], r * 6 * D + 5 * D, D, "small")
        wi = 0
        groups = []
        for e_ in range(16):
            groups.append((e_, [0, 1, 2, 3], 128, 0))
            groups.append((e_, [4, 5, 6, 7], 128, 0))
        if l == 0:
            for e_ in range(16):
                groups.append((e_, [8], 32, 1))
        for (e_, chunks, n, r) in groups:
            ntok = n * len(chunks)
            for ci, j in enumerate(chunks):
                xb = xs[ci % 2]
                fw.idma(out=xb[0:n, :], out_offset=None, in_=s["h2"].t[:, :],
                        in_offset=bass.IndirectOffsetOnAxis(ap=self.idxT[0:n, j, e_:e_ + 1], axis=0),
                        reads=[s["h2"], self.idxT], writes=[xb], key="e_g%d" % (ci % 2))
                for q4 in range(4):
                    pb = self.bank()
                    for jj in range(4):
                        kt = q4 * 4 + jj
                        fw.op("pe", lambda e, kt=kt, jj=jj: e.transpose(out=pb[:, jj * 128:jj * 128 + n], in_=xb[0:n, kt * 128:(kt + 1) * 128], identity=self.ident[0:n, 0:n]),
                              reads=[xb, self.ident], writes=[pb])
                    src = pb[:].rearrange("p (j t) -> p j t", j=4)[:, :, 0:n]
                    if q4 % 2:
                        fw.op("act", lambda e: e.activation(out=xsT[:, q4 * 4:(q4 + 1) * 4, ci * n:(ci + 1) * n], in_=src, func=AF.Copy), reads=[pb], writes=[xsT])
                    else:
                        fw.op("dve", lambda e: e.tensor_copy(out=xsT[:, q4 * 4:(q4 + 1) * 4, ci * n:(ci + 1) * n], in_=src), reads=[pb], writes=[xsT])
            for ft in range(KT):
                w1b = w1c[wi % 2]; w3b = w3c[wi % 2]; wi += 1
                fw.dma("sp", w1b[:], d["exp_w1"].t[l, e_, :, ft * 128:(ft + 1) * 128].rearrange("(kt p) n -> p kt n", p=128),
                       reads=[d["exp_w1"]], writes=[w1b], key="e_w1%d" % (wi % 2))
                fw.dma("act", w3b[:], d["exp_w3"].t[l, e_, :, ft * 128:(ft + 1) * 128].rearrange("(kt p) n -> p kt n", p=128),
                       reads=[d["exp_w3"]], writes=[w3b], key="e_w3%d" % (wi % 2))
                pa = self.bank(); pg = self.bank()
                for kt in range(KT):
                    fw.op("pe", lambda e, kt=kt: e.matmul(pa[:, 0:ntok], w1b[:, kt, :], xsT[:, kt, 0:ntok], start=(kt == 0), stop=(kt == KT - 1)), reads=[w1b, xsT], writes=[pa])
                for kt in range(KT):
                    fw.op("pe", lambda e, kt=kt: e.matmul(pg[:, 0:ntok], w3b[:, kt, :], xsT[:, kt, 0:ntok], start=(kt == 0), stop=(kt == KT - 1)), reads=[w3b, xsT], writes=[pg])
                fw.op("act", lambda e: e.activation(out=sa[:, 0:ntok], in_=pa[:, 0:ntok], func=AF.Silu), reads=[pa], writes=[sa])
                fw.op("dve", lambda e: e.tensor_tensor(out=zT[:, ft, 0:ntok], in0=pg[:, 0:ntok], in1=sa[:, 0:ntok], op=ALU.mult), reads=[pg, sa], writes=[zT])
            for dc in range(4):
                fw.dma("sp" if dc % 2 else "act", xsT[:], d["exp_w2"].t[l, e_, :, dc * 512:(dc + 1) * 512].rearrange("(kt p) n -> p kt n", p=128),
                       reads=[d["exp_w2"]], writes=[xsT])
                for ci, j in enumerate(chunks):
                    py = self.bank()
                    for ft in range(KT):
                        fw.op("pe", lambda e, ft=ft: e.matmul(py[0:n, :], zT[:, ft, ci * n:(ci + 1) * n], xsT[:, ft, :], start=(ft == 0), stop=(ft == KT - 1)), reads=[zT, xsT], writes=[py])
                    fw.op("dve", lambda e: e.scalar_tensor_tensor(out=ysc[ci][0:n, dc * 512:(dc + 1) * 512], in0=py[0:n, :], scalar=self.gT[0:n, j, e_:e_ + 1],
                                                                  in1=g2[r][0:n, dc * 512:(dc + 1) * 512], op0=ALU.mult, op1=ALU.mult),
                          reads=[py, self.gT, g2[r]], writes=[ysc[ci]])
            for ci, j in enumerate(chunks):
                fw.idma(out=s["xa"].t[:, :], out_offset=bass.IndirectOffsetOnAxis(ap=self.idxT[0:n, j, e_:e_ + 1], axis=0), in_=ysc[ci][0:n, :], in_offset=None,
                        reads=[ysc[ci], self.idxT], writes=[s["xa"]], key="e_sc", compute_op=ALU.add)

    def phase(self, fn, *a):
        fw = self.fw
        with ExitStack() as es:
            old = fw.es; fw.es = es
            fn(*a)
            fw.barrier()
            fw.es = old

    def p4_moe(self, l, es):
        fw = self.fw
        self.idxT = fw.sb("idxT", [128, 9, 16], I32); self.gT = fw.sb("gT", [128, 9, 16])
        with ExitStack() as es2:
            old = fw.es; fw.es = es2
            self.affT = fw.sb("affT", [16, AT])
            self.tk_vals = fw.sb("tk_vals", [16, 1152]); self.tk_idx = fw.sb("tk_idx", [16, 1152], U32)
            self.phase(self.p4a_router, l, None)
            self.phase(self.p4b_topk, l, None)
            fw.es = old
        self.phase(self.p4c_experts, l, None)

    def build(self):
        fw = self.fw
        xin = (self.d["x"], self.d["ctx"])
        for l in range(DEPTH):
            if self.test_br and l == 0:
                brin = fw.dram("brT_in", [3 * W, AT], kind="ExternalInput")
                for i in range(12):
                    fw.dma("sp", self.s["brT"].t[i * 128:(i + 1) * 128, :], brin.t[i * 128:(i + 1) * 128, :], reads=[brin], writes=[self.s["brT"]], key="brcp")
            self.phase(self.p0_mod, l, None)
            if self.stop_after == ("p0", l):
                break
            self.phase(self.p1_proj, l, None, xin)
            if self.stop_after == ("p1", l):
                break
            if not self.test_br:
                self.phase(self.p2a_s5, l, None)
                self.phase(self.p2a_glu, l, None)
            if self.stop_after == ("p2a", l):
                break
            if not self.test_br:
                self.phase(self.p2b_na_norm, l, None)
                self.phase(self.p2b_na, l, None)
            if self.stop_after == ("p2b", l):
                break
            if not self.test_br:
                self.phase(self.p2c_hy_short, l, None)
                for seg in ((0, 1) if l == 0 else (0,)):
                    self.phase(self.p2c_hy_filt, l, None, seg)
                    self.phase(self.p2c_hy_spec, l, None, seg)
                    self.phase(self.p2c_hy_conv, l, None, seg)
            if self.stop_after == ("p2c", l):
                break
            self.phase(self.p3_merge, l, None, xin)
            if self.stop_after == ("p3", l):
                break
            self.phase(self.p4_moe, l, None)
            if self.stop_after == ("p4", l):
                break
            xin = self.s["xa"]
        else:
            for i in range(64):
                fw.dma("sp" if i % 2 else "act", self.out.t[i * 128:(i + 1) * 128, :], self.s["xa"].t[i * 128:(i + 1) * 128, :], reads=[self.s["xa"]], writes=[self.out], key="outcp")
        for (oname, name, sl) in self.dbg:
            src = self.s[name]
            r0, r1, c0, c1 = sl
            o = fw.dram("dbg_" + oname, [r1 - r0, c1 - c0], kind="ExternalOutput")
            fw.dma("sp", o.t, src.t[r0:r1, c0:c1], reads=[src], writes=[o], key="dbg")
            self.dbg_out[oname] = o
        fw.finish([self.out] + list(self.dbg_out.values()))


def build_nc(stop_after=None, dbg=None, gather=True, test_br=False, wdepth=DEPTH):
    nc = bass.Bass("TRN2", target_bir_lowering=False)
    es = ExitStack()
    with es:
        k = K(nc, es, stop_after=stop_after, dbg=dbg, gather=gather, test_br=test_br, wdepth=wdepth)
        k.build()
    return nc, k


BIGW = ["w_ada", "w_in", "w_gate", "w_branch", "w_out", "exp_w1", "exp_w3", "exp_w2"]
WNAMES = ["hy_conv_w", "hy_conv_b", "hy_w1", "hy_b1", "hy_w2", "hy_b2", "hy_w3", "hy_b3", "hy_w4", "hy_freq", "hy_bias", "na_q_gain", "na_k_gain", "ssm_lam_re", "ssm_lam_im", "ssm_log_step", "ssm_b_re", "ssm_b_im", "ssm_c_re", "ssm_c_im", "ssm_d", "ssm_w_glu", "w_ada", "b_ada", "norm1", "norm2", "w_in", "w_gate", "b_gate", "w_branch", "w_out", "router", "exp_w1", "exp_w3", "exp_w2"]


def na_table(rpb):
    Lr = rpb.shape[0]
    col = np.arange(64)
    startc = np.clip(col - 8, 0, 48)
    kc = np.arange(64)
    inwin = (kc[None, :] >= startc[:, None]) & (kc[None, :] < startc[:, None] + 16)
    dc = np.clip(kc[None, :] - col[:, None] + 15, 0, 30)
    tab = np.empty((Lr, 8, 8, 8, 64, 64), np.float32)
    for dr0 in range(8):
        for j in range(8):
            v = rpb[:, :, j + dr0, :][:, :, dc]
            v = np.where(inwin[None, None], v, np.float32(-30000.0))
            tab[:, dr0, :, j] = np.transpose(v, (0, 1, 3, 2))
    tab = tab.reshape(Lr, 8, 8, 4, 128, 64)
    return np.ascontiguousarray(np.transpose(tab, (0, 1, 4, 2, 3, 5)))


def hyena_consts():
    n = np.arange(128, dtype=np.float64)
    ang = 2 * np.pi * np.outer(n, n) / 128.0
    F = np.stack([np.cos(ang), -np.sin(ang), np.sin(ang)], axis=1).astype(np.float32)
    angt = 2 * np.pi * np.outer(n, n) / 16384.0
    T = np.stack([np.cos(angt), -np.sin(angt)], axis=1).astype(np.float32)
    out = {"hyc_F": F, "hyc_T": T}
    for nm, length in (("L", L), ("c", CT)):
        t = np.linspace(0.0, 1.0, length, dtype=np.float32)[:, None]
        freqs = np.linspace(1e-4, 15, 16, dtype=np.float32)
        a = (np.float32(2.0 * np.pi / length) * np.arange(length, dtype=np.float32)[:, None] * freqs[None, :]).astype(np.float32)
        z = np.concatenate([t, np.cos(a), -np.sin(a)], axis=-1).astype(np.float32)
        mn, mx = np.log(1e-2) / 1.5, np.log(1e-2) / 0.3
        dec = np.exp(-t * np.abs(np.linspace(mn, mx, W, dtype=np.float32))[None, :]).astype(np.float32)
        out["hyc_z" + nm] = np.ascontiguousarray(z.T)
        out["hyc_d" + nm] = np.ascontiguousarray(dec.T)
    return out


def make_in_maps(inputs, cores, used=None, gather=True):
    maps = []
    shared = {}
    shards = {}
    for n in WNAMES:
        if used is not None and n not in used:
            continue
        a = np.ascontiguousarray(inputs[n])
        if n in BIGW and gather:
            a2 = a.reshape(DEPTH, -1, a.shape[-1])
            R = a2.shape[1]
            shards[n] = [np.ascontiguousarray(a2[:, c * (R // 8):(c + 1) * (R // 8), :]) for c in range(8)]
        else:
            shared[n] = a
    for n_, v_ in hyena_consts().items():
        if used is None or n_ in used:
            shared[n_] = v_
    if used is None or "na_tab" in used:
        shared["na_tab"] = na_table(np.asarray(inputs["na_rpb"], np.float32))
    for c in cores:
        b = c % 4
        m = dict(shared)
        for n in shards:
            m[n + "_sh"] = shards[n][c]
        if used is None or "x" in used:
            m["x"] = np.ascontiguousarray(inputs["x"][b])
        if used is None or "ctx" in used:
            m["ctx"] = np.ascontiguousarray(inputs["ctx"][b])
        m["cvec"] = np.ascontiguousarray(np.stack([inputs["c"][b], inputs["c_ctx"]], axis=0))
        maps.append(m)
    return maps


def kernel(**inputs):
    nc, k = build_nc(gather=False)
    cores = list(range(8))
    res = run_bass_kernel_spmd(nc, make_in_maps(inputs, cores, set(k.d.keys()), gather=False), core_ids=cores)
    out = np.stack([res.results[b]["out"] for b in range(4)], axis=0)
    return out.astype(np.float32)
```

```python
import numpy as np
import concourse.bass as bass
import concourse.mybir as mybir
from concourse.bass_utils import run_bass_kernel_spmd
from contextlib import ExitStack

F32 = mybir.dt.float32
BF16 = mybir.dt.bfloat16
U32 = mybir.dt.uint32
I32 = mybir.dt.int32
ALU = mybir.AluOpType
AF = mybir.ActivationFunctionType
AX = mybir.AxisListType

D = 2048
L = 8192
CT = 256
AT = L + CT
NT = AT // 128
KT = D // 128
W = 512
INW = 3584
DEPTH = 2
EPS = 1e-6
PI = float(np.pi)


class Buf:
    __slots__ = ("t", "last_w", "readers", "name", "space")

    def __init__(self, t, name="", space="sb"):
        self.t = t
        self.last_w = None
        self.readers = []
        self.name = name
        self.space = space

    def __getitem__(self, k):
        return self.t[k]


class _LV:
    def __init__(self, views):
        self.views = views

    def __getitem__(self, k):
        if isinstance(k, tuple):
            v = self.views[k[0]]
            return v[k[1:]] if len(k) > 1 else v
        return self.views[k]


class LayerView(Buf):
    __slots__ = ()

    def __init__(self, views, name):
        Buf.__init__(self, _LV(views), name, "dram")


class FW:
    ENG = ("pe", "act", "dve", "pool", "sp")

    def __init__(self, nc, es):
        self.nc = nc
        self.es = es
        self.root_es = es
        self.eng = {"pe": nc.tensor, "act": nc.scalar, "dve": nc.vector, "pool": nc.gpsimd, "sp": nc.sync}
        self.esem = {k: es.enter_context(nc.semaphore("es_" + k)) for k in self.ENG}
        self.ecnt = {k: 0 for k in self.ENG}
        self.dslots = []
        self.dcnt = []
        self.kmap = {}
        self.seen = {k: {} for k in self.ENG}
        self.n_inst = 0
        self.bufs = []
        self.uniq = 0

    def _reg(self, b):
        self.bufs.append(b)
        return b

    def sb(self, name, shape, dt=F32):
        self.uniq += 1
        name = "%s_%d" % (name, self.uniq)
        return self._reg(Buf(self.es.enter_context(self.nc.sbuf_tensor(name, list(shape), dt)), name, "sb"))

    def ps(self, name, shape, dt=F32):
        return self._reg(Buf(self.es.enter_context(self.nc.psum_tensor(name, list(shape), dt)), name, "ps"))

    def dram(self, name, shape, dt=F32, kind="Internal"):
        return self._reg(Buf(self.nc.dram_tensor(name, list(shape), dt, kind=kind).ap(), name, "dram"))

    def _slot(self, key):
        if key not in self.kmap:
            i = len(self.kmap)
            if i >= len(self.dslots):
                self.dslots.append(self.root_es.enter_context(self.nc.semaphore("ds%d" % i)))
                self.dcnt.append(0)
            self.kmap[key] = i
        return self.kmap[key]

    def _wait(self, e, ev):
        if ev is None:
            return
        kind, key, val = ev
        if kind == "d":
            val = self.dcnt[key]
            sem = self.dslots[key]
        else:
            sem = self.esem[key]
        k = (kind, key)
        if self.seen[e].get(k, 0) >= val:
            return
        self.seen[e][k] = val
        self.eng[e].wait_ge(sem, val)

    def _deps(self, e, reads, writes):
        for r in reads:
            self._wait(e, r.last_w)
        for w in writes:
            self._wait(e, w.last_w)
            for ev in w.readers:
                self._wait(e, ev)

    def _commit(self, ev, reads, writes):
        for r in reads:
            r.readers.append(ev)
            if len(r.readers) > 48:
                d = {}
                for x in r.readers:
                    k = (x[0], x[1])
                    if k not in d or d[k][2] < x[2]:
                        d[k] = x
                r.readers = list(d.values())
        for w in writes:
            w.last_w = ev
            w.readers = []

    def op(self, e, fn, reads=(), writes=()):
        self._deps(e, reads, writes)
        ins = fn(self.eng[e])
        self.ecnt[e] += 1
        ins.then_inc(self.esem[e], 1)
        ev = ("e", e, self.ecnt[e])
        self._commit(ev, reads, writes)
        self.n_inst += 1
        return ev

    def _stream(self, reads, writes, key):
        for b in list(writes) + list(reads):
            if b.space == "sb":
                return self._slot("b_" + b.name)
        if key is None:
            self.uniq += 1
            key = "u%d" % self.uniq
        return self._slot("k_" + key)

    def _issued(self, slot, ins, inc, reads, writes):
        self.dcnt[slot] += inc
        ins.then_inc(self.dslots[slot], inc)
        ev = ("d", slot, self.dcnt[slot])
        self._commit(ev, reads, writes)
        self.n_inst += 1
        return ev

    def dma(self, q, out, in_, reads=(), writes=(), key=None, **kw):
        slot = self._stream(reads, writes, key)
        self._deps(q, reads, writes)
        ins = self.eng[q].dma_start(out=out, in_=in_, **kw)
        return self._issued(slot, ins, 16, reads, writes)

    def idma(self, out, in_, out_offset=None, in_offset=None, reads=(), writes=(), key=None, **kw):
        slot = self._stream([r for r in reads if r.name != "idxT"], writes, None)
        self._deps("pool", reads, writes)
        ins = self.nc.gpsimd.indirect_dma_start(out=out, out_offset=out_offset, in_=in_, in_offset=in_offset, **kw)
        return self._issued(slot, ins, 16, reads, writes)

    def allgather(self, out_buf, out_ap, in_buf, in_ap):
        slot = self._stream([], [], None)
        self._deps("pool", [in_buf], [out_buf])
        ins = self.nc.gpsimd.collective_compute("AllGather", ALU.bypass, replica_groups=[list(range(8))], ins=[in_ap], outs=[out_ap])
        return self._issued(slot, ins, 1, [in_buf], [out_buf])

    def barrier(self):
        for e in self.ENG:
            for o in self.ENG:
                if o != e and self.ecnt[o] > 0:
                    self._wait(e, ("e", o, self.ecnt[o]))
            for slot in range(len(self.dslots)):
                if self.dcnt[slot] > 0:
                    self._wait(e, ("d", slot, self.dcnt[slot]))
        for b in self.bufs:
            b.last_w = None
            b.readers = []
        self.kmap = {}

    def finish(self, bufs):
        for slot in range(len(self.dslots)):
            if self.dcnt[slot] > 0:
                self._wait("sp", ("d", slot, self.dcnt[slot]))


def dap(buf, offset, pattern):
    return bass.AP(tensor=buf.t.tensor, offset=offset, ap=[list(p) for p in pattern])


class K:
    def __init__(self, nc, es, stop_after=None, dbg=None, gather=True, test_br=False, wdepth=DEPTH):
        self.nc = nc
        self.gather = gather
        self.test_br = test_br
        self.dumps_on = test_br
        self.fw = FW(nc, es)
        self.stop_after = stop_after
        self.dbg = dbg or []
        fw = self.fw
        shapes = {
            "x": [L, D], "ctx": [CT, D], "cvec": [2, D],
            "w_ada": [DEPTH, D, 6 * D], "b_ada": [DEPTH, 6 * D], "norm1": [DEPTH, D], "norm2": [DEPTH, D],
            "w_in": [DEPTH, D, INW], "w_gate": [DEPTH, D, 3 * D], "b_gate": [DEPTH, 3 * D],
            "w_branch": [DEPTH, 3, W, D], "w_out": [DEPTH, D, D], "router": [DEPTH, D, 16],
            "exp_w1": [DEPTH, 16, D, D], "exp_w3": [DEPTH, 16, D, D], "exp_w2": [DEPTH, 16, D, D],
            "ssm_lam_re": [DEPTH, 2, 32, 64], "ssm_lam_im": [DEPTH, 2, 32, 64], "ssm_log_step": [DEPTH, 2, 32],
            "ssm_b_re": [DEPTH, 2, 32, 64, 16], "ssm_b_im": [DEPTH, 2, 32, 64, 16],
            "ssm_c_re": [DEPTH, 2, 32, 16, 64], "ssm_c_im": [DEPTH, 2, 32, 16, 64],
            "ssm_d": [DEPTH, 512], "ssm_w_glu": [DEPTH, 512, 512],
            "na_q_gain": [DEPTH, 64], "na_k_gain": [DEPTH, 64], "na_tab": [DEPTH, 8, 128, 8, 4, 64],
            "hy_conv_w": [DEPTH, 3, 1536], "hy_conv_b": [DEPTH, 1536], "hy_w1": [DEPTH, 33, 64], "hy_b1": [DEPTH, 64],
            "hy_w2": [DEPTH, 64, 64], "hy_b2": [DEPTH, 64], "hy_w3": [DEPTH, 64, 64], "hy_b3": [DEPTH, 64],
            "hy_w4": [DEPTH, 64, 2048], "hy_freq": [DEPTH, 64], "hy_bias": [DEPTH, 2, 512],
            "hyc_F": [128, 3, 128], "hyc_T": [128, 2, 128], "hyc_zL": [33, L], "hyc_zc": [33, CT], "hyc_dL": [W, L], "hyc_dc": [W, CT],
        }

        BIG = set(BIGW)
        gather_mode = self.gather

        class Lazy(dict):
            def __missing__(s_, n):
                shp = shapes[n]
                if n in BIG and gather_mode:
                    R = int(np.prod(shp[1:-1])); C = shp[-1]
                    ext = fw.dram(n + "_sh", [DEPTH, R // 8, C], F32, kind="ExternalInput")
                    views = []
                    for l in range(DEPTH):
                        shl = fw.dram(n + "_shi%d" % l, [R // 8, C], F32)
                        fl = fw.dram(n + "_full%d" % l, [R, C], F32)
                        for r0 in range(0, R // 8, 128):
                            r1 = min(r0 + 128, R // 8)
                            fw.dma("sp" if (r0 // 128) % 2 else "act", shl.t[r0:r1, :], ext.t[l, r0:r1, :], reads=[ext], writes=[shl], key="shcp_%s%d" % (n, l))
                        fw.allgather(fl, fl.t, shl, shl.t)
                        views.append(fl.t.rearrange("(a r) c -> a r c", a=shp[1]) if len(shp) == 4 else fl.t)
                    b = LayerView(views, n)
                    fw._reg(b)
                    s_[n] = b
                    return b
                if n in BIG:
                    shp = [wdepth] + list(shp[1:])
                b = fw.dram(n, shp, F32, kind="ExternalInput")
                s_[n] = b
                return b
        d = Lazy()
        self.d = d
        if self.gather:
            for n in BIGW:
                d[n]
            fw.barrier()
        s = {}
        s["modD"] = fw.dram("modD", [2, 6 * D])
        s["hT"] = fw.dram("hT", [128, KT, AT])
        s["projT"] = fw.dram("projT", [INW, AT])
        s["vtok"] = fw.dram("vtok", [AT, W])
        s["brT"] = fw.dram("brT", [3 * W, AT])
        s["s5z"] = fw.dram("s5z", [W, AT])
        s["qnT"] = fw.dram("qnT", [W, AT]); s["knT"] = fw.dram("knT", [W, AT])
        s["hzT"] = fw.dram("hzT", [3 * W, AT]); s["hfT"] = fw.dram("hfT", [2048, L]); s["hfTc"] = fw.dram("hfTc", [2048, CT])
        s["hnrm"] = fw.dram("hnrm", [2, 2, W])
        s["hspec"] = fw.dram("hspec", [2, 128, 128, 2, 512]); s["hspecc"] = fw.dram("hspecc", [2, 128, 128, 2, 512])
        s["xa"] = fw.dram("xa", [AT, D])
        s["h2"] = fw.dram("h2", [AT, D])
        self.s = s
        self.out = fw.dram("out", [L, D], kind="ExternalOutput")
        self.dbg_out = {}
        self.ident = fw.sb("ident", [128, 128])
        io = fw.sb("iota_i", [128, 128], I32)
        fw.op("pool", lambda e: e.iota(io[:], pattern=[[1, 128]], base=0, channel_multiplier=-1), writes=[io])
        fw.op("dve", lambda e: e.tensor_single_scalar(out=self.ident[:], in_=io[:], scalar=0, op=ALU.is_equal), reads=[io], writes=[self.ident])
        self.pb = [fw.ps("pb%d" % i, [128, 512]) for i in range(8)]
        self.pbi = 0

    def dump(self, name, buf, ap):
        if not self.dumps_on:
            return
        o = self.fw.dram("dbg_" + name, list(ap.shape), kind="ExternalOutput")
        self.fw.dma("sp", o.t, ap, reads=[buf], writes=[o])
        self.dbg_out[name] = o

    def bank(self):
        b = self.pb[self.pbi % 8]
        self.pbi += 1
        return b

    def p0_mod(self, l, es):
        fw, d, s = self.fw, self.d, self.s
        cv = fw.sb("cv", [2, D]); sil2 = fw.sb("silc2", [128, KT, 2])
        wa = [fw.sb("wada%d" % i, [128, KT, 512]) for i in range(2)]
        bb = fw.sb("bada", [2, 512]); mo = [fw.sb("modo%d" % r, [1, 512]) for r in range(2)]
        fw.dma("sp", cv[:], d["cvec"].t, reads=[d["cvec"]], writes=[cv], key="small")
        pbt = self.bank()
        for kt in range(KT):
            fw.op("pe", lambda e, kt=kt: e.transpose(out=pbt[:, kt * 2:(kt + 1) * 2], in_=cv[0:2, kt * 128:(kt + 1) * 128], identity=self.ident[0:2, 0:2]),
                  reads=[cv, self.ident], writes=[pbt])
        fw.op("act", lambda e: e.activation(out=sil2[:].rearrange("p k r -> p (k r)"), in_=pbt[:, 0:32], func=AF.Silu), reads=[pbt], writes=[sil2])
        for j in range(24):
            wb = wa[j % 2]
            src = d["w_ada"].t[l, :, j * 512:(j + 1) * 512].rearrange("(kt p) n -> p kt n", p=128)
            fw.dma("sp" if j % 2 == 0 else "act", wb[:], src, reads=[d["w_ada"]], writes=[wb], key="wada%d" % (j % 2))
            fw.dma("pool", bb[:], dap(d["b_ada"], l * 6 * D + j * 512, [[0, 2], [1, 512]]), reads=[d["b_ada"]], writes=[bb], key="small")
            for r in range(2):
                pb = self.bank()
                for kt in range(KT):
                    fw.op("pe", lambda e, kt=kt: e.matmul(pb[0:1, :], sil2[:, kt, r:r + 1], wb[:, kt, :], start=(kt == 0), stop=(kt == KT - 1)),
                          reads=[sil2, wb], writes=[pb])
                fw.op("dve", lambda e: e.tensor_tensor(out=mo[r][:], in0=pb[0:1, :], in1=bb[0:1, :], op=ALU.add), reads=[pb, bb], writes=[mo[r]])
                fw.dma("sp", s["modD"].t[r:r + 1, j * 512:(j + 1) * 512], mo[r][:], reads=[mo[r]], writes=[s["modD"]], key="modst")

    def load_bcast(self, q, dst, src_buf, offset, n, key=None, ap=None):
        ap = dst[:] if ap is None else ap
        self.fw.dma(q, ap, dap(src_buf, offset, [[0, ap.shape[0]], [1, n]]), reads=[src_buf], writes=[dst], key=key)

    def p1_proj(self, l, es, xin):
        fw, d, s = self.fw, self.d, self.s
        A = [fw.sb("A1_%d" % r, [128, D]) for r in range(2)]
        Bv = [fw.sb("B1_%d" % r, [128, D]) for r in range(2)]
        tmp = fw.sb("p1tmp", [128, D])
        self.load_bcast("sp", tmp, d["norm1"], l * D, D, "small")
        for r in range(2):
            self.load_bcast("act", A[r], s["modD"], r * 6 * D + 1 * D, D, "small")
            self.load_bcast("pool", Bv[r], s["modD"], r * 6 * D + 0 * D, D, "small")
            fw.op("dve", lambda e, r=r: e.scalar_tensor_tensor(out=A[r][:], in0=A[r][:], scalar=1.0, in1=tmp[:], op0=ALU.add, op1=ALU.mult),
                  reads=[A[r], tmp], writes=[A[r]])
        xt = [fw.sb("xt%d" % i, [128, D]) for i in range(2)]
        ht = [fw.sb("ht%d" % i, [128, D]) for i in range(2)]
        st = fw.sb("p1st", [128, 4])
        hTb = [fw.sb("hTb%d" % i, [128, KT, 512]) for i in range(1)]
        wch = [fw.sb("wch%d" % i, [128, KT, 512]) for i in range(2)]
        stg = [fw.sb("stg%d" % i, [128, 512]) for i in range(3)]
        nblk = 17
        wi = 0
        si = 0
        for blk in range(nblk):
            ntile = 4 if blk < 16 else 2
            ntok = ntile * 128
            hb = hTb[0]
            r = 0 if blk < 16 else 1
            for ti in range(ntile):
                tt = blk * 4 + ti
                xb = xt[tt % 2]; hh = ht[tt % 2]
                if isinstance(xin, tuple):
                    src = xin[0].t[tt * 128:(tt + 1) * 128, :] if tt < 64 else xin[1].t[(tt - 64) * 128:(tt - 63) * 128, :]
                    srcb = xin[0] if tt < 64 else xin[1]
                else:
                    src = xin.t[tt * 128:(tt + 1) * 128, :]; srcb = xin
                fw.dma("sp", xb[:], src, reads=[srcb], writes=[xb], key="xld%d" % (tt % 2))
                fw.op("act", lambda e: e.activation(out=hh[:], in_=xb[:], func=AF.Square, accum_out=st[:, 0:1]), reads=[xb], writes=[hh, st])
                fw.op("act", lambda e: e.activation(out=st[:, 1:2], in_=st[:, 0:1], func=AF.Sqrt, scale=1.0 / D, bias=EPS), reads=[st], writes=[st])
                fw.op("dve", lambda e: e.reciprocal(out=st[:, 2:3], in_=st[:, 1:2]), reads=[st], writes=[st])
                fw.op("dve", lambda e: e.scalar_tensor_tensor(out=hh[:], in0=xb[:], scalar=st[:, 2:3], in1=A[r][:], op0=ALU.mult, op1=ALU.mult),
                      reads=[xb, st, A[r]], writes=[hh])
                fw.op("pool", lambda e: e.tensor_tensor(out=hh[:], in0=hh[:], in1=Bv[r][:], op=ALU.add), reads=[hh, Bv[r]], writes=[hh])
                for q4 in range(4):
                    pb = self.bank()
                    for j in range(4):
                        kt = q4 * 4 + j
                        fw.op("pe", lambda e, kt=kt, j=j: e.transpose(out=pb[:, j * 128:(j + 1) * 128], in_=hh[:, kt * 128:(kt + 1) * 128], identity=self.ident[:]),
                              reads=[hh, self.ident], writes=[pb])
                    eng = "act" if q4 % 2 == 0 else "dve"
                    if eng == "act":
                        fw.op("act", lambda e: e.activation(out=hb[:, q4 * 4:(q4 + 1) * 4, ti * 128:(ti + 1) * 128],
                                                            in_=pb[:].rearrange("p (j t) -> p j t", j=4), func=AF.Copy), reads=[pb], writes=[hb])
                    else:
                        fw.op("dve", lambda e: e.tensor_copy(out=hb[:, q4 * 4:(q4 + 1) * 4, ti * 128:(ti + 1) * 128],
                                                             in_=pb[:].rearrange("p (j t) -> p j t", j=4)), reads=[pb], writes=[hb])
            for q in range(4):
                fw.dma("sp" if q % 2 else "act", s["hT"].t[:, q * 4:(q + 1) * 4, blk * 512: blk * 512 + ntok], hb[:, q * 4:(q + 1) * 4, 0:ntok], reads=[hb], writes=[s["hT"]], key="hTst")
            if blk == 0 and l == 0:
                self.dump("hb_sb", hb, hb[:, 0, 0:128])
                self.dump("hT_dr", s["hT"], s["hT"].t[:, 0, 0:128])
            for cc in range(7):
                wb = wch[wi % 2]; wi += 1
                src = d["w_in"].t[l, :, cc * 512:(cc + 1) * 512].rearrange("(kt p) n -> p kt n", p=128)
                fw.dma("act" if wi % 2 else "sp", wb[:], src, reads=[d["w_in"]], writes=[wb], key="wch%d" % (wi % 2))
                if cc == 3:
                    for ti in range(ntile):
                        pb = self.bank()
                        for kt in range(KT):
                            fw.op("pe", lambda e, kt=kt: e.matmul(pb[:], hb[:, kt, ti * 128:(ti + 1) * 128], wb[:, kt, :], start=(kt == 0), stop=(kt == KT - 1)),
                                  reads=[hb, wb], writes=[pb])
                        sg = stg[si % 3]; si += 1
                        fw.op("act", lambda e: e.activation(out=sg[:], in_=pb[:], func=AF.Copy), reads=[pb], writes=[sg])
                        t0 = blk * 512 + ti * 128
                        fw.dma("pool", s["vtok"].t[t0:t0 + 128, :], sg[:], reads=[sg], writes=[s["vtok"]], key="pst")
                    continue
                for ct in range(4):
                    pb = self.bank()
                    for kt in range(KT):
                        fw.op("pe", lambda e, kt=kt: e.matmul(pb[:, 0:ntok], wb[:, kt, ct * 128:(ct + 1) * 128], hb[:, kt, 0:ntok], start=(kt == 0), stop=(kt == KT - 1)),
                              reads=[hb, wb], writes=[pb])
                    sg = stg[si % 3]; si += 1
                    if si % 2:
                        fw.op("act", lambda e: e.activation(out=sg[:, 0:ntok], in_=pb[:, 0:ntok], func=AF.Copy), reads=[pb], writes=[sg])
                    else:
                        fw.op("dve", lambda e: e.tensor_copy(out=sg[:, 0:ntok], in_=pb[:, 0:ntok]), reads=[pb], writes=[sg])
                    c0 = cc * 512 + ct * 128
                    fw.dma("pool", s["projT"].t[c0:c0 + 128, blk * 512: blk * 512 + ntok], sg[:, 0:ntok], reads=[sg], writes=[s["projT"]], key="pst")

    def sin_reduced(self, out, ang, tmp_i, tmp_f, bufs_r, bufs_w):
        fw = self.fw
        fw.op("dve", lambda e: e.tensor_scalar(out=tmp_i, in0=ang, scalar1=1.0 / (2 * PI), scalar2=None, op0=ALU.mult), reads=bufs_r, writes=bufs_w)
        fw.op("dve", lambda e: e.tensor_copy(out=tmp_f, in_=tmp_i), reads=bufs_w, writes=bufs_w)
        fw.op("dve", lambda e: e.scalar_tensor_tensor(out=ang, in0=tmp_f, scalar=-2 * PI, in1=ang, op0=ALU.mult, op1=ALU.add), reads=bufs_w + bufs_r, writes=bufs_r)
        fw.op("dve", lambda e: e.tensor_scalar(out=ang, in0=ang, scalar1=PI, scalar2=-PI, op0=ALU.min, op1=ALU.max), reads=bufs_r, writes=bufs_r)
        fw.op("act", lambda e: e.activation(out=out, in_=ang, func=AF.Sin), reads=bufs_r, writes=bufs_w)

    def p2a_s5(self, l, es):
        fw, d, s = self.fw, self.d, self.s
        T = AT
        NG = 32
        ld = fw.sb("s5_ld", [32, 3, 128])
        fw.dma("sp", ld[:, 0, :], d["ssm_lam_re"].t[l].rearrange("d (gp g2) p -> (d gp) (g2 p)", g2=2), reads=[d["ssm_lam_re"]], writes=[ld])
        fw.dma("sp", ld[:, 1, :], d["ssm_lam_im"].t[l].rearrange("d (gp g2) p -> (d gp) (g2 p)", g2=2), reads=[d["ssm_lam_im"]], writes=[ld])
        ls = fw.sb("s5_ls", [32, 2])
        fw.dma("sp", ls[:], d["ssm_log_step"].t[l].rearrange("d (gp g2) -> (d gp) g2", g2=2), reads=[d["ssm_log_step"]], writes=[ls])
        fw.op("dve", lambda e: e.tensor_copy(out=ld[:, 2, :].rearrange("a (g p) -> a g p", g=2), in_=ls[:].unsqueeze(2).to_broadcast([32, 2, 64])), reads=[ls], writes=[ld])
        par = fw.sb("s5_par", [128, 24, 32])
        P_ = lambda i: par[:, i, :]
        for i in range(3):
            pb = self.bank()
            fw.op("pe", lambda e, i=i: e.transpose(out=pb[:, 0:32], in_=ld[:, i, :], identity=self.ident[0:32, 0:32]), reads=[ld, self.ident], writes=[pb])
            fw.op("dve", lambda e, i=i: e.tensor_copy(out=P_(i), in_=pb[:, 0:32]), reads=[pb], writes=[par])
        LR, LI, STEP, MAG, ANG, ARE, AIM, TMP, DEN, NRE, CRE, CIM, T2, ANG2 = range(14)
        pi_ = fw.sb("s5_pari", [128, 32], I32)
        R, Wp = [par], [par, pi_]
        op = lambda eng, fn: fw.op(eng, fn, reads=[par, pi_], writes=[par, pi_])
        op("act", lambda e: e.activation(out=P_(STEP), in_=P_(2), func=AF.Exp))
        op("dve", lambda e: e.tensor_tensor(out=P_(MAG), in0=P_(LR), in1=P_(STEP), op=ALU.mult))
        op("act", lambda e: e.activation(out=P_(MAG), in_=P_(MAG), func=AF.Exp))
        op("dve", lambda e: e.tensor_tensor(out=P_(ANG), in0=P_(LI), in1=P_(STEP), op=ALU.mult))
        op("dve", lambda e: e.tensor_scalar(out=P_(ANG2), in0=P_(ANG), scalar1=PI / 2, scalar2=None, op0=ALU.add))
        self.sin_reduced(P_(AIM), P_(ANG), pi_[:], P_(TMP), [par], [par, pi_])
        self.sin_reduced(P_(ARE), P_(ANG2), pi_[:], P_(TMP), [par], [par, pi_])
        op("dve", lambda e: e.tensor_tensor(out=P_(ARE), in0=P_(ARE), in1=P_(MAG), op=ALU.mult))
        op("dve", lambda e: e.tensor_tensor(out=P_(AIM), in0=P_(AIM), in1=P_(MAG), op=ALU.mult))
        op("dve", lambda e: e.tensor_tensor(out=P_(DEN), in0=P_(LR), in1=P_(LR), op=ALU.mult))
        op("dve", lambda e: e.tensor_tensor(out=P_(TMP), in0=P_(LI), in1=P_(LI), op=ALU.mult))
        op("dve", lambda e: e.tensor_tensor(out=P_(DEN), in0=P_(DEN), in1=P_(TMP), op=ALU.add))
        op("dve", lambda e: e.reciprocal(out=P_(DEN), in_=P_(DEN)))
        op("dve", lambda e: e.tensor_scalar(out=P_(NRE), in0=P_(ARE), scalar1=-1.0, scalar2=None, op0=ALU.add))
        op("dve", lambda e: e.tensor_tensor(out=P_(CRE), in0=P_(NRE), in1=P_(LR), op=ALU.mult))
        op("dve", lambda e: e.tensor_tensor(out=P_(TMP), in0=P_(AIM), in1=P_(LI), op=ALU.mult))
        op("dve", lambda e: e.tensor_tensor(out=P_(CRE), in0=P_(CRE), in1=P_(TMP), op=ALU.add))
        op("dve", lambda e: e.tensor_tensor(out=P_(CRE), in0=P_(CRE), in1=P_(DEN), op=ALU.mult))
        op("dve", lambda e: e.tensor_tensor(out=P_(CIM), in0=P_(AIM), in1=P_(LR), op=ALU.mult))
        op("dve", lambda e: e.tensor_tensor(out=P_(TMP), in0=P_(NRE), in1=P_(LI), op=ALU.mult))
        op("dve", lambda e: e.tensor_tensor(out=P_(CIM), in0=P_(CIM), in1=P_(TMP), op=ALU.subtract))
        op("dve", lambda e: e.tensor_tensor(out=P_(CIM), in0=P_(CIM), in1=P_(DEN), op=ALU.mult))
        NS = 14
        pw = fw.sb("s5_pw", [128, NS, 3, 32])
        fw.op("dve", lambda e: e.tensor_copy(out=pw[:, 0, 0, :], in_=P_(ARE)), reads=[par], writes=[pw])
        fw.op("dve", lambda e: e.tensor_copy(out=pw[:, 0, 1, :], in_=P_(AIM)), reads=[par], writes=[pw])
        for k in range(1, NS):
            fw.op("dve", lambda e, k=k: e.tensor_tensor(out=P_(TMP), in0=pw[:, k - 1, 0, :], in1=pw[:, k - 1, 0, :], op=ALU.mult), reads=[pw, par], writes=[par])
            fw.op("dve", lambda e, k=k: e.tensor_tensor(out=P_(T2), in0=pw[:, k - 1, 1, :], in1=pw[:, k - 1, 1, :], op=ALU.mult), reads=[pw, par], writes=[par])
            fw.op("dve", lambda e, k=k: e.tensor_tensor(out=pw[:, k, 0, :], in0=P_(TMP), in1=P_(T2), op=ALU.subtract), reads=[pw, par], writes=[pw])
            fw.op("dve", lambda e, k=k: e.tensor_tensor(out=P_(TMP), in0=pw[:, k - 1, 0, :], in1=pw[:, k - 1, 1, :], op=ALU.mult), reads=[pw, par], writes=[par])
            fw.op("dve", lambda e, k=k: e.tensor_scalar(out=pw[:, k, 1, :], in0=P_(TMP), scalar1=2.0, scalar2=None, op0=ALU.mult), reads=[pw, par], writes=[pw])
        fw.op("dve", lambda e: e.tensor_scalar(out=pw[:, :, 2, :], in0=pw[:, :, 1, :], scalar1=-1.0, scalar2=None, op0=ALU.mult), reads=[pw], writes=[pw])
        braw = fw.sb("s5_braw", [128, 2, 32, 16]); bb = fw.sb("s5_bb", [128, 2, 32, 16]); btmp = fw.sb("s5_btmp", [128, 32, 16])
        for ri, nm in enumerate(("ssm_b_re", "ssm_b_im")):
            fw.dma("sp" if ri == 0 else "act", braw[:, ri, :, :], d[nm].t[l].rearrange("d (gp g2) p k -> (g2 p) (d gp) k", g2=2), reads=[d[nm]], writes=[braw])
        cre_b = P_(CRE).unsqueeze(2).to_broadcast([128, 32, 16]); cim_b = P_(CIM).unsqueeze(2).to_broadcast([128, 32, 16])
        fw.op("dve", lambda e: e.tensor_tensor(out=bb[:, 0, :, :], in0=braw[:, 0, :, :], in1=cre_b, op=ALU.mult), reads=[braw, par], writes=[bb])
        fw.op("dve", lambda e: e.tensor_tensor(out=btmp[:], in0=braw[:, 1, :, :], in1=cim_b, op=ALU.mult), reads=[braw, par], writes=[btmp])
        fw.op("dve", lambda e: e.tensor_tensor(out=bb[:, 0, :, :], in0=bb[:, 0, :, :], in1=btmp[:], op=ALU.subtract), reads=[bb, btmp], writes=[bb])
        fw.op("dve", lambda e: e.tensor_tensor(out=bb[:, 1, :, :], in0=braw[:, 1, :, :], in1=cre_b, op=ALU.mult), reads=[braw, par], writes=[bb])
        fw.op("dve", lambda e: e.tensor_tensor(out=btmp[:], in0=braw[:, 0, :, :], in1=cim_b, op=ALU.mult), reads=[braw, par, bb], writes=[btmp])
        fw.op("dve", lambda e: e.tensor_tensor(out=bb[:, 1, :, :], in0=bb[:, 1, :, :], in1=btmp[:], op=ALU.add), reads=[bb, btmp], writes=[bb])
        cpd = fw.sb("s5_cpd", [32, 2, 128])
        fw.op("pool", lambda e: e.memset(cpd[:], 0.0), writes=[cpd])
        A = [fw.sb("s5_A%d" % i, [128, T]) for i in range(2)]
        Bq = [fw.sb("s5_B%d" % i, [128, T]) for i in range(2)]
        yacc = fw.sb("s5_yacc", [128, T])
        uch = [fw.sb("s5_u%d" % i, [128, 512]) for i in range(2)]
        wB = fw.sb("s5_wB", [128, 2, 128]); lB = fw.sb("s5_lB", [128, 2, 128]); lC = fw.sb("s5_lC", [128, 2, 128])
        dcol = fw.sb("s5_dcol", [128, 4])
        drow = fw.sb("s5_drow", [4, 128])
        fw.dma("sp", drow[:], d["ssm_d"].t[l].rearrange("(c p) -> c p", p=128), reads=[d["ssm_d"]], writes=[drow])
        pbd = self.bank()
        fw.op("pe", lambda e: e.transpose(out=pbd[:, 0:4], in_=drow[:], identity=self.ident[0:4, 0:4]), reads=[drow, self.ident], writes=[pbd])
        fw.op("dve", lambda e: e.tensor_copy(out=dcol[:], in_=pbd[:, 0:4]), reads=[pbd], writes=[dcol])
        zst = [fw.sb("s5_z%d" % i, [128, 512]) for i in range(2)]
        chunks = [(i * 512, 512) for i in range(16)] + [(L, CT)]
        ui = 0
        for ct in range(4):
            first = True
            for dr in range(2):
                for g4 in range(4):
                    gp = ct * 4 + g4
                    gi = dr * 16 + gp
                    fw.op("pool", lambda e: e.memset(wB[:], 0.0), writes=[wB])
                    for ri in range(2):
                        for g2 in range(2):
                            c0 = 32 * g4 + 16 * g2
                            fw.op("dve", lambda e, ri=ri, g2=g2, c0=c0: e.tensor_copy(out=wB[64 * g2:64 * g2 + 64, ri, c0:c0 + 16], in_=bb[64 * g2:64 * g2 + 64, ri, gi, :]),
                                  reads=[bb], writes=[wB])
                    for ri in range(2):
                        pb = self.bank()
                        fw.op("pe", lambda e, ri=ri: e.transpose(out=pb[:, 0:128], in_=wB[:, ri, :], identity=self.ident[:]), reads=[wB, self.ident], writes=[pb])
                        fw.op("act", lambda e, ri=ri: e.activation(out=lB[:, ri, :], in_=pb[:, 0:128], func=AF.Copy), reads=[pb], writes=[lB])
                    fw.op("pool", lambda e: e.memset(lC[:], 0.0), writes=[lC])
                    for ri, nm in enumerate(("ssm_c_re", "ssm_c_im")):
                        for g2 in range(2):
                            fw.dma("sp" if g2 == 0 else "act", cpd[16 * g2:16 * g2 + 16, ri, 64 * g2:64 * g2 + 64], d[nm].t[l, dr, 2 * gp + g2, :, :], reads=[d[nm]], writes=[cpd])
                    for ri in range(2):
                        pb = self.bank()
                        fw.op("pe", lambda e, ri=ri: e.transpose(out=pb[:, 0:32], in_=cpd[:, ri, :], identity=self.ident[0:32, 0:32]), reads=[cpd, self.ident], writes=[pb])
                        fw.op("act", lambda e, ri=ri: e.activation(out=lC[:, ri, 32 * g4:32 * g4 + 32], in_=pb[:, 0:32], func=AF.Copy, scale=(1.0 if ri == 0 else -1.0)),
                              reads=[pb], writes=[lC])
                    for (c0, n) in chunks:
                        ub = uch[ui % 2]; ui += 1
                        fw.dma("sp" if ui % 2 else "act", ub[:, 0:n], s["projT"].t[ct * 128:(ct + 1) * 128, c0:c0 + n], reads=[s["projT"]], writes=[ub])
                        if dr == 0:
                            q0 = c0 + CT if c0 < L else 0
                        else:
                            q0 = c0
                        for ri in range(2):
                            pb = self.bank()
                            fw.op("pe", lambda e, ri=ri: e.matmul(pb[:, 0:n], lB[:, ri, :], ub[:, 0:n], start=True, stop=True), reads=[lB, ub], writes=[pb])
                            fw.op("act", lambda e, ri=ri: e.activation(out=A[ri][:, q0:q0 + n], in_=pb[:, 0:n], func=AF.Copy), reads=[pb], writes=[A[ri]])
                    src, dst = A, Bq
                    for k in range(NS):
                        sft = 1 << k
                        ar = pw[:, k, 0, gi:gi + 1]; ai = pw[:, k, 1, gi:gi + 1]; nai = pw[:, k, 2, gi:gi + 1]
                        if dr == 0:
                            o_sl = slice(sft, T); i_sl = slice(0, T - sft); h_sl = slice(0, sft)
                        else:
                            o_sl = slice(0, T - sft); i_sl = slice(sft, T); h_sl = slice(T - sft, T)
                        for ri in range(2):
                            fw.op("act", lambda e, ri=ri: e.activation(out=dst[ri][:, h_sl], in_=src[ri][:, h_sl], func=AF.Copy), reads=[src[ri]], writes=[dst[ri]])
                        fw.op("dve", lambda e: e.scalar_tensor_tensor(out=dst[0][:, o_sl], in0=src[0][:, i_sl], scalar=ar, in1=src[0][:, o_sl], op0=ALU.mult, op1=ALU.add),
                              reads=[src[0], pw], writes=[dst[0]])
                        fw.op("dve", lambda e: e.scalar_tensor_tensor(out=dst[0][:, o_sl], in0=src[1][:, i_sl], scalar=nai, in1=dst[0][:, o_sl], op0=ALU.mult, op1=ALU.add),
                              reads=[src[1], pw, dst[0]], writes=[dst[0]])
                        fw.op("dve", lambda e: e.scalar_tensor_tensor(out=dst[1][:, o_sl], in0=src[0][:, i_sl], scalar=ai, in1=src[1][:, o_sl], op0=ALU.mult, op1=ALU.add),
                              reads=[src[0], src[1], pw], writes=[dst[1]])
                        fw.op("dve", lambda e: e.scalar_tensor_tensor(out=dst[1][:, o_sl], in0=src[1][:, i_sl], scalar=ar, in1=dst[1][:, o_sl], op0=ALU.mult, op1=ALU.add),
                              reads=[src[1], pw, dst[1]], writes=[dst[1]])
                        src, dst = dst, src
                    hfin = src
                    for (c0, n) in chunks:
                        if dr == 0:
                            q0 = c0 + CT if c0 < L else 0
                        else:
                            q0 = c0
                        pb = self.bank()
                        fw.op("pe", lambda e: e.matmul(pb[:, 0:n], lC[:, 0, :], hfin[0][:, q0:q0 + n], start=True, stop=False), reads=[lC, hfin[0]], writes=[pb])
                        fw.op("pe", lambda e: e.matmul(pb[:, 0:n], lC[:, 1, :], hfin[1][:, q0:q0 + n], start=False, stop=True), reads=[lC, hfin[1]], writes=[pb])
                        if first:
                            fw.op("act", lambda e: e.activation(out=yacc[:, c0:c0 + n], in_=pb[:, 0:n], func=AF.Copy), reads=[pb], writes=[yacc])
                        else:
                            fw.op("pool" if False else "dve", lambda e: e.tensor_tensor(out=yacc[:, c0:c0 + n], in0=pb[:, 0:n], in1=yacc[:, c0:c0 + n], op=ALU.add), reads=[pb, yacc], writes=[yacc])
                    first = False
            for ci, (c0, n) in enumerate(chunks):
                ub = uch[ui % 2]; ui += 1
                fw.dma("sp", ub[:, 0:n], s["projT"].t[ct * 128:(ct + 1) * 128, c0:c0 + n], reads=[s["projT"]], writes=[ub])
                zb = zst[ci % 2]
                fw.op("dve", lambda e: e.scalar_tensor_tensor(out=zb[:, 0:n], in0=ub[:, 0:n], scalar=dcol[:, ct:ct + 1], in1=yacc[:, c0:c0 + n], op0=ALU.mult, op1=ALU.add),
                      reads=[ub, dcol, yacc], writes=[zb])
                fw.op("act", lambda e: e.activation(out=zb[:, 0:n], in_=zb[:, 0:n], func=AF.Gelu_apprx_tanh), reads=[zb], writes=[zb])
                fw.dma("act", s["s5z"].t[ct * 128:(ct + 1) * 128, c0:c0 + n], zb[:, 0:n], reads=[zb], writes=[s["s5z"]])

    def p2a_glu(self, l, es):
        fw, d, s = self.fw, self.d, self.s
        wg = fw.sb("glu_w", [128, 4, 512])
        fw.dma("sp", wg[:], d["ssm_w_glu"].t[l].rearrange("(k p) n -> p k n", p=128), reads=[d["ssm_w_glu"]], writes=[wg])
        zz = [fw.sb("glu_z%d" % i, [128, 4, 512]) for i in range(2)]
        og = [fw.sb("glu_o%d" % i, [128, 512]) for i in range(2)]
        chunks = [(i * 512, 512) for i in range(16)] + [(L, CT)]
        oi = 0
        for ci, (c0, n) in enumerate(chunks):
            zb = zz[ci % 2]
            fw.dma("sp", zb[:, :, 0:n], s["s5z"].t[:, c0:c0 + n].rearrange("(k p) t -> p k t", p=128), reads=[s["s5z"]], writes=[zb])
            for m in range(4):
                pb = self.bank()
                for k in range(4):
                    fw.op("pe", lambda e, k=k: e.matmul(pb[:, 0:n], wg[:, k, m * 128:(m + 1) * 128], zb[:, k, 0:n], start=(k == 0), stop=(k == 3)), reads=[wg, zb], writes=[pb])
                ob = og[oi % 2]; oi += 1
                fw.op("act", lambda e: e.activation(out=ob[:, 0:n], in_=pb[:, 0:n], func=AF.Sigmoid), reads=[pb], writes=[ob])
                fw.op("dve", lambda e: e.tensor_tensor(out=ob[:, 0:n], in0=ob[:, 0:n], in1=zb[:, m, 0:n], op=ALU.mult), reads=[ob, zb], writes=[ob])
                fw.dma("act", s["brT"].t[m * 128:(m + 1) * 128, c0:c0 + n], ob[:, 0:n], reads=[ob], writes=[s["brT"]])

    def p2b_na_norm(self, l, es):
        fw, d, s = self.fw, self.d, self.s
        bones = fw.sb("na_bones", [128, 128])
        fw.op("pool", lambda e: e.memset(bones[:], 0.0), writes=[bones])
        fw.op("pool", lambda e: e.memset(bones[0:64, 0:64], 1.0), writes=[bones])
        fw.op("pool", lambda e: e.memset(bones[64:128, 64:128], 1.0), writes=[bones])
        grow = fw.sb("na_grow", [2, 128]); gcol = fw.sb("na_gcol", [128, 2])
        for qi, nm in enumerate(("na_q_gain", "na_k_gain")):
            for rep in range(2):
                fw.dma("sp", grow[qi:qi + 1, rep * 64:(rep + 1) * 64], d[nm].t[l:l + 1, :], reads=[d[nm]], writes=[grow])
        pb = self.bank()
        fw.op("pe", lambda e: e.transpose(out=pb[:, 0:2], in_=grow[:], identity=self.ident[0:2, 0:2]), reads=[grow, self.ident], writes=[pb])
        fw.op("dve", lambda e: e.tensor_copy(out=gcol[:], in_=pb[:, 0:2]), reads=[pb], writes=[gcol])
        fw.op("dve", lambda e: e.tensor_scalar(out=gcol[:, 0:1], in0=gcol[:, 0:1], scalar1=0.125, scalar2=None, op0=ALU.mult), reads=[gcol], writes=[gcol])
        xq = [fw.sb("na_x%d" % i, [128, 512]) for i in range(2)]
        sq = [fw.sb("na_sq%d" % i, [128, 512]) for i in range(2)]
        rs = [fw.sb("na_rs%d" % i, [128, 512]) for i in range(2)]
        chunks = [(i * 512, 512) for i in range(16)] + [(L, CT)]
        it = 0
        for qi, dst in enumerate(("qnT", "knT")):
            for ct in range(4):
                row0 = W + qi * W + ct * 128
                for (c0, n) in chunks:
                    xb = xq[it % 2]; sb_ = sq[it % 2]; rb = rs[it % 2]; it += 1
                    fw.dma("sp", xb[:, 0:n], s["projT"].t[row0:row0 + 128, c0:c0 + n], reads=[s["projT"]], writes=[xb])
                    fw.op("act", lambda e: e.activation(out=sb_[:, 0:n], in_=xb[:, 0:n], func=AF.Square), reads=[xb], writes=[sb_])
                    pb = self.bank()
                    fw.op("pe", lambda e: e.matmul(pb[:, 0:n], bones[:], sb_[:, 0:n], start=True, stop=True), reads=[bones, sb_], writes=[pb])
                    fw.op("act", lambda e: e.activation(out=rb[:, 0:n], in_=pb[:, 0:n], func=AF.Sqrt, scale=1.0 / 64, bias=EPS), reads=[pb], writes=[rb])
                    fw.op("dve", lambda e: e.reciprocal(out=rb[:, 0:n], in_=rb[:, 0:n]), reads=[rb], writes=[rb])
                    fw.op("dve", lambda e: e.scalar_tensor_tensor(out=rb[:, 0:n], in0=xb[:, 0:n], scalar=gcol[:, qi:qi + 1], in1=rb[:, 0:n], op0=ALU.mult, op1=ALU.mult),
                          reads=[xb, gcol, rb], writes=[rb])
                    fw.dma("act", s[dst].t[ct * 128:(ct + 1) * 128, c0:c0 + n], rb[:, 0:n], reads=[rb], writes=[s[dst]])

    def p2b_na(self, l, es):
        fw, d, s = self.fw, self.d, self.s
        ones = fw.sb("na_ones", [128, 64])
        fw.op("pool", lambda e: e.memset(ones[:], 1.0), writes=[ones])
        tab3 = fw.sb("na_tab3", [128, 8, 4, 64]); tabe = fw.sb("na_tabe", [128, 8, 4, 64])
        fw.dma("sp", tab3[:], d["na_tab"].t[l, 3], reads=[d["na_tab"]], writes=[tab3])
        Kc = fw.sb("na_Kc", [128, 4, CT]); Vc = fw.sb("na_Vc", [128, 2, W])
        fw.dma("sp", Kc[:], s["knT"].t[:, L:AT].rearrange("(hp p) t -> p hp t", p=128), reads=[s["knT"]], writes=[Kc])
        fw.dma("act", Vc[:], s["vtok"].t[L:AT, :].rearrange("(j p) c -> p j c", p=128), reads=[s["vtok"]], writes=[Vc])
        Kw = [fw.sb("na_Kw%d" % i, [128, 4, 512]) for i in range(2)]
        Vw = [fw.sb("na_Vw%d" % i, [128, 4, W]) for i in range(2)]
        Qr = [fw.sb("na_Qr%d" % i, [128, 4, 64]) for i in range(2)]
        Pm = [fw.sb("na_P%d" % i, [128, 6, 64]) for i in range(2)]
        orow = [fw.sb("na_o%d" % i, [64, 8, 64]) for i in range(2)]
        rden = [fw.sb("na_rd%d" % i, [64, 64]) for i in range(2)]
        hi = 0
        for r in range(128):
            start = min(max(r - 4, 0), 120)
            dr0 = start - r + 7
            kb = Kw[r % 2]; vb = Vw[r % 2]; qb = Qr[r % 2]; ob = orow[r % 2]
            fw.dma("sp", kb[:], s["knT"].t[:, 64 * start:64 * start + 512].rearrange("(hp p) t -> p hp t", p=128), reads=[s["knT"]], writes=[kb])
            fw.dma("act", vb[:], s["vtok"].t[64 * start:64 * start + 512, :].rearrange("(j p) c -> p j c", p=128), reads=[s["vtok"]], writes=[vb])
            fw.dma("sp", qb[:], s["qnT"].t[:, 64 * r:64 * r + 64].rearrange("(hp p) t -> p hp t", p=128), reads=[s["qnT"]], writes=[qb])
            if dr0 == 3:
                tab = tab3
            else:
                tab = tabe
                fw.dma("act", tabe[:], d["na_tab"].t[l, dr0], reads=[d["na_tab"]], writes=[tabe])
            for h in range(8):
                hp, b = h // 2, 64 * (h % 2)
                pm = Pm[hi % 2]; rd = rden[hi % 2]; hi += 1
                ps = self.bank()
                for j in range(4):
                    fw.op("pe", lambda e, j=j: e.matmul(ps[:, j * 64:(j + 1) * 64], kb[b:b + 64, hp, j * 128:(j + 1) * 128], qb[b:b + 64, hp, :], start=True, stop=True),
                          reads=[kb, qb], writes=[ps])
                for j in range(2):
                    fw.op("pe", lambda e, j=j: e.matmul(ps[:, 256 + j * 64:256 + (j + 1) * 64], Kc[b:b + 64, hp, j * 128:(j + 1) * 128], qb[b:b + 64, hp, :], start=True, stop=True),
                          reads=[Kc, qb], writes=[ps])
                fw.op("dve", lambda e: e.tensor_tensor(out=pm[:, 0:4, :], in0=ps[:, 0:256].rearrange("p (j q) -> p j q", j=4), in1=tab[:, h, :, :], op=ALU.add),
                      reads=[ps, tab], writes=[pm])
                fw.op("act", lambda e: e.activation(out=pm[:, 0:4, :], in_=pm[:, 0:4, :], func=AF.Exp), reads=[pm], writes=[pm])
                fw.op("act", lambda e: e.activation(out=pm[:, 4:6, :], in_=ps[:, 256:384].rearrange("p (j q) -> p j q", j=2), func=AF.Exp), reads=[ps], writes=[pm])
                po = self.bank(); pd = self.bank()
                for j in range(6):
                    vsrc = vb[:, j, 64 * h:64 * h + 64] if j < 4 else Vc[:, j - 4, 64 * h:64 * h + 64]
                    fw.op("pe", lambda e, j=j, vsrc=vsrc: e.matmul(po[0:64, 0:64], vsrc, pm[:, j, :], start=(j == 0), stop=(j == 5)), reads=[vb, Vc, pm], writes=[po])
                for j in range(6):
                    fw.op("pe", lambda e, j=j: e.matmul(pd[0:64, 0:64], ones[:], pm[:, j, :], start=(j == 0), stop=(j == 5)), reads=[ones, pm], writes=[pd])
                fw.op("dve", lambda e: e.reciprocal(out=rd[:], in_=pd[0:64, 0:64]), reads=[pd], writes=[rd])
                fw.op("dve", lambda e: e.tensor_tensor(out=ob[:, h, :], in0=po[0:64, 0:64], in1=rd[:], op=ALU.mult), reads=[po, rd], writes=[ob])
            fw.dma("sp", s["brT"].t[W:2 * W, 64 * r:64 * r + 64].rearrange("(h d) q -> d h q", d=64), ob[:], reads=[ob], writes=[s["brT"]])
        if l == 0:
            Qc = fw.sb("na_Qc", [128, 4, CT]); Pc = fw.sb("na_Pc", [128, 2, CT]); oc = fw.sb("na_oc", [64, 8, CT]); rdc = fw.sb("na_rdc", [64, CT])
            fw.dma("sp", Qc[:], s["qnT"].t[:, L:AT].rearrange("(hp p) t -> p hp t", p=128), reads=[s["qnT"]], writes=[Qc])
            for h in range(8):
                hp, b = h // 2, 64 * (h % 2)
                ps = self.bank()
                for j in range(2):
                    fw.op("pe", lambda e, j=j: e.matmul(ps[:, j * CT:(j + 1) * CT], Kc[b:b + 64, hp, j * 128:(j + 1) * 128], Qc[b:b + 64, hp, :], start=True, stop=True),
                          reads=[Kc, Qc], writes=[ps])
                fw.op("act", lambda e: e.activation(out=Pc[:], in_=ps[:].rearrange("p (j q) -> p j q", j=2), func=AF.Exp), reads=[ps], writes=[Pc])
                po = self.bank(); pd = self.bank()
                for j in range(2):
                    fw.op("pe", lambda e, j=j: e.matmul(po[0:64, 0:CT], Vc[:, j, 64 * h:64 * h + 64], Pc[:, j, :], start=(j == 0), stop=(j == 1)), reads=[Vc, Pc], writes=[po])
                for j in range(2):
                    fw.op("pe", lambda e, j=j: e.matmul(pd[0:64, 0:CT], ones[:], Pc[:, j, :], start=(j == 0), stop=(j == 1)), reads=[ones, Pc], writes=[pd])
                fw.op("dve", lambda e: e.reciprocal(out=rdc[:], in_=pd[0:64, 0:CT]), reads=[pd], writes=[rdc])
                fw.op("dve", lambda e: e.tensor_tensor(out=oc[:, h, :], in0=po[0:64, 0:CT], in1=rdc[:], op=ALU.mult), reads=[po, rdc], writes=[oc])
            fw.dma("sp", s["brT"].t[W:2 * W, L:AT].rearrange("(h d) q -> d h q", d=64), oc[:], reads=[oc], writes=[s["brT"]])

    def p2c_hy_short(self, l, es):
        fw, d, s = self.fw, self.d, self.s
        wrow = fw.sb("hs_wrow", [4, 1536]); wcol = fw.sb("hs_wcol", [128, 12, 4])
        fw.dma("sp", wrow[0:3, :], d["hy_conv_w"].t[l], reads=[d["hy_conv_w"]], writes=[wrow])
        fw.dma("sp", wrow[3:4, :], d["hy_conv_b"].t[l:l + 1, :], reads=[d["hy_conv_b"]], writes=[wrow])
        for ct in range(12):
            pb = self.bank()
            fw.op("pe", lambda e: e.transpose(out=pb[:, 0:4], in_=wrow[:, ct * 128:(ct + 1) * 128], identity=self.ident[0:4, 0:4]), reads=[wrow, self.ident], writes=[pb])
            fw.op("dve", lambda e: e.tensor_copy(out=wcol[:, ct, :], in_=pb[:, 0:4]), reads=[pb], writes=[wcol])
        ub = [fw.sb("hs_u%d" % i, [128, L]) for i in range(2)]
        zb = [fw.sb("hs_z%d" % i, [128, L]) for i in range(2)]
        it = 0
        for ct in range(12):
            for (c0, n) in ((0, L), (L, CT)):
                u = ub[it % 2]; z = zb[it % 2]; it += 1
                fw.dma("sp", u[:, 0:n], s["projT"].t[2048 + ct * 128:2048 + (ct + 1) * 128, c0:c0 + n], reads=[s["projT"]], writes=[u])
                fw.op("dve", lambda e: e.tensor_scalar(out=z[:, 0:n], in0=u[:, 0:n], scalar1=wcol[:, ct, 1:2], scalar2=wcol[:, ct, 3:4], op0=ALU.mult, op1=ALU.add),
                      reads=[u, wcol], writes=[z])
                fw.op("dve", lambda e: e.scalar_tensor_tensor(out=z[:, 1:n], in0=u[:, 0:n - 1], scalar=wcol[:, ct, 0:1], in1=z[:, 1:n], op0=ALU.mult, op1=ALU.add),
                      reads=[u, wcol, z], writes=[z])
                fw.op("dve", lambda e: e.scalar_tensor_tensor(out=z[:, 0:n - 1], in0=u[:, 1:n], scalar=wcol[:, ct, 2:3], in1=z[:, 0:n - 1], op0=ALU.mult, op1=ALU.add),
                      reads=[u, wcol, z], writes=[z])
                fw.dma("act", s["hzT"].t[ct * 128:(ct + 1) * 128, c0:c0 + n], z[:, 0:n], reads=[z], writes=[s["hzT"]])

    def p2c_hy_filt(self, l, es, seg):
        fw, d, s = self.fw, self.d, self.s
        Lx, zname, dname = (L, "hyc_zL", "hyc_dL") if seg == 0 else (CT, "hyc_zc", "hyc_dc")
        CH = min(512, Lx); nch = Lx // CH
        hfT = s["hfT"] if seg == 0 else s["hfTc"]
        w1 = fw.sb("hf_w1", [33, 64]); w2 = fw.sb("hf_w2", [64, 64]); w3 = fw.sb("hf_w3", [64, 64]); w4 = fw.sb("hf_w4", [64, 2048])
        fw.dma("sp", w1[:], d["hy_w1"].t[l], reads=[d["hy_w1"]], writes=[w1]); fw.dma("sp", w2[:], d["hy_w2"].t[l], reads=[d["hy_w2"]], writes=[w2])
        fw.dma("sp", w3[:], d["hy_w3"].t[l], reads=[d["hy_w3"]], writes=[w3]); fw.dma("act", w4[:], d["hy_w4"].t[l], reads=[d["hy_w4"]], writes=[w4])
        brow = fw.sb("hf_brow", [4, 64]); bcol = fw.sb("hf_bcol", [64, 4])
        for i, nm in enumerate(("hy_b1", "hy_b2", "hy_b3", "hy_freq")):
            fw.dma("sp", brow[i:i + 1, :], d[nm].t[l:l + 1, :], reads=[d[nm]], writes=[brow])
        pb = self.bank()
        fw.op("pe", lambda e: e.transpose(out=pb[0:64, 0:4], in_=brow[:], identity=self.ident[0:4, 0:4]), reads=[brow, self.ident], writes=[pb])
        fw.op("dve", lambda e: e.tensor_copy(out=bcol[:], in_=pb[0:64, 0:4]), reads=[pb], writes=[bcol])
        zp = fw.sb("hf_zp", [33, Lx])
        fw.dma("sp", zp[:], d[zname].t, reads=[d[zname]], writes=[zp])
        h3 = fw.sb("hf_h3", [64, Lx])
        ha = fw.sb("hf_ha", [64, CH]); hb_ = fw.sb("hf_hb", [64, CH]); ti = fw.sb("hf_ti", [64, CH], I32); tf = fw.sb("hf_tf", [64, CH])
        for ch in range(nch):
            c0 = ch * CH
            cur_in = zp[:, c0:c0 + CH]; cur_buf = zp
            for li, (wt, kdim) in enumerate(((w1, 33), (w2, 64), (w3, 64))):
                pb = self.bank()
                fw.op("pe", lambda e: e.matmul(pb[0:64, 0:CH], wt[0:kdim, :], cur_in, start=True, stop=True), reads=[wt, cur_buf], writes=[pb])
                fw.op("dve", lambda e: e.tensor_scalar(out=ha[:], in0=pb[0:64, 0:CH], scalar1=bcol[:, li:li + 1], scalar2=bcol[:, 3:4], op0=ALU.add, op1=ALU.mult),
                      reads=[pb, bcol], writes=[ha])
                outap = (hb_[:] if li < 2 else h3[:, c0:c0 + CH]); outbuf = hb_ if li < 2 else h3
                self.sin_reduced(outap, ha[:], ti[:], tf[:], [ha], [outbuf, ti, tf])
                cur_in = hb_[:]; cur_buf = hb_
        dec = [fw.sb("hf_dec%d" % i, [128, CH]) for i in range(2)]
        hf = [fw.sb("hf_hf%d" % i, [128, CH]) for i in range(2)]
        junk = fw.sb("hf_junk", [128, CH])
        acc = fw.sb("hf_acc", [128, 2 * nch + 2]); nr = fw.sb("hf_nr", [128, 2])
        it = 0
        for o in range(2):
            for cti in range(4):
                for dr in range(2):
                    m0 = o * 1024 + dr * 512 + cti * 128
                    for ch in range(nch):
                        c0 = ch * CH
                        db = dec[it % 2]; hb2 = hf[it % 2]; it += 1
                        fw.dma("sp", db[:], d[dname].t[cti * 128:(cti + 1) * 128, c0:c0 + CH], reads=[d[dname]], writes=[db])
                        pb = self.bank()
                        fw.op("pe", lambda e: e.matmul(pb[:, 0:CH], w4[:, m0:m0 + 128], h3[:, c0:c0 + CH], start=True, stop=True), reads=[w4, h3], writes=[pb])
                        fw.op("dve", lambda e: e.tensor_tensor(out=hb2[:], in0=pb[:, 0:CH], in1=db[:], op=ALU.mult), reads=[pb, db], writes=[hb2])
                        if dr == 1 and ch == 0:
                            fw.op("dve", lambda e: e.memset(hb2[:, 0:1], 0.0), reads=[hb2], writes=[hb2])
                        fw.op("act", lambda e: e.activation(out=junk[:], in_=hb2[:], func=AF.Abs, accum_out=acc[:, dr * nch + ch:dr * nch + ch + 1]), reads=[hb2], writes=[junk, acc])
                        fw.dma("act", hfT.t[m0:m0 + 128, c0:c0 + CH], hb2[:], reads=[hb2], writes=[hfT])
                fw.op("dve", lambda e: e.tensor_reduce(out=nr[:, 0:1], in_=acc[:, 0:2 * nch], axis=AX.X, op=ALU.add), reads=[acc], writes=[nr])
                fw.op("dve", lambda e: e.reciprocal(out=nr[:, 1:2], in_=nr[:, 0:1]), reads=[nr], writes=[nr])
                fw.dma("sp", s["hnrm"].t[seg, o, cti * 128:(cti + 1) * 128].rearrange("(p a) -> p a", a=1), nr[:, 1:2], reads=[nr], writes=[s["hnrm"]])

    def hy_setup(self):
        fw, d = self.fw, self.d
        F3 = fw.sb("hy_F3", [128, 3, 128]); T2 = fw.sb("hy_T2", [128, 2, 128])
        fw.dma("sp", F3[:], d["hyc_F"].t, reads=[d["hyc_F"]], writes=[F3]); fw.dma("act", T2[:], d["hyc_T"].t, reads=[d["hyc_T"]], writes=[T2])
        FF = fw.sb("hy_FF", [128, 256]); FiFr = fw.sb("hy_FiFr", [128, 256]); FrnFi = fw.sb("hy_FrnFi", [128, 256])
        cp = lambda dst, src: fw.op("dve", lambda e: e.tensor_copy(out=dst, in_=src), reads=[F3], writes=[FF, FiFr, FrnFi])
        cp(FF[:, 0:128], F3[:, 0, :]); cp(FF[:, 128:256], F3[:, 1, :])
        cp(FiFr[:, 0:128], F3[:, 1, :]); cp(FiFr[:, 128:256], F3[:, 0, :])
        cp(FrnFi[:, 0:128], F3[:, 0, :]); cp(FrnFi[:, 128:256], F3[:, 2, :])
        self.hy = dict(F3=F3, T2=T2, FF=FF, FiFr=FiFr, FrnFi=FrnFi)
        self.hy_tmp = [fw.sb("hy_tmp%d" % i, [128, 512]) for i in range(2)]
        self.hy_B = [fw.sb("hy_B%d" % i, [128, 2, 512]) for i in range(2)]
        self.hy_bi = 0

    def hy_cmul(self, out, a_re, a_im, a_bufs, b_re, b_im, b_bufs, conj=False):
        fw = self.fw
        tmp = self.hy_tmp[0]
        o_re, o_im = out[:, 0, :], out[:, 1, :]
        fw.op("dve", lambda e: e.tensor_tensor(out=o_re, in0=a_re, in1=b_re, op=ALU.mult), reads=a_bufs + b_bufs, writes=[out])
        fw.op("dve", lambda e: e.tensor_tensor(out=tmp[:], in0=a_im, in1=b_im, op=ALU.mult), reads=a_bufs + b_bufs, writes=[tmp])
        fw.op("dve", lambda e: e.tensor_tensor(out=o_re, in0=o_re, in1=tmp[:], op=(ALU.add if conj else ALU.subtract)), reads=[out, tmp], writes=[out])
        fw.op("dve", lambda e: e.tensor_tensor(out=o_im, in0=a_re, in1=b_im, op=ALU.mult), reads=a_bufs + b_bufs, writes=[out])
        fw.op("dve", lambda e: e.tensor_tensor(out=tmp[:], in0=a_im, in1=b_re, op=ALU.mult), reads=a_bufs + b_bufs + [tmp], writes=[tmp])
        if conj:
            fw.op("dve", lambda e: e.tensor_tensor(out=o_im, in0=tmp[:], in1=o_im, op=ALU.subtract), reads=[out, tmp], writes=[out])
        else:
            fw.op("dve", lambda e: e.tensor_tensor(out=o_im, in0=o_im, in1=tmp[:], op=ALU.add), reads=[out, tmp], writes=[out])

    def hy_fft(self, X, nrow):
        fw, H = self.fw, self.hy
        p1 = [self.bank(), self.bank()]
        for c in range(4):
            fw.op("pe", lambda e, c=c: e.matmul(p1[c // 2][:, (c % 2) * 256:(c % 2) * 256 + 256], X[0:nrow, c, :], H["FF"][0:nrow, :], start=True, stop=True),
                  reads=[X, H["FF"]], writes=[p1[c // 2]])
        Bt = self.hy_B[self.hy_bi % 2]; self.hy_bi += 1
        twr = H["T2"][:, 0, :].unsqueeze(1).to_broadcast([128, 2, 128]); twi = H["T2"][:, 1, :].unsqueeze(1).to_broadcast([128, 2, 128])
        for half in range(2):
            pv = p1[half][:].rearrange("p (c r k) -> p c r k", c=2, r=2)
            ov = Bt[:, :, half * 256:(half + 1) * 256].rearrange("p r (c k) -> p r c k", c=2)
            tmp = self.hy_tmp[0]
            tv = tmp[:, 0:256].rearrange("p (c k) -> p c k", c=2)
            a_re, a_im = pv[:, :, 0, :], pv[:, :, 1, :]
            fw.op("dve", lambda e: e.tensor_tensor(out=ov[:, 0], in0=a_re, in1=twr, op=ALU.mult), reads=[p1[half], H["T2"]], writes=[Bt])
            fw.op("dve", lambda e: e.tensor_tensor(out=tv, in0=a_im, in1=twi, op=ALU.mult), reads=[p1[half], H["T2"]], writes=[tmp])
            fw.op("dve", lambda e: e.tensor_tensor(out=ov[:, 0], in0=ov[:, 0], in1=tv, op=ALU.subtract), reads=[Bt, tmp], writes=[Bt])
            fw.op("dve", lambda e: e.tensor_tensor(out=ov[:, 1], in0=a_re, in1=twi, op=ALU.mult), reads=[p1[half], H["T2"]], writes=[Bt])
            fw.op("dve", lambda e: e.tensor_tensor(out=tv, in0=a_im, in1=twr, op=ALU.mult), reads=[p1[half], H["T2"], tmp], writes=[tmp])
            fw.op("dve", lambda e: e.tensor_tensor(out=ov[:, 1], in0=ov[:, 1], in1=tv, op=ALU.add), reads=[Bt, tmp], writes=[Bt])
        pr, pi = self.bank(), self.bank()
        Fr, Fi, nFi = H["F3"][:, 0, :], H["F3"][:, 1, :], H["F3"][:, 2, :]
        fw.op("pe", lambda e: e.matmul(pr[:], Fr, Bt[:, 0, :], start=True, stop=False), reads=[H["F3"], Bt], writes=[pr])
        fw.op("pe", lambda e: e.matmul(pr[:], nFi, Bt[:, 1, :], start=False, stop=True), reads=[H["F3"], Bt], writes=[pr])
        fw.op("pe", lambda e: e.matmul(pi[:], Fr, Bt[:, 1, :], start=True, stop=False), reads=[H["F3"], Bt], writes=[pi])
        fw.op("pe", lambda e: e.matmul(pi[:], Fi, Bt[:, 0, :], start=False, stop=True), reads=[H["F3"], Bt], writes=[pi])
        return pr, pi

    def hy_ifft(self, Yh, nrow):
        fw, H = self.fw, self.hy
        p1 = [self.bank(), self.bank()]
        for c in range(4):
            dst = p1[c // 2][:, (c % 2) * 256:(c % 2) * 256 + 256]
            fw.op("pe", lambda e, c=c: e.matmul(dst, Yh[:, 0, c * 128:(c + 1) * 128], H["FrnFi"][:], start=True, stop=False), reads=[Yh, H["FrnFi"]], writes=[p1[c // 2]])
            fw.op("pe", lambda e, c=c: e.matmul(dst, Yh[:, 1, c * 128:(c + 1) * 128], H["FiFr"][:], start=False, stop=True), reads=[Yh, H["FiFr"]], writes=[p1[c // 2]])
        Et = self.hy_B[self.hy_bi % 2]; self.hy_bi += 1
        twr = H["T2"][:, 0, :].unsqueeze(1).to_broadcast([128, 2, 128]); twi = H["T2"][:, 1, :].unsqueeze(1).to_broadcast([128, 2, 128])
        for half in range(2):
            pv = p1[half][:].rearrange("p (c r k) -> p c r k", c=2, r=2)
            ov = Et[:, :, half * 256:(half + 1) * 256].rearrange("p r (c k) -> p r c k", c=2)
            tmp = self.hy_tmp[0]
            tv = tmp[:, 0:256].rearrange("p (c k) -> p c k", c=2)
            a_re, a_im = pv[:, :, 0, :], pv[:, :, 1, :]
            fw.op("dve", lambda e: e.tensor_tensor(out=ov[:, 0], in0=a_re, in1=twr, op=ALU.mult), reads=[p1[half], H["T2"]], writes=[Et])
            fw.op("dve", lambda e: e.tensor_tensor(out=tv, in0=a_im, in1=twi, op=ALU.mult), reads=[p1[half], H["T2"]], writes=[tmp])
            fw.op("dve", lambda e: e.tensor_tensor(out=ov[:, 0], in0=ov[:, 0], in1=tv, op=ALU.add), reads=[Et, tmp], writes=[Et])
            fw.op("dve", lambda e: e.tensor_tensor(out=ov[:, 1], in0=a_im, in1=twr, op=ALU.mult), reads=[p1[half], H["T2"]], writes=[Et])
            fw.op("dve", lambda e: e.tensor_tensor(out=tv, in0=a_re, in1=twi, op=ALU.mult), reads=[p1[half], H["T2"], tmp], writes=[tmp])
            fw.op("dve", lambda e: e.tensor_tensor(out=ov[:, 1], in0=ov[:, 1], in1=tv, op=ALU.subtract), reads=[Et, tmp], writes=[Et])
        po = self.bank()
        Fr, Fi = H["F3"][:, 0, 0:nrow], H["F3"][:, 1, 0:nrow]
        fw.op("pe", lambda e: e.matmul(po[0:nrow, :], Fr, Et[:, 0, :], start=True, stop=False), reads=[H["F3"], Et], writes=[po])
        fw.op("pe", lambda e: e.matmul(po[0:nrow, :], Fi, Et[:, 1, :], start=False, stop=True), reads=[H["F3"], Et], writes=[po])
        return po

    def p2c_hy_spec(self, l, es, seg):
        fw, d, s = self.fw, self.d, self.s
        self.hy_setup()
        Lx, nrow = (L, 64) if seg == 0 else (CT, 2)
        hfT = s["hfT"] if seg == 0 else s["hfTc"]
        spec = s["hspec"] if seg == 0 else s["hspecc"]
        rn = fw.sb("hp_rn", [128, 2, 512])
        for o in range(2):
            self.load_bcast("sp", rn, s["hnrm"], (seg * 2 + o) * 512, 512, ap=rn[:, o, :])
        Xf = [fw.sb("hp_Xf%d" % i, [64, 4, 128]) for i in range(2)]; Xb = [fw.sb("hp_Xb%d" % i, [64, 4, 128]) for i in range(2)]
        Sf = [fw.sb("hp_Sf%d" % i, [128, 2, 512]) for i in range(2)]; Hs = [fw.sb("hp_H%d" % i, [128, 2, 512]) for i in range(2)]
        it = 0
        for o in range(2):
            for g in range(128):
                c0 = g * 4
                xf = Xf[it % 2]; xb = Xb[it % 2]; sf = Sf[it % 2]; hs = Hs[it % 2]; it += 1
                r0 = o * 1024 + c0
                fw.dma("sp", xf[0:nrow], hfT.t[r0:r0 + 4, :].rearrange("c (a b) -> a c b", b=128), reads=[hfT], writes=[xf])
                fw.dma("act", xb[0:nrow], hfT.t[r0 + 512:r0 + 516, :].rearrange("c (a b) -> a c b", b=128), reads=[hfT], writes=[xb])
                pr, pi = self.hy_fft(xf, nrow)
                fw.op("act", lambda e: e.activation(out=sf[:, 0, :], in_=pr[:], func=AF.Copy), reads=[pr], writes=[sf])
                fw.op("act", lambda e: e.activation(out=sf[:, 1, :], in_=pi[:], func=AF.Copy), reads=[pi], writes=[sf])
                pr2, pi2 = self.hy_fft(xb, nrow)
                rnb = rn[:, o, c0:c0 + 4].unsqueeze(2).to_broadcast([128, 4, 128])
                v4 = lambda ap: ap.rearrange("p (c k) -> p c k", c=4)
                fw.op("dve", lambda e: e.tensor_tensor(out=hs[:, 0, :], in0=pr2[:], in1=sf[:, 0, :], op=ALU.add), reads=[pr2, sf], writes=[hs])
                fw.op("dve", lambda e: e.tensor_tensor(out=hs[:, 1, :], in0=sf[:, 1, :], in1=pi2[:], op=ALU.subtract), reads=[pi2, sf], writes=[hs])
                fw.op("dve", lambda e: e.tensor_tensor(out=v4(hs[:, 0, :]), in0=v4(hs[:, 0, :]), in1=rnb, op=ALU.mult), reads=[hs, rn], writes=[hs])
                fw.op("dve", lambda e: e.tensor_tensor(out=v4(hs[:, 1, :]), in0=v4(hs[:, 1, :]), in1=rnb, op=ALU.mult), reads=[hs, rn], writes=[hs])
                fw.dma("sp", spec.t[o, g], hs[:], reads=[hs], writes=[spec])

    def p2c_hy_conv(self, l, es, seg):
        fw, d, s = self.fw, self.d, self.s
        self.hy_setup()
        Lx, nrow, col0 = (L, 64, 0) if seg == 0 else (CT, 2, L)
        spec = s["hspec"] if seg == 0 else s["hspecc"]
        bias = fw.sb("hc_bias", [64, 2, 512])
        for o in range(2):
            self.load_bcast("sp", bias, d["hy_bias"], (l * 2 + o) * 512, 512, ap=bias[:, o, :])
        Y0 = [fw.sb("hc_Y0_%d" % i, [64, 4, 128]) for i in range(2)]; P0 = [fw.sb("hc_P0_%d" % i, [64, 4, 128]) for i in range(2)]
        P1 = [fw.sb("hc_P1_%d" % i, [64, 4, 128]) for i in range(2)]; Y1 = [fw.sb("hc_Y1_%d" % i, [64, 4, 128]) for i in range(2)]
        Hs = [fw.sb("hc_H%d" % i, [128, 2, 512]) for i in range(4)]; Yh = [fw.sb("hc_Yh%d" % i, [128, 2, 512]) for i in range(2)]
        invn = 1.0 / 16384.0
        f3 = lambda ap: ap.rearrange("p c k -> p (c k)")
        for g in range(128):
            c0 = g * 4
            y0 = Y0[g % 2]; p0 = P0[g % 2]; p1 = P1[g % 2]; y1 = Y1[g % 2]
            ld = lambda q, dst, row: fw.dma(q, dst[0:nrow], s["hzT"].t[row:row + 4, col0:col0 + Lx].rearrange("c (a b) -> a c b", b=128), reads=[s["hzT"]], writes=[dst])
            ld("sp", y0, 1024 + c0); ld("act", p0, c0); ld("sp", p1, 512 + c0)
            cur = y0
            for o in range(2):
                hs = Hs[(2 * g + o) % 4]; yh = Yh[o]
                fw.dma("act", hs[:], spec.t[o, g], reads=[spec], writes=[hs])
                pr, pi = self.hy_fft(cur, nrow)
                self.hy_cmul(yh, pr[:], pi[:], [pr, pi], hs[:, 0, :], hs[:, 1, :], [hs])
                po = self.hy_ifft(yh, nrow)
                part = p0 if o == 0 else p1
                dst = y1
                bb_ = bias[0:nrow, o, c0:c0 + 4].unsqueeze(2).to_broadcast([nrow, 4, 128])
                tmpb = self.hy_tmp[1]
                tv = tmpb[0:nrow, :].rearrange("p (c k) -> p c k", c=4)
                fw.op("dve", lambda e: e.tensor_tensor(out=tv, in0=cur[0:nrow], in1=bb_, op=ALU.mult), reads=[cur, bias], writes=[tmpb])
                fw.op("dve", lambda e: e.scalar_tensor_tensor(out=tmpb[0:nrow, :], in0=po[0:nrow, :], scalar=invn, in1=tmpb[0:nrow, :], op0=ALU.mult, op1=ALU.add),
                      reads=[po, tmpb], writes=[tmpb])
                fw.op("dve", lambda e: e.tensor_tensor(out=f3(dst[0:nrow]), in0=tmpb[0:nrow, :], in1=f3(part[0:nrow]), op=ALU.mult), reads=[tmpb, part], writes=[dst])
                cur = y1
            fw.dma("sp", s["brT"].t[2 * W + c0:2 * W + c0 + 4, col0:col0 + Lx].rearrange("c (a b) -> a c b", b=128), y1[0:nrow], reads=[y1], writes=[s["brT"]])

    def p3_merge(self, l, es, xin):
        fw, d, s = self.fw, self.d, self.s
        TB = 256
        hTb = fw.sb("m_hTb", [128, KT, TB])
        brb = [fw.sb("m_brb%d" % i, [128, 4, TB]) for i in range(3)]
        wg = [fw.sb("m_wg%d" % i, [128, KT, 512]) for i in range(2)]
        wbr = [fw.sb("m_wbr%d" % i, [128, 4, 512]) for i in range(2)]
        bg = [fw.sb("m_bg%d" % i, [128, 512]) for i in range(2)]
        mixed = [fw.sb("m_mix%d" % i, [128, D]) for i in range(2)]
        gate = [fw.sb("m_gate%d" % i, [128, 512]) for i in range(2)]
        mT = fw.sb("m_mT", [128, KT, 128])
        g1 = [fw.sb("m_g1_%d" % r, [128, D]) for r in range(2)]
        xt = fw.sb("m_xt", [128, D]); ot = fw.sb("m_ot", [128, D])
        for r in range(2):
            self.load_bcast("sp", g1[r], s["modD"], r * 6 * D + 2 * D, D, "small")
        wi = 0
        nblk = AT // TB
        if l == 0:
            self.dump("hTd0", s["hT"], s["hT"].t[:, 0, 0:128])
            self.dump("hTd15", s["hT"], s["hT"].t[:, 15, 0:128])
            self.dump("hTd0b", s["hT"], s["hT"].t[:, 0, 640:768])
        for blk in range(nblk):
            t0 = blk * TB
            r = 0 if t0 < L else 1
            if r == 1 and l == DEPTH - 1:
                continue
            fw.dma("sp", hTb[:], s["hT"].t[:, :, t0:t0 + TB], reads=[s["hT"]], writes=[hTb], key="m_ld")
            for i in range(3):
                fw.dma("act", brb[i][:], s["brT"].t[i * W:(i + 1) * W, t0:t0 + TB].rearrange("(k p) t -> p k t", p=128),
                       reads=[s["brT"]], writes=[brb[i]], key="m_ld")
            for i in range(3):
                for cc in range(4):
                    wgb = wg[wi % 2]; wbb = wbr[wi % 2]; bgb = bg[wi % 2]; wi += 1
                    c0 = i * D + cc * 512
                    fw.dma("sp", wgb[:], d["w_gate"].t[l, :, c0:c0 + 512].rearrange("(kt p) n -> p kt n", p=128),
                           reads=[d["w_gate"]], writes=[wgb], key="m_wg%d" % (wi % 2))
                    fw.dma("act", wbb[:], d["w_branch"].t[l, i, :, cc * 512:(cc + 1) * 512].rearrange("(k p) n -> p k n", p=128),
                           reads=[d["w_branch"]], writes=[wbb], key="m_wb%d" % (wi % 2))
                    self.load_bcast("pool", bgb, d["b_gate"], l * 3 * D + c0, 512, "m_bg%d" % (wi % 2))
                    for ti in range(TB // 128):
                        pg = self.bank()
                        for kt in range(KT):
                            fw.op("pe", lambda e, kt=kt: e.matmul(pg[:], hTb[:, kt, ti * 128:(ti + 1) * 128], wgb[:, kt, :], start=(kt == 0), stop=(kt == KT - 1)),
                                  reads=[hTb, wgb], writes=[pg])
                        pbp = self.bank()
                        for k4 in range(4):
                            fw.op("pe", lambda e, k4=k4: e.matmul(pbp[:], brb[i][:, k4, ti * 128:(ti + 1) * 128], wbb[:, k4, :], start=(k4 == 0), stop=(k4 == 3)),
                                  reads=[brb[i], wbb], writes=[pbp])
                        gt = gate[ti % 2]
                        fw.op("dve", lambda e: e.tensor_tensor(out=gt[:], in0=pg[:], in1=bgb[:], op=ALU.add), reads=[pg, bgb], writes=[gt])
                        fw.op("act", lambda e: e.activation(out=gt[:], in_=gt[:], func=AF.Sigmoid), reads=[gt], writes=[gt])
                        if blk == 0 and ti == 0 and i == 0 and cc == 0 and l == 0:
                            self.dump("gate00", gt, gt[:])
                            self.dump("hTb0", hTb, hTb[:, 0, 0:128])
                            self.dump("hTb15", hTb, hTb[:, 15, 0:128])
                            self.dump("wgb0", wgb, wgb[:, 0, :])
                            self.dump("bgb", bgb, bgb[:])
                        mx = mixed[ti]
                        if i == 0:
                            fw.op("dve", lambda e: e.tensor_tensor(out=mx[:, cc * 512:(cc + 1) * 512], in0=pbp[:], in1=gt[:], op=ALU.mult),
                                  reads=[pbp, gt], writes=[mx])
                        else:
                            fw.op("dve", lambda e: e.tensor_tensor(out=gt[:], in0=pbp[:], in1=gt[:], op=ALU.mult), reads=[pbp, gt], writes=[gt])
                            fw.op("pool", lambda e: e.tensor_tensor(out=mx[:, cc * 512:(cc + 1) * 512], in0=mx[:, cc * 512:(cc + 1) * 512], in1=gt[:], op=ALU.add),
                                  reads=[mx, gt], writes=[mx])
            for ti in range(TB // 128):
                mx = mixed[ti]
                if blk == 0 and ti == 0 and l == 0:
                    self.dump("mixed0", mx, mx[:])
                for q4 in range(4):
                    pb = self.bank()
                    for j in range(4):
                        kt = q4 * 4 + j
                        fw.op("pe", lambda e, kt=kt, j=j: e.transpose(out=pb[:, j * 128:(j + 1) * 128], in_=mx[:, kt * 128:(kt + 1) * 128], identity=self.ident[:]),
                              reads=[mx, self.ident], writes=[pb])
                    fw.op("act", lambda e: e.activation(out=mT[:, q4 * 4:(q4 + 1) * 4, :], in_=pb[:].rearrange("p (j t) -> p j t", j=4), func=AF.Copy),
                          reads=[pb], writes=[mT])
                tt0 = t0 + ti * 128
                if isinstance(xin, tuple):
                    srcb = xin[0] if tt0 < L else xin[1]
                    src = srcb.t[tt0:tt0 + 128, :] if tt0 < L else srcb.t[tt0 - L:tt0 - L + 128, :]
                else:
                    srcb = xin; src = xin.t[tt0:tt0 + 128, :]
                fw.dma("sp", xt[:], src, reads=[srcb], writes=[xt], key="m_x")
                for cc in range(4):
                    wgb = wg[wi % 2]; wi += 1
                    fw.dma("sp" if cc % 2 else "act", wgb[:], d["w_out"].t[l, :, cc * 512:(cc + 1) * 512].rearrange("(kt p) n -> p kt n", p=128),
                           reads=[d["w_out"]], writes=[wgb], key="m_wg%d" % (wi % 2))
                    po = self.bank()
                    for kt in range(KT):
                        fw.op("pe", lambda e, kt=kt: e.matmul(po[:], mT[:, kt, :], wgb[:, kt, :], start=(kt == 0), stop=(kt == KT - 1)),
                              reads=[mT, wgb], writes=[po])
                    fw.op("dve", lambda e: e.tensor_tensor(out=ot[:, cc * 512:(cc + 1) * 512], in0=po[:], in1=g1[r][:, cc * 512:(cc + 1) * 512], op=ALU.mult),
                          reads=[po, g1[r]], writes=[ot])
                fw.op("pool", lambda e: e.tensor_tensor(out=ot[:], in0=ot[:], in1=xt[:], op=ALU.add), reads=[ot, xt], writes=[ot])
                fw.dma("sp", s["xa"].t[tt0:tt0 + 128, :], ot[:], reads=[ot], writes=[s["xa"]], key="m_st")

    def p4a_router(self, l, es):
        fw, d, s = self.fw, self.d, self.s
        A = [fw.sb("r_A%d" % r, [128, D]) for r in range(2)]
        Bv = [fw.sb("r_B%d" % r, [128, D]) for r in range(2)]
        tmp = fw.sb("r_tmp", [128, D])
        self.load_bcast("sp", tmp, d["norm2"], l * D, D, "small")
        for r in range(2):
            self.load_bcast("act", A[r], s["modD"], r * 6 * D + 4 * D, D, "small")
            self.load_bcast("pool", Bv[r], s["modD"], r * 6 * D + 3 * D, D, "small")
            fw.op("dve", lambda e, r=r: e.scalar_tensor_tensor(out=A[r][:], in0=A[r][:], scalar=1.0, in1=tmp[:], op0=ALU.add, op1=ALU.mult),
                  reads=[A[r], tmp], writes=[A[r]])
        rt = fw.sb("r_rt", [128, KT, 16])
        fw.dma("sp", rt[:], d["router"].t[l].rearrange("(kt p) n -> p kt n", p=128), reads=[d["router"]], writes=[rt], key="small")
        xt = [fw.sb("r_xt%d" % i, [128, D]) for i in range(2)]
        hh = [fw.sb("r_hh%d" % i, [128, D]) for i in range(2)]
        hT = fw.sb("r_hT", [128, KT, 128])
        st = fw.sb("r_st", [128, 8]); lg = fw.sb("r_lg", [128, 16]); ex = fw.sb("r_ex", [128, 16])
        affT = self.affT
        ntile = NT if l == 0 else 64
        for tt in range(ntile):
            r = 0 if tt < 64 else 1
            xb = xt[tt % 2]; h = hh[tt % 2]
            fw.dma("sp", xb[:], s["xa"].t[tt * 128:(tt + 1) * 128, :], reads=[s["xa"]], writes=[xb], key="r_x%d" % (tt % 2))
            fw.op("act", lambda e: e.activation(out=h[:], in_=xb[:], func=AF.Square, accum_out=st[:, 0:1]), reads=[xb], writes=[h, st])
            fw.op("act", lambda e: e.activation(out=st[:, 1:2], in_=st[:, 0:1], func=AF.Sqrt, scale=1.0 / D, bias=EPS), reads=[st], writes=[st])
            fw.op("dve", lambda e: e.reciprocal(out=st[:, 2:3], in_=st[:, 1:2]), reads=[st], writes=[st])
            fw.op("dve", lambda e: e.scalar_tensor_tensor(out=h[:], in0=xb[:], scalar=st[:, 2:3], in1=A[r][:], op0=ALU.mult, op1=ALU.mult),
                  reads=[xb, st, A[r]], writes=[h])
            fw.op("pool", lambda e: e.tensor_tensor(out=h[:], in0=h[:], in1=Bv[r][:], op=ALU.add), reads=[h, Bv[r]], writes=[h])
            fw.dma("act", s["h2"].t[tt * 128:(tt + 1) * 128, :], h[:], reads=[h], writes=[s["h2"]], key="r_st")
            for q4 in range(4):
                pb = self.bank()
                for j in range(4):
                    kt = q4 * 4 + j
                    fw.op("pe", lambda e, kt=kt, j=j: e.transpose(out=pb[:, j * 128:(j + 1) * 128], in_=h[:, kt * 128:(kt + 1) * 128], identity=self.ident[:]),
                          reads=[h, self.ident], writes=[pb])
                fw.op("act" if q4 % 2 else "dve",
                      (lambda e: e.activation(out=hT[:, q4 * 4:(q4 + 1) * 4, :], in_=pb[:].rearrange("p (j t) -> p j t", j=4), func=AF.Copy)) if q4 % 2 else
                      (lambda e: e.tensor_copy(out=hT[:, q4 * 4:(q4 + 1) * 4, :], in_=pb[:].rearrange("p (j t) -> p j t", j=4))),
                      reads=[pb], writes=[hT])
            pl = self.bank()
            for kt in range(KT):
                fw.op("pe", lambda e, kt=kt: e.matmul(pl[:, 0:16], hT[:, kt, :], rt[:, kt, :], start=(kt == 0), stop=(kt == KT - 1)), reads=[hT, rt], writes=[pl])
            fw.op("dve", lambda e: e.tensor_copy(out=lg[:], in_=pl[:, 0:16]), reads=[pl], writes=[lg])
            fw.op("dve", lambda e: e.tensor_reduce(out=st[:, 3:4], in_=lg[:], axis=AX.X, op=ALU.max), reads=[lg], writes=[st])
            fw.op("dve", lambda e: e.tensor_scalar(out=st[:, 4:5], in0=st[:, 3:4], scalar1=-1.0, scalar2=None, op0=ALU.mult), reads=[st], writes=[st])
            fw.op("act", lambda e: e.activation(out=ex[:], in_=lg[:], func=AF.Exp, bias=st[:, 4:5], scale=1.0, accum_out=st[:, 5:6]), reads=[lg, st], writes=[ex, st])
            fw.op("dve", lambda e: e.reciprocal(out=st[:, 6:7], in_=st[:, 5:6]), reads=[st], writes=[st])
            fw.op("dve", lambda e: e.tensor_scalar(out=ex[:], in0=ex[:], scalar1=st[:, 6:7], scalar2=None, op0=ALU.mult), reads=[ex, st], writes=[ex])
            pt = self.bank()
            fw.op("pe", lambda e: e.transpose(out=pt[0:16, 0:128], in_=ex[:], identity=self.ident[:]), reads=[ex, self.ident], writes=[pt])
            fw.op("act", lambda e: e.activation(out=affT[0:16, tt * 128:(tt + 1) * 128], in_=pt[0:16, 0:128], func=AF.Copy), reads=[pt], writes=[affT])

    def p4b_topk(self, l, es):
        fw = self.fw
        affT = self.affT
        vals, idxu = self.tk_vals, self.tk_idx
        segs = [(0, L, 1024, 0)] + ([(L, AT, 32, 1024)] if l == 0 else [])
        for (a, b, cap, off) in segs:
            for rd in range(cap // 8):
                o = off + rd * 8
                fw.op("dve", lambda e: e.max(out=vals[0:16, o:o + 8], in_=affT[0:16, a:b]), reads=[affT], writes=[vals])
                fw.op("dve", lambda e: e.max_index(out=idxu[0:16, o:o + 8], in_max=vals[0:16, o:o + 8], in_values=affT[0:16, a:b]), reads=[affT, vals], writes=[idxu])
                fw.op("dve", lambda e: e.match_replace(out=affT[0:16, a:b], in_to_replace=vals[0:16, o:o + 8], in_values=affT[0:16, a:b], imm_value=-1.0),
                      reads=[affT, vals], writes=[affT])
        nch = 9 if l == 0 else 8
        idxf = fw.sb("tk_idxf", [16, 1152])
        fw.op("dve", lambda e: e.tensor_copy(out=idxf[:], in_=idxu[:]), reads=[idxu], writes=[idxf])
        if l == 0:
            fw.op("dve", lambda e: e.tensor_scalar(out=idxf[:, 1024:1056], in0=idxf[:, 1024:1056], scalar1=float(L), scalar2=None, op0=ALU.add), reads=[idxf], writes=[idxf])
        for j in range(nch):
            n = 128 if j < 8 else 32
            pt = self.bank()
            fw.op("pe", lambda e: e.transpose(out=pt[0:n, 0:16], in_=idxf[0:16, j * 128:j * 128 + n], identity=self.ident[0:16, 0:16]), reads=[idxf, self.ident], writes=[pt])
            fw.op("dve", lambda e: e.tensor_copy(out=self.idxT[0:n, j, :], in_=pt[0:n, 0:16]), reads=[pt], writes=[self.idxT])
            pt2 = self.bank()
            fw.op("pe", lambda e: e.transpose(out=pt2[0:n, 0:16], in_=vals[0:16, j * 128:j * 128 + n], identity=self.ident[0:16, 0:16]), reads=[vals, self.ident], writes=[pt2])
            fw.op("act", lambda e: e.activation(out=self.gT[0:n, j, :], in_=pt2[0:n, 0:16], func=AF.Copy), reads=[pt2], writes=[self.gT])

    def p4c_experts(self, l, es):
        fw, d, s = self.fw, self.d, self.s
        xs = [fw.sb("e_xs%d" % i, [128, D]) for i in range(2)]
        xsT = fw.sb("e_xsT", [128, KT, 512], BF16)
        zT = fw.sb("e_zT", [128, KT, 512], BF16)
        w1c = [fw.sb("e_w1c%d" % i, [128, KT, 128]) for i in range(2)]
        w3c = [fw.sb("e_w3c%d" % i, [128, KT, 128]) for i in range(2)]
        w1h = [fw.sb("e_w1h%d" % i, [128, KT, 128], BF16) for i in range(2)]
        w3h = [fw.sb("e_w3h%d" % i, [128, KT, 128], BF16) for i in range(2)]
        w2f = fw.sb("e_w2f", [128, KT, 512]); w2h = fw.sb("e_w2h", [128, KT, 512], BF16)
        ysc = [fw.sb("e_ysc%d" % i, [128, D]) for i in range(4)]
        sa = fw.sb("e_sa", [128, 512])
        g2 = [fw.sb("e_g2_%d" % r, [128, D]) for r in range(2)]
        for r in range(2):
            self.load_bcast("sp", g2[r], s["modD"], r * 6 * D + 5 * D, D, "small")
        wi = 0
        groups = []
        for e_ in range(16):
            groups.append((e_, [0, 1, 2, 3], 128, 0))
            groups.append((e_, [4, 5, 6, 7], 128, 0))
        if l == 0:
            for e_ in range(16):
                groups.append((e_, [8], 32, 1))
        for (e_, chunks, n, r) in groups:
            ntok = n * len(chunks)
            for ci, j in enumerate(chunks):
                xb = xs[ci % 2]
                fw.idma(out=xb[0:n, :], out_offset=None, in_=s["h2"].t[:, :],
                        in_offset=bass.IndirectOffsetOnAxis(ap=self.idxT[0:n, j, e_:e_ + 1], axis=0),
                        reads=[s["h2"], self.idxT], writes=[xb], key="e_g%d" % (ci % 2))
                for q4 in range(4):
                    pb = self.bank()
                    for jj in range(4):
                        kt = q4 * 4 + jj
                        fw.op("pe", lambda e, kt=kt, jj=jj: e.transpose(out=pb[:, jj * 128:jj * 128 + n], in_=xb[0:n, kt * 128:(kt + 1) * 128], identity=self.ident[0:n, 0:n]),
                              reads=[xb, self.ident], writes=[pb])
                    src = pb[:].rearrange("p (j t) -> p j t", j=4)[:, :, 0:n]
                    if q4 % 2:
                        fw.op("act", lambda e: e.activation(out=xsT[:, q4 * 4:(q4 + 1) * 4, ci * n:(ci + 1) * n], in_=src, func=AF.Copy), reads=[pb], writes=[xsT])
                    else:
                        fw.op("dve", lambda e: e.tensor_copy(out=xsT[:, q4 * 4:(q4 + 1) * 4, ci * n:(ci + 1) * n], in_=src), reads=[pb], writes=[xsT])
            for ft in range(KT):
                w1f = w1c[wi % 2]; w3f = w3c[wi % 2]; w1b = w1h[wi % 2]; w3b = w3h[wi % 2]; wi += 1
                fw.dma("sp", w1f[:], d["exp_w1"].t[l, e_, :, ft * 128:(ft + 1) * 128].rearrange("(kt p) n -> p kt n", p=128),
                       reads=[d["exp_w1"]], writes=[w1f])
                fw.dma("act", w3f[:], d["exp_w3"].t[l, e_, :, ft * 128:(ft + 1) * 128].rearrange("(kt p) n -> p kt n", p=128),
                       reads=[d["exp_w3"]], writes=[w3f])
                fw.op("pool", lambda e: e.tensor_copy(out=w1b[:], in_=w1f[:]), reads=[w1f], writes=[w1b])
                fw.op("pool", lambda e: e.tensor_copy(out=w3b[:], in_=w3f[:]), reads=[w3f], writes=[w3b])
                pa = self.bank(); pg = self.bank()
                for kt in range(KT):
                    fw.op("pe", lambda e, kt=kt: e.matmul(pa[:, 0:ntok], w1b[:, kt, :], xsT[:, kt, 0:ntok], start=(kt == 0), stop=(kt == KT - 1)), reads=[w1b, xsT], writes=[pa])
                for kt in range(KT):
                    fw.op("pe", lambda e, kt=kt: e.matmul(pg[:, 0:ntok], w3b[:, kt, :], xsT[:, kt, 0:ntok], start=(kt == 0), stop=(kt == KT - 1)), reads=[w3b, xsT], writes=[pg])
                fw.op("act", lambda e: e.activation(out=sa[:, 0:ntok], in_=pa[:, 0:ntok], func=AF.Silu), reads=[pa], writes=[sa])
                fw.op("dve", lambda e: e.tensor_tensor(out=zT[:, ft, 0:ntok], in0=pg[:, 0:ntok], in1=sa[:, 0:ntok], op=ALU.mult), reads=[pg, sa], writes=[zT])
            for dc in range(4):
                fw.dma("sp" if dc % 2 else "act", w2f[:], d["exp_w2"].t[l, e_, :, dc * 512:(dc + 1) * 512].rearrange("(kt p) n -> p kt n", p=128),
                       reads=[d["exp_w2"]], writes=[w2f])
                fw.op("pool", lambda e: e.tensor_copy(out=w2h[:], in_=w2f[:]), reads=[w2f], writes=[w2h])
                for ci, j in enumerate(chunks):
                    py = self.bank()
                    for ft in range(KT):
                        fw.op("pe", lambda e, ft=ft: e.matmul(py[0:n, :], zT[:, ft, ci * n:(ci + 1) * n], w2h[:, ft, :], start=(ft == 0), stop=(ft == KT - 1)), reads=[zT, w2h], writes=[py])
                    fw.op("dve", lambda e: e.scalar_tensor_tensor(out=ysc[ci][0:n, dc * 512:(dc + 1) * 512], in0=py[0:n, :], scalar=self.gT[0:n, j, e_:e_ + 1],
                                                                  in1=g2[r][0:n, dc * 512:(dc + 1) * 512], op0=ALU.mult, op1=ALU.mult),
                          reads=[py, self.gT, g2[r]], writes=[ysc[ci]])
            for ci, j in enumerate(chunks):
                fw.idma(out=s["xa"].t[:, :], out_offset=bass.IndirectOffsetOnAxis(ap=self.idxT[0:n, j, e_:e_ + 1], axis=0), in_=ysc[ci][0:n, :], in_offset=None,
                        reads=[ysc[ci], self.idxT], writes=[s["xa"]], key="e_sc", compute_op=ALU.add)

    def phase(self, fn, *a):
        fw = self.fw
        with ExitStack() as es:
            old = fw.es; fw.es = es
            fn(*a)
            fw.barrier()
            fw.es = old

    def p4_moe(self, l, es):
        fw = self.fw
        self.idxT = fw.sb("idxT", [128, 9, 16], I32); self.gT = fw.sb("gT", [128, 9, 16])
        with ExitStack() as es2:
            old = fw.es; fw.es = es2
            self.affT = fw.sb("affT", [16, AT])
            self.tk_vals = fw.sb("tk_vals", [16, 1152]); self.tk_idx = fw.sb("tk_idx", [16, 1152], U32)
            self.phase(self.p4a_router, l, None)
            self.phase(self.p4b_topk, l, None)
            fw.es = old
        self.phase(self.p4c_experts, l, None)

    def build(self):
        fw = self.fw
        xin = (self.d["x"], self.d["ctx"])
        for l in range(DEPTH):
            if self.test_br and l == 0:
                brin = fw.dram("brT_in", [3 * W, AT], kind="ExternalInput")
                for i in range(12):
                    fw.dma("sp", self.s["brT"].t[i * 128:(i + 1) * 128, :], brin.t[i * 128:(i + 1) * 128, :], reads=[brin], writes=[self.s["brT"]], key="brcp")
            self.phase(self.p0_mod, l, None)
            if self.stop_after == ("p0", l):
                break
            self.phase(self.p1_proj, l, None, xin)
            if self.stop_after == ("p1", l):
                break
            if not self.test_br:
                self.phase(self.p2a_s5, l, None)
                self.phase(self.p2a_glu, l, None)
            if self.stop_after == ("p2a", l):
                break
            if not self.test_br:
                self.phase(self.p2b_na_norm, l, None)
                self.phase(self.p2b_na, l, None)
            if self.stop_after == ("p2b", l):
                break
            if not self.test_br:
                self.phase(self.p2c_hy_short, l, None)
                for seg in ((0, 1) if l == 0 else (0,)):
                    self.phase(self.p2c_hy_filt, l, None, seg)
                    self.phase(self.p2c_hy_spec, l, None, seg)
                    self.phase(self.p2c_hy_conv, l, None, seg)
            if self.stop_after == ("p2c", l):
                break
            self.phase(self.p3_merge, l, None, xin)
            if self.stop_after == ("p3", l):
                break
            self.phase(self.p4_moe, l, None)
            if self.stop_after == ("p4", l):
                break
            xin = self.s["xa"]
        else:
            for i in range(64):
                fw.dma("sp" if i % 2 else "act", self.out.t[i * 128:(i + 1) * 128, :], self.s["xa"].t[i * 128:(i + 1) * 128, :], reads=[self.s["xa"]], writes=[self.out], key="outcp")
        for (oname, name, sl) in self.dbg:
            src = self.s[name]
            r0, r1, c0, c1 = sl
            o = fw.dram("dbg_" + oname, [r1 - r0, c1 - c0], kind="ExternalOutput")
            fw.dma("sp", o.t, src.t[r0:r1, c0:c1], reads=[src], writes=[o], key="dbg")
            self.dbg_out[oname] = o
        fw.finish([self.out] + list(self.dbg_out.values()))


def build_nc(stop_after=None, dbg=None, gather=True, test_br=False, wdepth=DEPTH):
    nc = bass.Bass("TRN2", target_bir_lowering=False)
    es = ExitStack()
    with es:
        k = K(nc, es, stop_after=stop_after, dbg=dbg, gather=gather, test_br=test_br, wdepth=wdepth)
        k.build()
    return nc, k


BIGW = ["w_ada", "w_in", "w_gate", "w_branch", "w_out", "exp_w1", "exp_w3", "exp_w2"]
WNAMES = ["hy_conv_w", "hy_conv_b", "hy_w1", "hy_b1", "hy_w2", "hy_b2", "hy_w3", "hy_b3", "hy_w4", "hy_freq", "hy_bias", "na_q_gain", "na_k_gain", "ssm_lam_re", "ssm_lam_im", "ssm_log_step", "ssm_b_re", "ssm_b_im", "ssm_c_re", "ssm_c_im", "ssm_d", "ssm_w_glu", "w_ada", "b_ada", "norm1", "norm2", "w_in", "w_gate", "b_gate", "w_branch", "w_out", "router", "exp_w1", "exp_w3", "exp_w2"]


def na_table(rpb):
    Lr = rpb.shape[0]
    col = np.arange(64)
    startc = np.clip(col - 8, 0, 48)
    kc = np.arange(64)
    inwin = (kc[None, :] >= startc[:, None]) & (kc[None, :] < startc[:, None] + 16)
    dc = np.clip(kc[None, :] - col[:, None] + 15, 0, 30)
    tab = np.empty((Lr, 8, 8, 8, 64, 64), np.float32)
    for dr0 in range(8):
        for j in range(8):
            v = rpb[:, :, j + dr0, :][:, :, dc]
            v = np.where(inwin[None, None], v, np.float32(-30000.0))
            tab[:, dr0, :, j] = np.transpose(v, (0, 1, 3, 2))
    tab = tab.reshape(Lr, 8, 8, 4, 128, 64)
    return np.ascontiguousarray(np.transpose(tab, (0, 1, 4, 2, 3, 5)))


def hyena_consts():
    n = np.arange(128, dtype=np.float64)
    ang = 2 * np.pi * np.outer(n, n) / 128.0
    F = np.stack([np.cos(ang), -np.sin(ang), np.sin(ang)], axis=1).astype(np.float32)
    angt = 2 * np.pi * np.outer(n, n) / 16384.0
    T = np.stack([np.cos(angt), -np.sin(angt)], axis=1).astype(np.float32)
    out = {"hyc_F": F, "hyc_T": T}
    for nm, length in (("L", L), ("c", CT)):
        t = np.linspace(0.0, 1.0, length, dtype=np.float32)[:, None]
        freqs = np.linspace(1e-4, 15, 16, dtype=np.float32)
        a = (np.float32(2.0 * np.pi / length) * np.arange(length, dtype=np.float32)[:, None] * freqs[None, :]).astype(np.float32)
        z = np.concatenate([t, np.cos(a), -np.sin(a)], axis=-1).astype(np.float32)
        mn, mx = np.log(1e-2) / 1.5, np.log(1e-2) / 0.3
        dec = np.exp(-t * np.abs(np.linspace(mn, mx, W, dtype=np.float32))[None, :]).astype(np.float32)
        out["hyc_z" + nm] = np.ascontiguousarray(z.T)
        out["hyc_d" + nm] = np.ascontiguousarray(dec.T)
    return out


def make_in_maps(inputs, cores, used=None, gather=True):
    maps = []
    shared = {}
    shards = {}
    for n in WNAMES:
        if used is not None and n not in used:
            continue
        a = np.ascontiguousarray(inputs[n])
        if n in BIGW and gather:
            a2 = a.reshape(DEPTH, -1, a.shape[-1])
            R = a2.shape[1]
            shards[n] = [np.ascontiguousarray(a2[:, c * (R // 8):(c + 1) * (R // 8), :]) for c in range(8)]
        else:
            shared[n] = a
    for n_, v_ in hyena_consts().items():
        if used is None or n_ in used:
            shared[n_] = v_
    if used is None or "na_tab" in used:
        shared["na_tab"] = na_table(np.asarray(inputs["na_rpb"], np.float32))
    for c in cores:
        b = c % 4
        m = dict(shared)
        for n in shards:
            m[n + "_sh"] = shards[n][c]
        if used is None or "x" in used:
            m["x"] = np.ascontiguousarray(inputs["x"][b])
        if used is None or "ctx" in used:
            m["ctx"] = np.ascontiguousarray(inputs["ctx"][b])
        m["cvec"] = np.ascontiguousarray(np.stack([inputs["c"][b], inputs["c_ctx"]], axis=0))
        maps.append(m)
    return maps


def kernel(**inputs):
    nc, k = build_nc(gather=False)
    cores = list(range(8))
    res = run_bass_kernel_spmd(nc, make_in_maps(inputs, cores, set(k.d.keys()), gather=False), core_ids=cores)
    out = np.stack([res.results[b]["out"] for b in range(4)], axis=0)
    return out.astype(np.float32)
```

```python
import numpy as np
import concourse.bass as bass
import concourse.mybir as mybir
from concourse.bass_utils import run_bass_kernel_spmd
from contextlib import ExitStack

F32 = mybir.dt.float32
BF16 = mybir.dt.bfloat16
U32 = mybir.dt.uint32
I32 = mybir.dt.int32
ALU = mybir.AluOpType
AF = mybir.ActivationFunctionType
AX = mybir.AxisListType

D = 2048
L = 8192
CT = 256
AT = L + CT
NT = AT // 128
KT = D // 128
W = 512
INW = 3584
DEPTH = 2
EPS = 1e-6
PI = float(np.pi)


class Buf:
    __slots__ = ("t", "last_w", "readers", "name", "space")

    def __init__(self, t, name="", space="sb"):
        self.t = t
        self.last_w = None
        self.readers = []
        self.name = name
        self.space = space

    def __getitem__(self, k):
        return self.t[k]


class _LV:
    def __init__(self, views):
        self.views = views

    def __getitem__(self, k):
        if isinstance(k, tuple):
            v = self.views[k[0]]
            return v[k[1:]] if len(k) > 1 else v
        return self.views[k]


class LayerView(Buf):
    __slots__ = ()

    def __init__(self, views, name):
        Buf.__init__(self, _LV(views), name, "dram")


class FW:
    ENG = ("pe", "act", "dve", "pool", "sp")

    def __init__(self, nc, es):
        self.nc = nc
        self.es = es
        self.root_es = es
        self.eng = {"pe": nc.tensor, "act": nc.scalar, "dve": nc.vector, "pool": nc.gpsimd, "sp": nc.sync}
        self.esem = {k: es.enter_context(nc.semaphore("es_" + k)) for k in self.ENG}
        self.ecnt = {k: 0 for k in self.ENG}
        self.dslots = []
        self.dcnt = []
        self.kmap = {}
        self.seen = {k: {} for k in self.ENG}
        self.n_inst = 0
        self.bufs = []
        self.uniq = 0

    def _reg(self, b):
        self.bufs.append(b)
        return b

    def sb(self, name, shape, dt=F32):
        self.uniq += 1
        name = "%s_%d" % (name, self.uniq)
        return self._reg(Buf(self.es.enter_context(self.nc.sbuf_tensor(name, list(shape), dt)), name, "sb"))

    def ps(self, name, shape, dt=F32):
        return self._reg(Buf(self.es.enter_context(self.nc.psum_tensor(name, list(shape), dt)), name, "ps"))

    def dram(self, name, shape, dt=F32, kind="Internal"):
        return self._reg(Buf(self.nc.dram_tensor(name, list(shape), dt, kind=kind).ap(), name, "dram"))

    def _slot(self, key):
        if key not in self.kmap:
            i = len(self.kmap)
            if i >= len(self.dslots):
                self.dslots.append(self.root_es.enter_context(self.nc.semaphore("ds%d" % i)))
                self.dcnt.append(0)
            self.kmap[key] = i
        return self.kmap[key]

    def _wait(self, e, ev):
        if ev is None:
            return
        kind, key, val = ev
        if kind == "e" and key == "pe" and e == "pe":
            return
        if kind == "d":
            val = self.dcnt[key]
            sem = self.dslots[key]
        else:
            sem = self.esem[key]
        k = (kind, key)
        if self.seen[e].get(k, 0) >= val:
            return
        self.seen[e][k] = val
        self.eng[e].wait_ge(sem, val)

    def _deps(self, e, reads, writes):
        for r in reads:
            self._wait(e, r.last_w)
        for w in writes:
            self._wait(e, w.last_w)
            for ev in w.readers:
                self._wait(e, ev)

    def _commit(self, ev, reads, writes):
        for r in reads:
            r.readers.append(ev)
            if len(r.readers) > 48:
                d = {}
                for x in r.readers:
                    k = (x[0], x[1])
                    if k not in d or d[k][2] < x[2]:
                        d[k] = x
                r.readers = list(d.values())
        for w in writes:
            w.last_w = ev
            w.readers = []

    def op(self, e, fn, reads=(), writes=()):
        self._deps(e, reads, writes)
        ins = fn(self.eng[e])
        self.ecnt[e] += 1
        ins.then_inc(self.esem[e], 1)
        ev = ("e", e, self.ecnt[e])
        self._commit(ev, reads, writes)
        self.n_inst += 1
        return ev

    def _stream(self, reads, writes, key):
        for b in list(writes) + list(reads):
            if b.space == "sb":
                return self._slot("b_" + b.name)
        if key is None:
            self.uniq += 1
            key = "u%d" % self.uniq
        return self._slot("k_" + key)

    def _issued(self, slot, ins, inc, reads, writes):
        self.dcnt[slot] += inc
        ins.then_inc(self.dslots[slot], inc)
        ev = ("d", slot, self.dcnt[slot])
        self._commit(ev, reads, writes)
        self.n_inst += 1
        return ev

    def dma(self, q, out, in_, reads=(), writes=(), key=None, **kw):
        slot = self._stream(reads, writes, key)
        self._deps(q, reads, writes)
        ins = self.eng[q].dma_start(out=out, in_=in_, **kw)
        return self._issued(slot, ins, 16, reads, writes)

    def idma(self, out, in_, out_offset=None, in_offset=None, reads=(), writes=(), key=None, **kw):
        slot = self._stream([r for r in reads if r.name != "idxT"], writes, None)
        self._deps("pool", reads, writes)
        ins = self.nc.gpsimd.indirect_dma_start(out=out, out_offset=out_offset, in_=in_, in_offset=in_offset, **kw)
        return self._issued(slot, ins, 16, reads, writes)

    def allgather(self, out_buf, out_ap, in_buf, in_ap):
        slot = self._stream([], [], None)
        self._deps("pool", [in_buf], [out_buf])
        ins = self.nc.gpsimd.collective_compute("AllGather", ALU.bypass, replica_groups=[list(range(8))], ins=[in_ap], outs=[out_ap])
        return self._issued(slot, ins, 1, [in_buf], [out_buf])

    def barrier(self):
        for e in self.ENG:
            for o in self.ENG:
                if o != e and self.ecnt[o] > 0:
                    self._wait(e, ("e", o, self.ecnt[o]))
            for slot in range(len(self.dslots)):
                if self.dcnt[slot] > 0:
                    self._wait(e, ("d", slot, self.dcnt[slot]))
        for b in self.bufs:
            b.last_w = None
            b.readers = []
        self.kmap = {}

    def finish(self, bufs):
        for slot in range(len(self.dslots)):
            if self.dcnt[slot] > 0:
                self._wait("sp", ("d", slot, self.dcnt[slot]))


def dap(buf, offset, pattern):
    return bass.AP(tensor=buf.t.tensor, offset=offset, ap=[list(p) for p in pattern])


class K:
    def __init__(self, nc, es, stop_after=None, dbg=None, gather=True, test_br=False, wdepth=DEPTH):
        self.nc = nc
        self.gather = gather
        self.test_br = test_br
        self.dumps_on = test_br
        self.fw = FW(nc, es)
        self.stop_after = stop_after
        self.dbg = dbg or []
        fw = self.fw
        shapes = {
            "x": [L, D], "ctx": [CT, D], "cvec": [2, D],
            "w_ada": [DEPTH, D, 6 * D], "b_ada": [DEPTH, 6 * D], "norm1": [DEPTH, D], "norm2": [DEPTH, D],
            "w_in": [DEPTH, D, INW], "w_gate": [DEPTH, D, 3 * D], "b_gate": [DEPTH, 3 * D],
            "w_branch": [DEPTH, 3, W, D], "w_out": [DEPTH, D, D], "router": [DEPTH, D, 16],
            "exp_w1": [DEPTH, 16, D, D], "exp_w3": [DEPTH, 16, D, D], "exp_w2": [DEPTH, 16, D, D],
            "ssm_lam_re": [DEPTH, 2, 32, 64], "ssm_lam_im": [DEPTH, 2, 32, 64], "ssm_log_step": [DEPTH, 2, 32],
            "ssm_b_re": [DEPTH, 2, 32, 64, 16], "ssm_b_im": [DEPTH, 2, 32, 64, 16],
            "ssm_c_re": [DEPTH, 2, 32, 16, 64], "ssm_c_im": [DEPTH, 2, 32, 16, 64],
            "ssm_d": [DEPTH, 512], "ssm_w_glu": [DEPTH, 512, 512],
            "na_q_gain": [DEPTH, 64], "na_k_gain": [DEPTH, 64], "na_tab": [DEPTH, 8, 128, 8, 4, 64],
            "hy_conv_w": [DEPTH, 3, 1536], "hy_conv_b": [DEPTH, 1536], "hy_w1": [DEPTH, 33, 64], "hy_b1": [DEPTH, 64],
            "hy_w2": [DEPTH, 64, 64], "hy_b2": [DEPTH, 64], "hy_w3": [DEPTH, 64, 64], "hy_b3": [DEPTH, 64],
            "hy_w4": [DEPTH, 64, 2048], "hy_freq": [DEPTH, 64], "hy_bias": [DEPTH, 2, 512],
            "hyc_F": [128, 3, 128], "hyc_T": [128, 2, 128], "hyc_zL": [33, L], "hyc_zc": [33, CT], "hyc_dL": [W, L], "hyc_dc": [W, CT],
        }

        BIG = set(BIGW)
        gather_mode = self.gather

        class Lazy(dict):
            def __missing__(s_, n):
                shp = shapes[n]
                if n in BIG and gather_mode:
                    R = int(np.prod(shp[1:-1])); C = shp[-1]
                    ext = fw.dram(n + "_sh", [DEPTH, R // 8, C], F32, kind="ExternalInput")
                    views = []
                    for l in range(DEPTH):
                        shl = fw.dram(n + "_shi%d" % l, [R // 8, C], F32)
                        fl = fw.dram(n + "_full%d" % l, [R, C], F32)
                        for r0 in range(0, R // 8, 128):
                            r1 = min(r0 + 128, R // 8)
                            fw.dma("sp" if (r0 // 128) % 2 else "act", shl.t[r0:r1, :], ext.t[l, r0:r1, :], reads=[ext], writes=[shl], key="shcp_%s%d" % (n, l))
                        fw.allgather(fl, fl.t, shl, shl.t)
                        views.append(fl.t.rearrange("(a r) c -> a r c", a=shp[1]) if len(shp) == 4 else fl.t)
                    b = LayerView(views, n)
                    fw._reg(b)
                    s_[n] = b
                    return b
                if n in BIG:
                    shp = [wdepth] + list(shp[1:])
                b = fw.dram(n, shp, F32, kind="ExternalInput")
                s_[n] = b
                return b
        d = Lazy()
        self.d = d
        if self.gather:
            for n in BIGW:
                d[n]
            fw.barrier()
        s = {}
        s["modD"] = fw.dram("modD", [2, 6 * D])
        s["hT"] = fw.dram("hT", [128, KT, AT])
        s["projT"] = fw.dram("projT", [INW, AT])
        s["vtok"] = fw.dram("vtok", [AT, W])
        s["brT"] = fw.dram("brT", [3 * W, AT])
        s["s5z"] = fw.dram("s5z", [W, AT])
        s["qnT"] = fw.dram("qnT", [W, AT]); s["knT"] = fw.dram("knT", [W, AT])
        s["hzT"] = fw.dram("hzT", [3 * W, AT]); s["hfT"] = fw.dram("hfT", [2048, L]); s["hfTc"] = fw.dram("hfTc", [2048, CT])
        s["hnrm"] = fw.dram("hnrm", [2, 2, W])
        s["hspec"] = fw.dram("hspec", [2, 128, 128, 2, 512]); s["hspecc"] = fw.dram("hspecc", [2, 128, 128, 2, 512])
        s["xa"] = fw.dram("xa", [AT, D])
        s["h2"] = fw.dram("h2", [AT, D])
        self.s = s
        self.out = fw.dram("out", [L, D], kind="ExternalOutput")
        self.dbg_out = {}
        self.ident = fw.sb("ident", [128, 128])
        io = fw.sb("iota_i", [128, 128], I32)
        fw.op("pool", lambda e: e.iota(io[:], pattern=[[1, 128]], base=0, channel_multiplier=-1), writes=[io])
        fw.op("dve", lambda e: e.tensor_single_scalar(out=self.ident[:], in_=io[:], scalar=0, op=ALU.is_equal), reads=[io], writes=[self.ident])
        self.pb = [fw.ps("pb%d" % i, [128, 512]) for i in range(8)]
        self.pbi = 0

    def dump(self, name, buf, ap):
        if not self.dumps_on:
            return
        o = self.fw.dram("dbg_" + name, list(ap.shape), kind="ExternalOutput")
        self.fw.dma("sp", o.t, ap, reads=[buf], writes=[o])
        self.dbg_out[name] = o

    def bank(self):
        b = self.pb[self.pbi % 8]
        self.pbi += 1
        return b

    def p0_mod(self, l, es):
        fw, d, s = self.fw, self.d, self.s
        cv = fw.sb("cv", [2, D]); sil2 = fw.sb("silc2", [128, KT, 2])
        wa = [fw.sb("wada%d" % i, [128, KT, 512]) for i in range(2)]
        bb = fw.sb("bada", [2, 512]); mo = [fw.sb("modo%d" % r, [1, 512]) for r in range(2)]
        fw.dma("sp", cv[:], d["cvec"].t, reads=[d["cvec"]], writes=[cv], key="small")
        pbt = self.bank()
        for kt in range(KT):
            fw.op("pe", lambda e, kt=kt: e.transpose(out=pbt[:, kt * 2:(kt + 1) * 2], in_=cv[0:2, kt * 128:(kt + 1) * 128], identity=self.ident[0:2, 0:2]),
                  reads=[cv, self.ident], writes=[pbt])
        fw.op("act", lambda e: e.activation(out=sil2[:].rearrange("p k r -> p (k r)"), in_=pbt[:, 0:32], func=AF.Silu), reads=[pbt], writes=[sil2])
        for j in range(24):
            wb = wa[j % 2]
            src = d["w_ada"].t[l, :, j * 512:(j + 1) * 512].rearrange("(kt p) n -> p kt n", p=128)
            fw.dma("sp" if j % 2 == 0 else "act", wb[:], src, reads=[d["w_ada"]], writes=[wb], key="wada%d" % (j % 2))
            fw.dma("pool", bb[:], dap(d["b_ada"], l * 6 * D + j * 512, [[0, 2], [1, 512]]), reads=[d["b_ada"]], writes=[bb], key="small")
            for r in range(2):
                pb = self.bank()
                for kt in range(KT):
                    fw.op("pe", lambda e, kt=kt: e.matmul(pb[0:1, :], sil2[:, kt, r:r + 1], wb[:, kt, :], start=(kt == 0), stop=(kt == KT - 1)),
                          reads=[sil2, wb], writes=[pb])
                fw.op("dve", lambda e: e.tensor_tensor(out=mo[r][:], in0=pb[0:1, :], in1=bb[0:1, :], op=ALU.add), reads=[pb, bb], writes=[mo[r]])
                fw.dma("sp", s["modD"].t[r:r + 1, j * 512:(j + 1) * 512], mo[r][:], reads=[mo[r]], writes=[s["modD"]], key="modst")

    def load_bcast(self, q, dst, src_buf, offset, n, key=None, ap=None):
        ap = dst[:] if ap is None else ap
        self.fw.dma(q, ap, dap(src_buf, offset, [[0, ap.shape[0]], [1, n]]), reads=[src_buf], writes=[dst], key=key)

    def p1_proj(self, l, es, xin):
        fw, d, s = self.fw, self.d, self.s
        A = [fw.sb("A1_%d" % r, [128, D]) for r in range(2)]
        Bv = [fw.sb("B1_%d" % r, [128, D]) for r in range(2)]
        tmp = fw.sb("p1tmp", [128, D])
        self.load_bcast("sp", tmp, d["norm1"], l * D, D, "small")
        for r in range(2):
            self.load_bcast("act", A[r], s["modD"], r * 6 * D + 1 * D, D, "small")
            self.load_bcast("pool", Bv[r], s["modD"], r * 6 * D + 0 * D, D, "small")
            fw.op("dve", lambda e, r=r: e.scalar_tensor_tensor(out=A[r][:], in0=A[r][:], scalar=1.0, in1=tmp[:], op0=ALU.add, op1=ALU.mult),
                  reads=[A[r], tmp], writes=[A[r]])
        xt = [fw.sb("xt%d" % i, [128, D]) for i in range(2)]
        ht = [fw.sb("ht%d" % i, [128, D]) for i in range(2)]
        st = fw.sb("p1st", [128, 4])
        hTb = [fw.sb("hTb%d" % i, [128, KT, 512]) for i in range(1)]
        wch = [fw.sb("wch%d" % i, [128, KT, 512]) for i in range(2)]
        stg = [fw.sb("stg%d" % i, [128, 512]) for i in range(3)]
        nblk = 17
        wi = 0
        si = 0
        for blk in range(nblk):
            ntile = 4 if blk < 16 else 2
            ntok = ntile * 128
            hb = hTb[0]
            r = 0 if blk < 16 else 1
            for ti in range(ntile):
                tt = blk * 4 + ti
                xb = xt[tt % 2]; hh = ht[tt % 2]
                if isinstance(xin, tuple):
                    src = xin[0].t[tt * 128:(tt + 1) * 128, :] if tt < 64 else xin[1].t[(tt - 64) * 128:(tt - 63) * 128, :]
                    srcb = xin[0] if tt < 64 else xin[1]
                else:
                    src = xin.t[tt * 128:(tt + 1) * 128, :]; srcb = xin
                fw.dma("sp", xb[:], src, reads=[srcb], writes=[xb], key="xld%d" % (tt % 2))
                fw.op("act", lambda e: e.activation(out=hh[:], in_=xb[:], func=AF.Square, accum_out=st[:, 0:1]), reads=[xb], writes=[hh, st])
                fw.op("act", lambda e: e.activation(out=st[:, 1:2], in_=st[:, 0:1], func=AF.Sqrt, scale=1.0 / D, bias=EPS), reads=[st], writes=[st])
                fw.op("dve", lambda e: e.reciprocal(out=st[:, 2:3], in_=st[:, 1:2]), reads=[st], writes=[st])
                fw.op("dve", lambda e: e.scalar_tensor_tensor(out=hh[:], in0=xb[:], scalar=st[:, 2:3], in1=A[r][:], op0=ALU.mult, op1=ALU.mult),
                      reads=[xb, st, A[r]], writes=[hh])
                fw.op("pool", lambda e: e.tensor_tensor(out=hh[:], in0=hh[:], in1=Bv[r][:], op=ALU.add), reads=[hh, Bv[r]], writes=[hh])
                for q4 in range(4):
                    pb = self.bank()
                    for j in range(4):
                        kt = q4 * 4 + j
                        fw.op("pe", lambda e, kt=kt, j=j: e.transpose(out=pb[:, j * 128:(j + 1) * 128], in_=hh[:, kt * 128:(kt + 1) * 128], identity=self.ident[:]),
                              reads=[hh, self.ident], writes=[pb])
                    eng = "act" if q4 % 2 == 0 else "dve"
                    if eng == "act":
                        fw.op("act", lambda e: e.activation(out=hb[:, q4 * 4:(q4 + 1) * 4, ti * 128:(ti + 1) * 128],
                                                            in_=pb[:].rearrange("p (j t) -> p j t", j=4), func=AF.Copy), reads=[pb], writes=[hb])
                    else:
                        fw.op("dve", lambda e: e.tensor_copy(out=hb[:, q4 * 4:(q4 + 1) * 4, ti * 128:(ti + 1) * 128],
                                                             in_=pb[:].rearrange("p (j t) -> p j t", j=4)), reads=[pb], writes=[hb])
            for q in range(4):
                fw.dma("sp" if q % 2 else "act", s["hT"].t[:, q * 4:(q + 1) * 4, blk * 512: blk * 512 + ntok], hb[:, q * 4:(q + 1) * 4, 0:ntok], reads=[hb], writes=[s["hT"]], key="hTst")
            if blk == 0 and l == 0:
                self.dump("hb_sb", hb, hb[:, 0, 0:128])
                self.dump("hT_dr", s["hT"], s["hT"].t[:, 0, 0:128])
            for cc in range(7):
                wb = wch[wi % 2]; wi += 1
                src = d["w_in"].t[l, :, cc * 512:(cc + 1) * 512].rearrange("(kt p) n -> p kt n", p=128)
                fw.dma("act" if wi % 2 else "sp", wb[:], src, reads=[d["w_in"]], writes=[wb], key="wch%d" % (wi % 2))
                if cc == 3:
                    for ti in range(ntile):
                        pb = self.bank()
                        for kt in range(KT):
                            fw.op("pe", lambda e, kt=kt: e.matmul(pb[:], hb[:, kt, ti * 128:(ti + 1) * 128], wb[:, kt, :], start=(kt == 0), stop=(kt == KT - 1)),
                                  reads=[hb, wb], writes=[pb])
                        sg = stg[si % 3]; si += 1
                        fw.op("act", lambda e: e.activation(out=sg[:], in_=pb[:], func=AF.Copy), reads=[pb], writes=[sg])
                        t0 = blk * 512 + ti * 128
                        fw.dma("pool", s["vtok"].t[t0:t0 + 128, :], sg[:], reads=[sg], writes=[s["vtok"]], key="pst")
                    continue
                for ct in range(4):
                    pb = self.bank()
                    for kt in range(KT):
                        fw.op("pe", lambda e, kt=kt: e.matmul(pb[:, 0:ntok], wb[:, kt, ct * 128:(ct + 1) * 128], hb[:, kt, 0:ntok], start=(kt == 0), stop=(kt == KT - 1)),
                              reads=[hb, wb], writes=[pb])
                    sg = stg[si % 3]; si += 1
                    if si % 2:
                        fw.op("act", lambda e: e.activation(out=sg[:, 0:ntok], in_=pb[:, 0:ntok], func=AF.Copy), reads=[pb], writes=[sg])
                    else:
                        fw.op("dve", lambda e: e.tensor_copy(out=sg[:, 0:ntok], in_=pb[:, 0:ntok]), reads=[pb], writes=[sg])
                    c0 = cc * 512 + ct * 128
                    fw.dma("pool", s["projT"].t[c0:c0 + 128, blk * 512: blk * 512 + ntok], sg[:, 0:ntok], reads=[sg], writes=[s["projT"]], key="pst")

    def sin_reduced(self, out, ang, tmp_i, tmp_f, bufs_r, bufs_w):
        fw = self.fw
        fw.op("dve", lambda e: e.tensor_scalar(out=tmp_i, in0=ang, scalar1=1.0 / (2 * PI), scalar2=None, op0=ALU.mult), reads=bufs_r, writes=bufs_w)
        fw.op("dve", lambda e: e.tensor_copy(out=tmp_f, in_=tmp_i), reads=bufs_w, writes=bufs_w)
        fw.op("dve", lambda e: e.scalar_tensor_tensor(out=ang, in0=tmp_f, scalar=-2 * PI, in1=ang, op0=ALU.mult, op1=ALU.add), reads=bufs_w + bufs_r, writes=bufs_r)
        fw.op("dve", lambda e: e.tensor_scalar(out=ang, in0=ang, scalar1=PI, scalar2=-PI, op0=ALU.min, op1=ALU.max), reads=bufs_r, writes=bufs_r)
        fw.op("act", lambda e: e.activation(out=out, in_=ang, func=AF.Sin), reads=bufs_r, writes=bufs_w)

    def p2a_s5(self, l, es):
        fw, d, s = self.fw, self.d, self.s
        T = AT
        NG = 32
        ld = fw.sb("s5_ld", [32, 3, 128])
        fw.dma("sp", ld[:, 0, :], d["ssm_lam_re"].t[l].rearrange("d (gp g2) p -> (d gp) (g2 p)", g2=2), reads=[d["ssm_lam_re"]], writes=[ld])
        fw.dma("sp", ld[:, 1, :], d["ssm_lam_im"].t[l].rearrange("d (gp g2) p -> (d gp) (g2 p)", g2=2), reads=[d["ssm_lam_im"]], writes=[ld])
        ls = fw.sb("s5_ls", [32, 2])
        fw.dma("sp", ls[:], d["ssm_log_step"].t[l].rearrange("d (gp g2) -> (d gp) g2", g2=2), reads=[d["ssm_log_step"]], writes=[ls])
        fw.op("dve", lambda e: e.tensor_copy(out=ld[:, 2, :].rearrange("a (g p) -> a g p", g=2), in_=ls[:].unsqueeze(2).to_broadcast([32, 2, 64])), reads=[ls], writes=[ld])
        par = fw.sb("s5_par", [128, 24, 32])
        P_ = lambda i: par[:, i, :]
        for i in range(3):
            pb = self.bank()
            fw.op("pe", lambda e, i=i: e.transpose(out=pb[:, 0:32], in_=ld[:, i, :], identity=self.ident[0:32, 0:32]), reads=[ld, self.ident], writes=[pb])
            fw.op("dve", lambda e, i=i: e.tensor_copy(out=P_(i), in_=pb[:, 0:32]), reads=[pb], writes=[par])
        LR, LI, STEP, MAG, ANG, ARE, AIM, TMP, DEN, NRE, CRE, CIM, T2, ANG2 = range(14)
        pi_ = fw.sb("s5_pari", [128, 32], I32)
        R, Wp = [par], [par, pi_]
        op = lambda eng, fn: fw.op(eng, fn, reads=[par, pi_], writes=[par, pi_])
        op("act", lambda e: e.activation(out=P_(STEP), in_=P_(2), func=AF.Exp))
        op("dve", lambda e: e.tensor_tensor(out=P_(MAG), in0=P_(LR), in1=P_(STEP), op=ALU.mult))
        op("act", lambda e: e.activation(out=P_(MAG), in_=P_(MAG), func=AF.Exp))
        op("dve", lambda e: e.tensor_tensor(out=P_(ANG), in0=P_(LI), in1=P_(STEP), op=ALU.mult))
        op("dve", lambda e: e.tensor_scalar(out=P_(ANG2), in0=P_(ANG), scalar1=PI / 2, scalar2=None, op0=ALU.add))
        self.sin_reduced(P_(AIM), P_(ANG), pi_[:], P_(TMP), [par], [par, pi_])
        self.sin_reduced(P_(ARE), P_(ANG2), pi_[:], P_(TMP), [par], [par, pi_])
        op("dve", lambda e: e.tensor_tensor(out=P_(ARE), in0=P_(ARE), in1=P_(MAG), op=ALU.mult))
        op("dve", lambda e: e.tensor_tensor(out=P_(AIM), in0=P_(AIM), in1=P_(MAG), op=ALU.mult))
        op("dve", lambda e: e.tensor_tensor(out=P_(DEN), in0=P_(LR), in1=P_(LR), op=ALU.mult))
        op("dve", lambda e: e.tensor_tensor(out=P_(TMP), in0=P_(LI), in1=P_(LI), op=ALU.mult))
        op("dve", lambda e: e.tensor_tensor(out=P_(DEN), in0=P_(DEN), in1=P_(TMP), op=ALU.add))
        op("dve", lambda e: e.reciprocal(out=P_(DEN), in_=P_(DEN)))
        op("dve", lambda e: e.tensor_scalar(out=P_(NRE), in0=P_(ARE), scalar1=-1.0, scalar2=None, op0=ALU.add))
        op("dve", lambda e: e.tensor_tensor(out=P_(CRE), in0=P_(NRE), in1=P_(LR), op=ALU.mult))
        op("dve", lambda e: e.tensor_tensor(out=P_(TMP), in0=P_(AIM), in1=P_(LI), op=ALU.mult))
        op("dve", lambda e: e.tensor_tensor(out=P_(CRE), in0=P_(CRE), in1=P_(TMP), op=ALU.add))
        op("dve", lambda e: e.tensor_tensor(out=P_(CRE), in0=P_(CRE), in1=P_(DEN), op=ALU.mult))
        op("dve", lambda e: e.tensor_tensor(out=P_(CIM), in0=P_(AIM), in1=P_(LR), op=ALU.mult))
        op("dve", lambda e: e.tensor_tensor(out=P_(TMP), in0=P_(NRE), in1=P_(LI), op=ALU.mult))
        op("dve", lambda e: e.tensor_tensor(out=P_(CIM), in0=P_(CIM), in1=P_(TMP), op=ALU.subtract))
        op("dve", lambda e: e.tensor_tensor(out=P_(CIM), in0=P_(CIM), in1=P_(DEN), op=ALU.mult))
        NS = 14
        pw = fw.sb("s5_pw", [128, NS, 3, 32])
        fw.op("dve", lambda e: e.tensor_copy(out=pw[:, 0, 0, :], in_=P_(ARE)), reads=[par], writes=[pw])
        fw.op("dve", lambda e: e.tensor_copy(out=pw[:, 0, 1, :], in_=P_(AIM)), reads=[par], writes=[pw])
        for k in range(1, NS):
            fw.op("dve", lambda e, k=k: e.tensor_tensor(out=P_(TMP), in0=pw[:, k - 1, 0, :], in1=pw[:, k - 1, 0, :], op=ALU.mult), reads=[pw, par], writes=[par])
            fw.op("dve", lambda e, k=k: e.tensor_tensor(out=P_(T2), in0=pw[:, k - 1, 1, :], in1=pw[:, k - 1, 1, :], op=ALU.mult), reads=[pw, par], writes=[par])
            fw.op("dve", lambda e, k=k: e.tensor_tensor(out=pw[:, k, 0, :], in0=P_(TMP), in1=P_(T2), op=ALU.subtract), reads=[pw, par], writes=[pw])
            fw.op("dve", lambda e, k=k: e.tensor_tensor(out=P_(TMP), in0=pw[:, k - 1, 0, :], in1=pw[:, k - 1, 1, :], op=ALU.mult), reads=[pw, par], writes=[par])
            fw.op("dve", lambda e, k=k: e.tensor_scalar(out=pw[:, k, 1, :], in0=P_(TMP), scalar1=2.0, scalar2=None, op0=ALU.mult), reads=[pw, par], writes=[pw])
        fw.op("dve", lambda e: e.tensor_scalar(out=pw[:, :, 2, :], in0=pw[:, :, 1, :], scalar1=-1.0, scalar2=None, op0=ALU.mult), reads=[pw], writes=[pw])
        braw = fw.sb("s5_braw", [128, 2, 32, 16]); bb = fw.sb("s5_bb", [128, 2, 32, 16]); btmp = fw.sb("s5_btmp", [128, 32, 16])
        for ri, nm in enumerate(("ssm_b_re", "ssm_b_im")):
            fw.dma("sp" if ri == 0 else "act", braw[:, ri, :, :], d[nm].t[l].rearrange("d (gp g2) p k -> (g2 p) (d gp) k", g2=2), reads=[d[nm]], writes=[braw])
        cre_b = P_(CRE).unsqueeze(2).to_broadcast([128, 32, 16]); cim_b = P_(CIM).unsqueeze(2).to_broadcast([128, 32, 16])
        fw.op("dve", lambda e: e.tensor_tensor(out=bb[:, 0, :, :], in0=braw[:, 0, :, :], in1=cre_b, op=ALU.mult), reads=[braw, par], writes=[bb])
        fw.op("dve", lambda e: e.tensor_tensor(out=btmp[:], in0=braw[:, 1, :, :], in1=cim_b, op=ALU.mult), reads=[braw, par], writes=[btmp])
        fw.op("dve", lambda e: e.tensor_tensor(out=bb[:, 0, :, :], in0=bb[:, 0, :, :], in1=btmp[:], op=ALU.subtract), reads=[bb, btmp], writes=[bb])
        fw.op("dve", lambda e: e.tensor_tensor(out=bb[:, 1, :, :], in0=braw[:, 1, :, :], in1=cre_b, op=ALU.mult), reads=[braw, par], writes=[bb])
        fw.op("dve", lambda e: e.tensor_tensor(out=btmp[:], in0=braw[:, 0, :, :], in1=cim_b, op=ALU.mult), reads=[braw, par, bb], writes=[btmp])
        fw.op("dve", lambda e: e.tensor_tensor(out=bb[:, 1, :, :], in0=bb[:, 1, :, :], in1=btmp[:], op=ALU.add), reads=[bb, btmp], writes=[bb])
        cpd = fw.sb("s5_cpd", [32, 2, 128])
        fw.op("pool", lambda e: e.memset(cpd[:], 0.0), writes=[cpd])
        A = [fw.sb("s5_A%d" % i, [128, T]) for i in range(2)]
        Bq = [fw.sb("s5_B%d" % i, [128, T]) for i in range(2)]
        yacc = fw.sb("s5_yacc", [128, T])
        uch = [fw.sb("s5_u%d" % i, [128, 512]) for i in range(2)]
        wB = fw.sb("s5_wB", [128, 2, 128]); lB = fw.sb("s5_lB", [128, 2, 128]); lC = fw.sb("s5_lC", [128, 2, 128])
        dcol = fw.sb("s5_dcol", [128, 4])
        drow = fw.sb("s5_drow", [4, 128])
        fw.dma("sp", drow[:], d["ssm_d"].t[l].rearrange("(c p) -> c p", p=128), reads=[d["ssm_d"]], writes=[drow])
        pbd = self.bank()
        fw.op("pe", lambda e: e.transpose(out=pbd[:, 0:4], in_=drow[:], identity=self.ident[0:4, 0:4]), reads=[drow, self.ident], writes=[pbd])
        fw.op("dve", lambda e: e.tensor_copy(out=dcol[:], in_=pbd[:, 0:4]), reads=[pbd], writes=[dcol])
        zst = [fw.sb("s5_z%d" % i, [128, 512]) for i in range(2)]
        chunks = [(i * 512, 512) for i in range(16)] + [(L, CT)]
        ui = 0
        for ct in range(4):
            first = True
            for dr in range(2):
                for g4 in range(4):
                    gp = ct * 4 + g4
                    gi = dr * 16 + gp
                    fw.op("pool", lambda e: e.memset(wB[:], 0.0), writes=[wB])
                    for ri in range(2):
                        for g2 in range(2):
                            c0 = 32 * g4 + 16 * g2
                            fw.op("dve", lambda e, ri=ri, g2=g2, c0=c0: e.tensor_copy(out=wB[64 * g2:64 * g2 + 64, ri, c0:c0 + 16], in_=bb[64 * g2:64 * g2 + 64, ri, gi, :]),
                                  reads=[bb], writes=[wB])
                    for ri in range(2):
                        pb = self.bank()
                        fw.op("pe", lambda e, ri=ri: e.transpose(out=pb[:, 0:128], in_=wB[:, ri, :], identity=self.ident[:]), reads=[wB, self.ident], writes=[pb])
                        fw.op("act", lambda e, ri=ri: e.activation(out=lB[:, ri, :], in_=pb[:, 0:128], func=AF.Copy), reads=[pb], writes=[lB])
                    fw.op("pool", lambda e: e.memset(lC[:], 0.0), writes=[lC])
                    for ri, nm in enumerate(("ssm_c_re", "ssm_c_im")):
                        for g2 in range(2):
                            fw.dma("sp" if g2 == 0 else "act", cpd[16 * g2:16 * g2 + 16, ri, 64 * g2:64 * g2 + 64], d[nm].t[l, dr, 2 * gp + g2, :, :], reads=[d[nm]], writes=[cpd])
                    for ri in range(2):
                        pb = self.bank()
                        fw.op("pe", lambda e, ri=ri: e.transpose(out=pb[:, 0:32], in_=cpd[:, ri, :], identity=self.ident[0:32, 0:32]), reads=[cpd, self.ident], writes=[pb])
                        fw.op("act", lambda e, ri=ri: e.activation(out=lC[:, ri, 32 * g4:32 * g4 + 32], in_=pb[:, 0:32], func=AF.Copy, scale=(1.0 if ri == 0 else -1.0)),
                              reads=[pb], writes=[lC])
                    for (c0, n) in chunks:
                        ub = uch[ui % 2]; ui += 1
                        fw.dma("sp" if ui % 2 else "act", ub[:, 0:n], s["projT"].t[ct * 128:(ct + 1) * 128, c0:c0 + n], reads=[s["projT"]], writes=[ub])
                        if dr == 0:
                            q0 = c0 + CT if c0 < L else 0
                        else:
                            q0 = c0
                        for ri in range(2):
                            pb = self.bank()
                            fw.op("pe", lambda e, ri=ri: e.matmul(pb[:, 0:n], lB[:, ri, :], ub[:, 0:n], start=True, stop=True), reads=[lB, ub], writes=[pb])
                            fw.op("act", lambda e, ri=ri: e.activation(out=A[ri][:, q0:q0 + n], in_=pb[:, 0:n], func=AF.Copy), reads=[pb], writes=[A[ri]])
                    src, dst = A, Bq
                    for k in range(NS):
                        sft = 1 << k
                        ar = pw[:, k, 0, gi:gi + 1]; ai = pw[:, k, 1, gi:gi + 1]; nai = pw[:, k, 2, gi:gi + 1]
                        if dr == 0:
                            o_sl = slice(sft, T); i_sl = slice(0, T - sft); h_sl = slice(0, sft)
                        else:
                            o_sl = slice(0, T - sft); i_sl = slice(sft, T); h_sl = slice(T - sft, T)
                        for ri in range(2):
                            fw.op("act", lambda e, ri=ri: e.activation(out=dst[ri][:, h_sl], in_=src[ri][:, h_sl], func=AF.Copy), reads=[src[ri]], writes=[dst[ri]])
                        fw.op("dve", lambda e: e.scalar_tensor_tensor(out=dst[0][:, o_sl], in0=src[0][:, i_sl], scalar=ar, in1=src[0][:, o_sl], op0=ALU.mult, op1=ALU.add),
                              reads=[src[0], pw], writes=[dst[0]])
                        fw.op("dve", lambda e: e.scalar_tensor_tensor(out=dst[0][:, o_sl], in0=src[1][:, i_sl], scalar=nai, in1=dst[0][:, o_sl], op0=ALU.mult, op1=ALU.add),
                              reads=[src[1], pw, dst[0]], writes=[dst[0]])
                        fw.op("dve", lambda e: e.scalar_tensor_tensor(out=dst[1][:, o_sl], in0=src[0][:, i_sl], scalar=ai, in1=src[1][:, o_sl], op0=ALU.mult, op1=ALU.add),
                              reads=[src[0], src[1], pw], writes=[dst[1]])
                        fw.op("dve", lambda e: e.scalar_tensor_tensor(out=dst[1][:, o_sl], in0=src[1][:, i_sl], scalar=ar, in1=dst[1][:, o_sl], op0=ALU.mult, op1=ALU.add),
                              reads=[src[1], pw, dst[1]], writes=[dst[1]])
                        src, dst = dst, src
                    hfin = src
                    for (c0, n) in chunks:
                        if dr == 0:
                            q0 = c0 + CT if c0 < L else 0
                        else:
                            q0 = c0
                        pb = self.bank()
                        fw.op("pe", lambda e: e.matmul(pb[:, 0:n], lC[:, 0, :], hfin[0][:, q0:q0 + n], start=True, stop=False), reads=[lC, hfin[0]], writes=[pb])
                        fw.op("pe", lambda e: e.matmul(pb[:, 0:n], lC[:, 1, :], hfin[1][:, q0:q0 + n], start=False, stop=True), reads=[lC, hfin[1]], writes=[pb])
                        if first:
                            fw.op("act", lambda e: e.activation(out=yacc[:, c0:c0 + n], in_=pb[:, 0:n], func=AF.Copy), reads=[pb], writes=[yacc])
                        else:
                            fw.op("pool" if False else "dve", lambda e: e.tensor_tensor(out=yacc[:, c0:c0 + n], in0=pb[:, 0:n], in1=yacc[:, c0:c0 + n], op=ALU.add), reads=[pb, yacc], writes=[yacc])
                    first = False
            for ci, (c0, n) in enumerate(chunks):
                ub = uch[ui % 2]; ui += 1
                fw.dma("sp", ub[:, 0:n], s["projT"].t[ct * 128:(ct + 1) * 128, c0:c0 + n], reads=[s["projT"]], writes=[ub])
                zb = zst[ci % 2]
                fw.op("dve", lambda e: e.scalar_tensor_tensor(out=zb[:, 0:n], in0=ub[:, 0:n], scalar=dcol[:, ct:ct + 1], in1=yacc[:, c0:c0 + n], op0=ALU.mult, op1=ALU.add),
                      reads=[ub, dcol, yacc], writes=[zb])
                fw.op("act", lambda e: e.activation(out=zb[:, 0:n], in_=zb[:, 0:n], func=AF.Gelu_apprx_tanh), reads=[zb], writes=[zb])
                fw.dma("act", s["s5z"].t[ct * 128:(ct + 1) * 128, c0:c0 + n], zb[:, 0:n], reads=[zb], writes=[s["s5z"]])

    def p2a_glu(self, l, es):
        fw, d, s = self.fw, self.d, self.s
        wg = fw.sb("glu_w", [128, 4, 512])
        fw.dma("sp", wg[:], d["ssm_w_glu"].t[l].rearrange("(k p) n -> p k n", p=128), reads=[d["ssm_w_glu"]], writes=[wg])
        zz = [fw.sb("glu_z%d" % i, [128, 4, 512]) for i in range(2)]
        og = [fw.sb("glu_o%d" % i, [128, 512]) for i in range(2)]
        chunks = [(i * 512, 512) for i in range(16)] + [(L, CT)]
        oi = 0
        for ci, (c0, n) in enumerate(chunks):
            zb = zz[ci % 2]
            fw.dma("sp", zb[:, :, 0:n], s["s5z"].t[:, c0:c0 + n].rearrange("(k p) t -> p k t", p=128), reads=[s["s5z"]], writes=[zb])
            for m in range(4):
                pb = self.bank()
                for k in range(4):
                    fw.op("pe", lambda e, k=k: e.matmul(pb[:, 0:n], wg[:, k, m * 128:(m + 1) * 128], zb[:, k, 0:n], start=(k == 0), stop=(k == 3)), reads=[wg, zb], writes=[pb])
                ob = og[oi % 2]; oi += 1
                fw.op("act", lambda e: e.activation(out=ob[:, 0:n], in_=pb[:, 0:n], func=AF.Sigmoid), reads=[pb], writes=[ob])
                fw.op("dve", lambda e: e.tensor_tensor(out=ob[:, 0:n], in0=ob[:, 0:n], in1=zb[:, m, 0:n], op=ALU.mult), reads=[ob, zb], writes=[ob])
                fw.dma("act", s["brT"].t[m * 128:(m + 1) * 128, c0:c0 + n], ob[:, 0:n], reads=[ob], writes=[s["brT"]])

    def p2b_na_norm(self, l, es):
        fw, d, s = self.fw, self.d, self.s
        bones = fw.sb("na_bones", [128, 128])
        fw.op("pool", lambda e: e.memset(bones[:], 0.0), writes=[bones])
        fw.op("pool", lambda e: e.memset(bones[0:64, 0:64], 1.0), writes=[bones])
        fw.op("pool", lambda e: e.memset(bones[64:128, 64:128], 1.0), writes=[bones])
        grow = fw.sb("na_grow", [2, 128]); gcol = fw.sb("na_gcol", [128, 2])
        for qi, nm in enumerate(("na_q_gain", "na_k_gain")):
            for rep in range(2):
                fw.dma("sp", grow[qi:qi + 1, rep * 64:(rep + 1) * 64], d[nm].t[l:l + 1, :], reads=[d[nm]], writes=[grow])
        pb = self.bank()
        fw.op("pe", lambda e: e.transpose(out=pb[:, 0:2], in_=grow[:], identity=self.ident[0:2, 0:2]), reads=[grow, self.ident], writes=[pb])
        fw.op("dve", lambda e: e.tensor_copy(out=gcol[:], in_=pb[:, 0:2]), reads=[pb], writes=[gcol])
        fw.op("dve", lambda e: e.tensor_scalar(out=gcol[:, 0:1], in0=gcol[:, 0:1], scalar1=0.125, scalar2=None, op0=ALU.mult), reads=[gcol], writes=[gcol])
        xq = [fw.sb("na_x%d" % i, [128, 512]) for i in range(2)]
        sq = [fw.sb("na_sq%d" % i, [128, 512]) for i in range(2)]
        rs = [fw.sb("na_rs%d" % i, [128, 512]) for i in range(2)]
        chunks = [(i * 512, 512) for i in range(16)] + [(L, CT)]
        it = 0
        for qi, dst in enumerate(("qnT", "knT")):
            for ct in range(4):
                row0 = W + qi * W + ct * 128
                for (c0, n) in chunks:
                    xb = xq[it % 2]; sb_ = sq[it % 2]; rb = rs[it % 2]; it += 1
                    fw.dma("sp", xb[:, 0:n], s["projT"].t[row0:row0 + 128, c0:c0 + n], reads=[s["projT"]], writes=[xb])
                    fw.op("act", lambda e: e.activation(out=sb_[:, 0:n], in_=xb[:, 0:n], func=AF.Square), reads=[xb], writes=[sb_])
                    pb = self.bank()
                    fw.op("pe", lambda e: e.matmul(pb[:, 0:n], bones[:], sb_[:, 0:n], start=True, stop=True), reads=[bones, sb_], writes=[pb])
                    fw.op("act", lambda e: e.activation(out=rb[:, 0:n], in_=pb[:, 0:n], func=AF.Sqrt, scale=1.0 / 64, bias=EPS), reads=[pb], writes=[rb])
                    fw.op("dve", lambda e: e.reciprocal(out=rb[:, 0:n], in_=rb[:, 0:n]), reads=[rb], writes=[rb])
                    fw.op("dve", lambda e: e.scalar_tensor_tensor(out=rb[:, 0:n], in0=xb[:, 0:n], scalar=gcol[:, qi:qi + 1], in1=rb[:, 0:n], op0=ALU.mult, op1=ALU.mult),
                          reads=[xb, gcol, rb], writes=[rb])
                    fw.dma("act", s[dst].t[ct * 128:(ct + 1) * 128, c0:c0 + n], rb[:, 0:n], reads=[rb], writes=[s[dst]])

    def p2b_na(self, l, es):
        fw, d, s = self.fw, self.d, self.s
        ones = fw.sb("na_ones", [128, 64])
        fw.op("pool", lambda e: e.memset(ones[:], 1.0), writes=[ones])
        tab3 = fw.sb("na_tab3", [128, 8, 4, 64]); tabe = fw.sb("na_tabe", [128, 8, 4, 64])
        fw.dma("sp", tab3[:], d["na_tab"].t[l, 3], reads=[d["na_tab"]], writes=[tab3])
        Kc = fw.sb("na_Kc", [128, 4, CT]); Vc = fw.sb("na_Vc", [128, 2, W])
        fw.dma("sp", Kc[:], s["knT"].t[:, L:AT].rearrange("(hp p) t -> p hp t", p=128), reads=[s["knT"]], writes=[Kc])
        fw.dma("act", Vc[:], s["vtok"].t[L:AT, :].rearrange("(j p) c -> p j c", p=128), reads=[s["vtok"]], writes=[Vc])
        Kw = [fw.sb("na_Kw%d" % i, [128, 4, 512]) for i in range(2)]
        Vw = [fw.sb("na_Vw%d" % i, [128, 4, W]) for i in range(2)]
        Qr = [fw.sb("na_Qr%d" % i, [128, 4, 64]) for i in range(2)]
        Pm = [fw.sb("na_P%d" % i, [128, 6, 64]) for i in range(2)]
        orow = [fw.sb("na_o%d" % i, [64, 8, 64]) for i in range(2)]
        rden = [fw.sb("na_rd%d" % i, [64, 64]) for i in range(2)]
        hi = 0
        for r in range(128):
            start = min(max(r - 4, 0), 120)
            dr0 = start - r + 7
            kb = Kw[r % 2]; vb = Vw[r % 2]; qb = Qr[r % 2]; ob = orow[r % 2]
            fw.dma("sp", kb[:], s["knT"].t[:, 64 * start:64 * start + 512].rearrange("(hp p) t -> p hp t", p=128), reads=[s["knT"]], writes=[kb])
            fw.dma("act", vb[:], s["vtok"].t[64 * start:64 * start + 512, :].rearrange("(j p) c -> p j c", p=128), reads=[s["vtok"]], writes=[vb])
            fw.dma("sp", qb[:], s["qnT"].t[:, 64 * r:64 * r + 64].rearrange("(hp p) t -> p hp t", p=128), reads=[s["qnT"]], writes=[qb])
            if dr0 == 3:
                tab = tab3
            else:
                tab = tabe
                fw.dma("act", tabe[:], d["na_tab"].t[l, dr0], reads=[d["na_tab"]], writes=[tabe])
            for h in range(8):
                hp, b = h // 2, 64 * (h % 2)
                pm = Pm[hi % 2]; rd = rden[hi % 2]; hi += 1
                ps = self.bank()
                for j in range(4):
                    fw.op("pe", lambda e, j=j: e.matmul(ps[:, j * 64:(j + 1) * 64], kb[b:b + 64, hp, j * 128:(j + 1) * 128], qb[b:b + 64, hp, :], start=True, stop=True),
                          reads=[kb, qb], writes=[ps])
                for j in range(2):
                    fw.op("pe", lambda e, j=j: e.matmul(ps[:, 256 + j * 64:256 + (j + 1) * 64], Kc[b:b + 64, hp, j * 128:(j + 1) * 128], qb[b:b + 64, hp, :], start=True, stop=True),
                          reads=[Kc, qb], writes=[ps])
                fw.op("dve", lambda e: e.tensor_tensor(out=pm[:, 0:4, :], in0=ps[:, 0:256].rearrange("p (j q) -> p j q", j=4), in1=tab[:, h, :, :], op=ALU.add),
                      reads=[ps, tab], writes=[pm])
                fw.op("act", lambda e: e.activation(out=pm[:, 0:4, :], in_=pm[:, 0:4, :], func=AF.Exp), reads=[pm], writes=[pm])
                fw.op("act", lambda e: e.activation(out=pm[:, 4:6, :], in_=ps[:, 256:384].rearrange("p (j q) -> p j q", j=2), func=AF.Exp), reads=[ps], writes=[pm])
                po = self.bank(); pd = self.bank()
                for j in range(6):
                    vsrc = vb[:, j, 64 * h:64 * h + 64] if j < 4 else Vc[:, j - 4, 64 * h:64 * h + 64]
                    fw.op("pe", lambda e, j=j, vsrc=vsrc: e.matmul(po[0:64, 0:64], vsrc, pm[:, j, :], start=(j == 0), stop=(j == 5)), reads=[vb, Vc, pm], writes=[po])
                for j in range(6):
                    fw.op("pe", lambda e, j=j: e.matmul(pd[0:64, 0:64], ones[:], pm[:, j, :], start=(j == 0), stop=(j == 5)), reads=[ones, pm], writes=[pd])
                fw.op("dve", lambda e: e.reciprocal(out=rd[:], in_=pd[0:64, 0:64]), reads=[pd], writes=[rd])
                fw.op("dve", lambda e: e.tensor_tensor(out=ob[:, h, :], in0=po[0:64, 0:64], in1=rd[:], op=ALU.mult), reads=[po, rd], writes=[ob])
            fw.dma("sp", s["brT"].t[W:2 * W, 64 * r:64 * r + 64].rearrange("(h d) q -> d h q", d=64), ob[:], reads=[ob], writes=[s["brT"]])
        if l == 0:
            Qc = fw.sb("na_Qc", [128, 4, CT]); Pc = fw.sb("na_Pc", [128, 2, CT]); oc = fw.sb("na_oc", [64, 8, CT]); rdc = fw.sb("na_rdc", [64, CT])
            fw.dma("sp", Qc[:], s["qnT"].t[:, L:AT].rearrange("(hp p) t -> p hp t", p=128), reads=[s["qnT"]], writes=[Qc])
            for h in range(8):
                hp, b = h // 2, 64 * (h % 2)
                ps = self.bank()
                for j in range(2):
                    fw.op("pe", lambda e, j=j: e.matmul(ps[:, j * CT:(j + 1) * CT], Kc[b:b + 64, hp, j * 128:(j + 1) * 128], Qc[b:b + 64, hp, :], start=True, stop=True),
                          reads=[Kc, Qc], writes=[ps])
                fw.op("act", lambda e: e.activation(out=Pc[:], in_=ps[:].rearrange("p (j q) -> p j q", j=2), func=AF.Exp), reads=[ps], writes=[Pc])
                po = self.bank(); pd = self.bank()
                for j in range(2):
                    fw.op("pe", lambda e, j=j: e.matmul(po[0:64, 0:CT], Vc[:, j, 64 * h:64 * h + 64], Pc[:, j, :], start=(j == 0), stop=(j == 1)), reads=[Vc, Pc], writes=[po])
                for j in range(2):
                    fw.op("pe", lambda e, j=j: e.matmul(pd[0:64, 0:CT], ones[:], Pc[:, j, :], start=(j == 0), stop=(j == 1)), reads=[ones, Pc], writes=[pd])
                fw.op("dve", lambda e: e.reciprocal(out=rdc[:], in_=pd[0:64, 0:CT]), reads=[pd], writes=[rdc])
                fw.op("dve", lambda e: e.tensor_tensor(out=oc[:, h, :], in0=po[0:64, 0:CT], in1=rdc[:], op=ALU.mult), reads=[po, rdc], writes=[oc])
            fw.dma("sp", s["brT"].t[W:2 * W, L:AT].rearrange("(h d) q -> d h q", d=64), oc[:], reads=[oc], writes=[s["brT"]])

    def p2c_hy_short(self, l, es):
        fw, d, s = self.fw, self.d, self.s
        wrow = fw.sb("hs_wrow", [4, 1536]); wcol = fw.sb("hs_wcol", [128, 12, 4])
        fw.dma("sp", wrow[0:3, :], d["hy_conv_w"].t[l], reads=[d["hy_conv_w"]], writes=[wrow])
        fw.dma("sp", wrow[3:4, :], d["hy_conv_b"].t[l:l + 1, :], reads=[d["hy_conv_b"]], writes=[wrow])
        for ct in range(12):
            pb = self.bank()
            fw.op("pe", lambda e: e.transpose(out=pb[:, 0:4], in_=wrow[:, ct * 128:(ct + 1) * 128], identity=self.ident[0:4, 0:4]), reads=[wrow, self.ident], writes=[pb])
            fw.op("dve", lambda e: e.tensor_copy(out=wcol[:, ct, :], in_=pb[:, 0:4]), reads=[pb], writes=[wcol])
        ub = [fw.sb("hs_u%d" % i, [128, L]) for i in range(2)]
        zb = [fw.sb("hs_z%d" % i, [128, L]) for i in range(2)]
        it = 0
        for ct in range(12):
            for (c0, n) in ((0, L), (L, CT)):
                u = ub[it % 2]; z = zb[it % 2]; it += 1
                fw.dma("sp", u[:, 0:n], s["projT"].t[2048 + ct * 128:2048 + (ct + 1) * 128, c0:c0 + n], reads=[s["projT"]], writes=[u])
                fw.op("dve", lambda e: e.tensor_scalar(out=z[:, 0:n], in0=u[:, 0:n], scalar1=wcol[:, ct, 1:2], scalar2=wcol[:, ct, 3:4], op0=ALU.mult, op1=ALU.add),
                      reads=[u, wcol], writes=[z])
                fw.op("dve", lambda e: e.scalar_tensor_tensor(out=z[:, 1:n], in0=u[:, 0:n - 1], scalar=wcol[:, ct, 0:1], in1=z[:, 1:n], op0=ALU.mult, op1=ALU.add),
                      reads=[u, wcol, z], writes=[z])
                fw.op("dve", lambda e: e.scalar_tensor_tensor(out=z[:, 0:n - 1], in0=u[:, 1:n], scalar=wcol[:, ct, 2:3], in1=z[:, 0:n - 1], op0=ALU.mult, op1=ALU.add),
                      reads=[u, wcol, z], writes=[z])
                fw.dma("act", s["hzT"].t[ct * 128:(ct + 1) * 128, c0:c0 + n], z[:, 0:n], reads=[z], writes=[s["hzT"]])

    def p2c_hy_filt(self, l, es, seg):
        fw, d, s = self.fw, self.d, self.s
        Lx, zname, dname = (L, "hyc_zL", "hyc_dL") if seg == 0 else (CT, "hyc_zc", "hyc_dc")
        CH = min(512, Lx); nch = Lx // CH
        hfT = s["hfT"] if seg == 0 else s["hfTc"]
        w1 = fw.sb("hf_w1", [33, 64]); w2 = fw.sb("hf_w2", [64, 64]); w3 = fw.sb("hf_w3", [64, 64]); w4 = fw.sb("hf_w4", [64, 2048])
        fw.dma("sp", w1[:], d["hy_w1"].t[l], reads=[d["hy_w1"]], writes=[w1]); fw.dma("sp", w2[:], d["hy_w2"].t[l], reads=[d["hy_w2"]], writes=[w2])
        fw.dma("sp", w3[:], d["hy_w3"].t[l], reads=[d["hy_w3"]], writes=[w3]); fw.dma("act", w4[:], d["hy_w4"].t[l], reads=[d["hy_w4"]], writes=[w4])
        brow = fw.sb("hf_brow", [4, 64]); bcol = fw.sb("hf_bcol", [64, 4])
        for i, nm in enumerate(("hy_b1", "hy_b2", "hy_b3", "hy_freq")):
            fw.dma("sp", brow[i:i + 1, :], d[nm].t[l:l + 1, :], reads=[d[nm]], writes=[brow])
        pb = self.bank()
        fw.op("pe", lambda e: e.transpose(out=pb[0:64, 0:4], in_=brow[:], identity=self.ident[0:4, 0:4]), reads=[brow, self.ident], writes=[pb])
        fw.op("dve", lambda e: e.tensor_copy(out=bcol[:], in_=pb[0:64, 0:4]), reads=[pb], writes=[bcol])
        zp = fw.sb("hf_zp", [33, Lx])
        fw.dma("sp", zp[:], d[zname].t, reads=[d[zname]], writes=[zp])
        h3 = fw.sb("hf_h3", [64, Lx])
        ha = fw.sb("hf_ha", [64, CH]); hb_ = fw.sb("hf_hb", [64, CH]); ti = fw.sb("hf_ti", [64, CH], I32); tf = fw.sb("hf_tf", [64, CH])
        for ch in range(nch):
            c0 = ch * CH
            cur_in = zp[:, c0:c0 + CH]; cur_buf = zp
            for li, (wt, kdim) in enumerate(((w1, 33), (w2, 64), (w3, 64))):
                pb = self.bank()
                fw.op("pe", lambda e: e.matmul(pb[0:64, 0:CH], wt[0:kdim, :], cur_in, start=True, stop=True), reads=[wt, cur_buf], writes=[pb])
                fw.op("dve", lambda e: e.tensor_scalar(out=ha[:], in0=pb[0:64, 0:CH], scalar1=bcol[:, li:li + 1], scalar2=bcol[:, 3:4], op0=ALU.add, op1=ALU.mult),
                      reads=[pb, bcol], writes=[ha])
                outap = (hb_[:] if li < 2 else h3[:, c0:c0 + CH]); outbuf = hb_ if li < 2 else h3
                self.sin_reduced(outap, ha[:], ti[:], tf[:], [ha], [outbuf, ti, tf])
                cur_in = hb_[:]; cur_buf = hb_
        dec = [fw.sb("hf_dec%d" % i, [128, CH]) for i in range(2)]
        hf = [fw.sb("hf_hf%d" % i, [128, CH]) for i in range(2)]
        junk = fw.sb("hf_junk", [128, CH])
        acc = fw.sb("hf_acc", [128, 2 * nch + 2]); nr = fw.sb("hf_nr", [128, 2])
        it = 0
        for o in range(2):
            for cti in range(4):
                for dr in range(2):
                    m0 = o * 1024 + dr * 512 + cti * 128
                    for ch in range(nch):
                        c0 = ch * CH
                        db = dec[it % 2]; hb2 = hf[it % 2]; it += 1
                        fw.dma("sp", db[:], d[dname].t[cti * 128:(cti + 1) * 128, c0:c0 + CH], reads=[d[dname]], writes=[db])
                        pb = self.bank()
                        fw.op("pe", lambda e: e.matmul(pb[:, 0:CH], w4[:, m0:m0 + 128], h3[:, c0:c0 + CH], start=True, stop=True), reads=[w4, h3], writes=[pb])
                        fw.op("dve", lambda e: e.tensor_tensor(out=hb2[:], in0=pb[:, 0:CH], in1=db[:], op=ALU.mult), reads=[pb, db], writes=[hb2])
                        if dr == 1 and ch == 0:
                            fw.op("dve", lambda e: e.memset(hb2[:, 0:1], 0.0), reads=[hb2], writes=[hb2])
                        fw.op("act", lambda e: e.activation(out=junk[:], in_=hb2[:], func=AF.Abs, accum_out=acc[:, dr * nch + ch:dr * nch + ch + 1]), reads=[hb2], writes=[junk, acc])
                        fw.dma("act", hfT.t[m0:m0 + 128, c0:c0 + CH], hb2[:], reads=[hb2], writes=[hfT])
                fw.op("dve", lambda e: e.tensor_reduce(out=nr[:, 0:1], in_=acc[:, 0:2 * nch], axis=AX.X, op=ALU.add), reads=[acc], writes=[nr])
                fw.op("dve", lambda e: e.reciprocal(out=nr[:, 1:2], in_=nr[:, 0:1]), reads=[nr], writes=[nr])
                fw.dma("sp", s["hnrm"].t[seg, o, cti * 128:(cti + 1) * 128].rearrange("(p a) -> p a", a=1), nr[:, 1:2], reads=[nr], writes=[s["hnrm"]])

    def hy_setup(self):
        fw, d = self.fw, self.d
        F3 = fw.sb("hy_F3", [128, 3, 128]); T2 = fw.sb("hy_T2", [128, 2, 128])
        fw.dma("sp", F3[:], d["hyc_F"].t, reads=[d["hyc_F"]], writes=[F3]); fw.dma("act", T2[:], d["hyc_T"].t, reads=[d["hyc_T"]], writes=[T2])
        FF = fw.sb("hy_FF", [128, 256]); FiFr = fw.sb("hy_FiFr", [128, 256]); FrnFi = fw.sb("hy_FrnFi", [128, 256])
        cp = lambda dst, src: fw.op("dve", lambda e: e.tensor_copy(out=dst, in_=src), reads=[F3], writes=[FF, FiFr, FrnFi])
        cp(FF[:, 0:128], F3[:, 0, :]); cp(FF[:, 128:256], F3[:, 1, :])
        cp(FiFr[:, 0:128], F3[:, 1, :]); cp(FiFr[:, 128:256], F3[:, 0, :])
        cp(FrnFi[:, 0:128], F3[:, 0, :]); cp(FrnFi[:, 128:256], F3[:, 2, :])
        self.hy = dict(F3=F3, T2=T2, FF=FF, FiFr=FiFr, FrnFi=FrnFi)
        self.hy_tmp = [fw.sb("hy_tmp%d" % i, [128, 512]) for i in range(2)]
        self.hy_B = [fw.sb("hy_B%d" % i, [128, 2, 512]) for i in range(2)]
        self.hy_bi = 0

    def hy_cmul(self, out, a_re, a_im, a_bufs, b_re, b_im, b_bufs, conj=False):
        fw = self.fw
        tmp = self.hy_tmp[0]
        o_re, o_im = out[:, 0, :], out[:, 1, :]
        fw.op("dve", lambda e: e.tensor_tensor(out=o_re, in0=a_re, in1=b_re, op=ALU.mult), reads=a_bufs + b_bufs, writes=[out])
        fw.op("dve", lambda e: e.tensor_tensor(out=tmp[:], in0=a_im, in1=b_im, op=ALU.mult), reads=a_bufs + b_bufs, writes=[tmp])
        fw.op("dve", lambda e: e.tensor_tensor(out=o_re, in0=o_re, in1=tmp[:], op=(ALU.add if conj else ALU.subtract)), reads=[out, tmp], writes=[out])
        fw.op("dve", lambda e: e.tensor_tensor(out=o_im, in0=a_re, in1=b_im, op=ALU.mult), reads=a_bufs + b_bufs, writes=[out])
        fw.op("dve", lambda e: e.tensor_tensor(out=tmp[:], in0=a_im, in1=b_re, op=ALU.mult), reads=a_bufs + b_bufs + [tmp], writes=[tmp])
        if conj:
            fw.op("dve", lambda e: e.tensor_tensor(out=o_im, in0=tmp[:], in1=o_im, op=ALU.subtract), reads=[out, tmp], writes=[out])
        else:
            fw.op("dve", lambda e: e.tensor_tensor(out=o_im, in0=o_im, in1=tmp[:], op=ALU.add), reads=[out, tmp], writes=[out])

    def hy_fft(self, X, nrow):
        fw, H = self.fw, self.hy
        p1 = [self.bank(), self.bank()]
        for c in range(4):
            fw.op("pe", lambda e, c=c: e.matmul(p1[c // 2][:, (c % 2) * 256:(c % 2) * 256 + 256], X[0:nrow, c, :], H["FF"][0:nrow, :], start=True, stop=True),
                  reads=[X, H["FF"]], writes=[p1[c // 2]])
        Bt = self.hy_B[self.hy_bi % 2]; self.hy_bi += 1
        twr = H["T2"][:, 0, :].unsqueeze(1).to_broadcast([128, 2, 128]); twi = H["T2"][:, 1, :].unsqueeze(1).to_broadcast([128, 2, 128])
        for half in range(2):
            pv = p1[half][:].rearrange("p (c r k) -> p c r k", c=2, r=2)
            ov = Bt[:, :, half * 256:(half + 1) * 256].rearrange("p r (c k) -> p r c k", c=2)
            tmp = self.hy_tmp[0]
            tv = tmp[:, 0:256].rearrange("p (c k) -> p c k", c=2)
            a_re, a_im = pv[:, :, 0, :], pv[:, :, 1, :]
            fw.op("dve", lambda e: e.tensor_tensor(out=ov[:, 0], in0=a_re, in1=twr, op=ALU.mult), reads=[p1[half], H["T2"]], writes=[Bt])
            fw.op("dve", lambda e: e.tensor_tensor(out=tv, in0=a_im, in1=twi, op=ALU.mult), reads=[p1[half], H["T2"]], writes=[tmp])
            fw.op("dve", lambda e: e.tensor_tensor(out=ov[:, 0], in0=ov[:, 0], in1=tv, op=ALU.subtract), reads=[Bt, tmp], writes=[Bt])
            fw.op("dve", lambda e: e.tensor_tensor(out=ov[:, 1], in0=a_re, in1=twi, op=ALU.mult), reads=[p1[half], H["T2"]], writes=[Bt])
            fw.op("dve", lambda e: e.tensor_tensor(out=tv, in0=a_im, in1=twr, op=ALU.mult), reads=[p1[half], H["T2"], tmp], writes=[tmp])
            fw.op("dve", lambda e: e.tensor_tensor(out=ov[:, 1], in0=ov[:, 1], in1=tv, op=ALU.add), reads=[Bt, tmp], writes=[Bt])
        pr, pi = self.bank(), self.bank()
        Fr, Fi, nFi = H["F3"][:, 0, :], H["F3"][:, 1, :], H["F3"][:, 2, :]
        fw.op("pe", lambda e: e.matmul(pr[:], Fr, Bt[:, 0, :], start=True, stop=False), reads=[H["F3"], Bt], writes=[pr])
        fw.op("pe", lambda e: e.matmul(pr[:], nFi, Bt[:, 1, :], start=False, stop=True), reads=[H["F3"], Bt], writes=[pr])
        fw.op("pe", lambda e: e.matmul(pi[:], Fr, Bt[:, 1, :], start=True, stop=False), reads=[H["F3"], Bt], writes=[pi])
        fw.op("pe", lambda e: e.matmul(pi[:], Fi, Bt[:, 0, :], start=False, stop=True), reads=[H["F3"], Bt], writes=[pi])
        return pr, pi

    def hy_ifft(self, Yh, nrow):
        fw, H = self.fw, self.hy
        p1 = [self.bank(), self.bank()]
        for c in range(4):
            dst = p1[c // 2][:, (c % 2) * 256:(c % 2) * 256 + 256]
            fw.op("pe", lambda e, c=c: e.matmul(dst, Yh[:, 0, c * 128:(c + 1) * 128], H["FrnFi"][:], start=True, stop=False), reads=[Yh, H["FrnFi"]], writes=[p1[c // 2]])
            fw.op("pe", lambda e, c=c: e.matmul(dst, Yh[:, 1, c * 128:(c + 1) * 128], H["FiFr"][:], start=False, stop=True), reads=[Yh, H["FiFr"]], writes=[p1[c // 2]])
        Et = self.hy_B[self.hy_bi % 2]; self.hy_bi += 1
        twr = H["T2"][:, 0, :].unsqueeze(1).to_broadcast([128, 2, 128]); twi = H["T2"][:, 1, :].unsqueeze(1).to_broadcast([128, 2, 128])
        for half in range(2):
            pv = p1[half][:].rearrange("p (c r k) -> p c r k", c=2, r=2)
            ov = Et[:, :, half * 256:(half + 1) * 256].rearrange("p r (c k) -> p r c k", c=2)
            tmp = self.hy_tmp[0]
            tv = tmp[:, 0:256].rearrange("p (c k) -> p c k", c=2)
            a_re, a_im = pv[:, :, 0, :], pv[:, :, 1, :]
            fw.op("dve", lambda e: e.tensor_tensor(out=ov[:, 0], in0=a_re, in1=twr, op=ALU.mult), reads=[p1[half], H["T2"]], writes=[Et])
            fw.op("dve", lambda e: e.tensor_tensor(out=tv, in0=a_im, in1=twi, op=ALU.mult), reads=[p1[half], H["T2"]], writes=[tmp])
            fw.op("dve", lambda e: e.tensor_tensor(out=ov[:, 0], in0=ov[:, 0], in1=tv, op=ALU.add), reads=[Et, tmp], writes=[Et])
            fw.op("dve", lambda e: e.tensor_tensor(out=ov[:, 1], in0=a_im, in1=twr, op=ALU.mult), reads=[p1[half], H["T2"]], writes=[Et])
            fw.op("dve", lambda e: e.tensor_tensor(out=tv, in0=a_re, in1=twi, op=ALU.mult), reads=[p1[half], H["T2"], tmp], writes=[tmp])
            fw.op("dve", lambda e: e.tensor_tensor(out=ov[:, 1], in0=ov[:, 1], in1=tv, op=ALU.subtract), reads=[Et, tmp], writes=[Et])
        po = self.bank()
        Fr, Fi = H["F3"][:, 0, 0:nrow], H["F3"][:, 1, 0:nrow]
        fw.op("pe", lambda e: e.matmul(po[0:nrow, :], Fr, Et[:, 0, :], start=True, stop=False), reads=[H["F3"], Et], writes=[po])
        fw.op("pe", lambda e: e.matmul(po[0:nrow, :], Fi, Et[:, 1, :], start=False, stop=True), reads=[H["F3"], Et], writes=[po])
        return po

    def p2c_hy_spec(self, l, es, seg):
        fw, d, s = self.fw, self.d, self.s
        self.hy_setup()
        Lx, nrow = (L, 64) if seg == 0 else (CT, 2)
        hfT = s["hfT"] if seg == 0 else s["hfTc"]
        spec = s["hspec"] if seg == 0 else s["hspecc"]
        rn = fw.sb("hp_rn", [128, 2, 512])
        for o in range(2):
            self.load_bcast("sp", rn, s["hnrm"], (seg * 2 + o) * 512, 512, ap=rn[:, o, :])
        Xf = [fw.sb("hp_Xf%d" % i, [64, 4, 128]) for i in range(2)]; Xb = [fw.sb("hp_Xb%d" % i, [64, 4, 128]) for i in range(2)]
        Sf = [fw.sb("hp_Sf%d" % i, [128, 2, 512]) for i in range(2)]; Hs = [fw.sb("hp_H%d" % i, [128, 2, 512]) for i in range(2)]
        it = 0
        for o in range(2):
            for g in range(128):
                c0 = g * 4
                xf = Xf[it % 2]; xb = Xb[it % 2]; sf = Sf[it % 2]; hs = Hs[it % 2]; it += 1
                r0 = o * 1024 + c0
                fw.dma("sp", xf[0:nrow], hfT.t[r0:r0 + 4, :].rearrange("c (a b) -> a c b", b=128), reads=[hfT], writes=[xf])
                fw.dma("act", xb[0:nrow], hfT.t[r0 + 512:r0 + 516, :].rearrange("c (a b) -> a c b", b=128), reads=[hfT], writes=[xb])
                pr, pi = self.hy_fft(xf, nrow)
                fw.op("act", lambda e: e.activation(out=sf[:, 0, :], in_=pr[:], func=AF.Copy), reads=[pr], writes=[sf])
                fw.op("act", lambda e: e.activation(out=sf[:, 1, :], in_=pi[:], func=AF.Copy), reads=[pi], writes=[sf])
                pr2, pi2 = self.hy_fft(xb, nrow)
                rnb = rn[:, o, c0:c0 + 4].unsqueeze(2).to_broadcast([128, 4, 128])
                v4 = lambda ap: ap.rearrange("p (c k) -> p c k", c=4)
                fw.op("dve", lambda e: e.tensor_tensor(out=hs[:, 0, :], in0=pr2[:], in1=sf[:, 0, :], op=ALU.add), reads=[pr2, sf], writes=[hs])
                fw.op("dve", lambda e: e.tensor_tensor(out=hs[:, 1, :], in0=sf[:, 1, :], in1=pi2[:], op=ALU.subtract), reads=[pi2, sf], writes=[hs])
                fw.op("dve", lambda e: e.tensor_tensor(out=v4(hs[:, 0, :]), in0=v4(hs[:, 0, :]), in1=rnb, op=ALU.mult), reads=[hs, rn], writes=[hs])
                fw.op("dve", lambda e: e.tensor_tensor(out=v4(hs[:, 1, :]), in0=v4(hs[:, 1, :]), in1=rnb, op=ALU.mult), reads=[hs, rn], writes=[hs])
                fw.dma("sp", spec.t[o, g], hs[:], reads=[hs], writes=[spec])

    def p2c_hy_conv(self, l, es, seg):
        fw, d, s = self.fw, self.d, self.s
        self.hy_setup()
        Lx, nrow, col0 = (L, 64, 0) if seg == 0 else (CT, 2, L)
        spec = s["hspec"] if seg == 0 else s["hspecc"]
        bias = fw.sb("hc_bias", [64, 2, 512])
        for o in range(2):
            self.load_bcast("sp", bias, d["hy_bias"], (l * 2 + o) * 512, 512, ap=bias[:, o, :])
        Y0 = [fw.sb("hc_Y0_%d" % i, [64, 4, 128]) for i in range(2)]; P0 = [fw.sb("hc_P0_%d" % i, [64, 4, 128]) for i in range(2)]
        P1 = [fw.sb("hc_P1_%d" % i, [64, 4, 128]) for i in range(2)]; Y1 = [fw.sb("hc_Y1_%d" % i, [64, 4, 128]) for i in range(2)]
        Hs = [fw.sb("hc_H%d" % i, [128, 2, 512]) for i in range(4)]; Yh = [fw.sb("hc_Yh%d" % i, [128, 2, 512]) for i in range(2)]
        invn = 1.0 / 16384.0
        f3 = lambda ap: ap.rearrange("p c k -> p (c k)")
        for g in range(128):
            c0 = g * 4
            y0 = Y0[g % 2]; p0 = P0[g % 2]; p1 = P1[g % 2]; y1 = Y1[g % 2]
            ld = lambda q, dst, row: fw.dma(q, dst[0:nrow], s["hzT"].t[row:row + 4, col0:col0 + Lx].rearrange("c (a b) -> a c b", b=128), reads=[s["hzT"]], writes=[dst])
            ld("sp", y0, 1024 + c0); ld("act", p0, c0); ld("sp", p1, 512 + c0)
            cur = y0
            for o in range(2):
                hs = Hs[(2 * g + o) % 4]; yh = Yh[o]
                fw.dma("act", hs[:], spec.t[o, g], reads=[spec], writes=[hs])
                pr, pi = self.hy_fft(cur, nrow)
                self.hy_cmul(yh, pr[:], pi[:], [pr, pi], hs[:, 0, :], hs[:, 1, :], [hs])
                po = self.hy_ifft(yh, nrow)
                part = p0 if o == 0 else p1
                dst = y1
                bb_ = bias[0:nrow, o, c0:c0 + 4].unsqueeze(2).to_broadcast([nrow, 4, 128])
                tmpb = self.hy_tmp[1]
                tv = tmpb[0:nrow, :].rearrange("p (c k) -> p c k", c=4)
                fw.op("dve", lambda e: e.tensor_tensor(out=tv, in0=cur[0:nrow], in1=bb_, op=ALU.mult), reads=[cur, bias], writes=[tmpb])
                fw.op("dve", lambda e: e.scalar_tensor_tensor(out=tmpb[0:nrow, :], in0=po[0:nrow, :], scalar=invn, in1=tmpb[0:nrow, :], op0=ALU.mult, op1=ALU.add),
                      reads=[po, tmpb], writes=[tmpb])
                fw.op("dve", lambda e: e.tensor_tensor(out=f3(dst[0:nrow]), in0=tmpb[0:nrow, :], in1=f3(part[0:nrow]), op=ALU.mult), reads=[tmpb, part], writes=[dst])
                cur = y1
            fw.dma("sp", s["brT"].t[2 * W + c0:2 * W + c0 + 4, col0:col0 + Lx].rearrange("c (a b) -> a c b", b=128), y1[0:nrow], reads=[y1], writes=[s["brT"]])

    def p3_merge(self, l, es, xin):
        fw, d, s = self.fw, self.d, self.s
        TB = 256
        hTb = fw.sb("m_hTb", [128, KT, TB])
        brb = [fw.sb("m_brb%d" % i, [128, 4, TB]) for i in range(3)]
        wg = [fw.sb("m_wg%d" % i, [128, KT, 512]) for i in range(2)]
        wbr = [fw.sb("m_wbr%d" % i, [128, 4, 512]) for i in range(2)]
        bg = [fw.sb("m_bg%d" % i, [128, 512]) for i in range(2)]
        mixed = [fw.sb("m_mix%d" % i, [128, D]) for i in range(2)]
        gate = [fw.sb("m_gate%d" % i, [128, 512]) for i in range(2)]
        mT = fw.sb("m_mT", [128, KT, 128])
        g1 = [fw.sb("m_g1_%d" % r, [128, D]) for r in range(2)]
        xt = fw.sb("m_xt", [128, D]); ot = fw.sb("m_ot", [128, D])
        for r in range(2):
            self.load_bcast("sp", g1[r], s["modD"], r * 6 * D + 2 * D, D, "small")
        wi = 0
        nblk = AT // TB
        if l == 0:
            self.dump("hTd0", s["hT"], s["hT"].t[:, 0, 0:128])
            self.dump("hTd15", s["hT"], s["hT"].t[:, 15, 0:128])
            self.dump("hTd0b", s["hT"], s["hT"].t[:, 0, 640:768])
        for blk in range(nblk):
            t0 = blk * TB
            r = 0 if t0 < L else 1
            if r == 1 and l == DEPTH - 1:
                continue
            fw.dma("sp", hTb[:], s["hT"].t[:, :, t0:t0 + TB], reads=[s["hT"]], writes=[hTb], key="m_ld")
            for i in range(3):
                fw.dma("act", brb[i][:], s["brT"].t[i * W:(i + 1) * W, t0:t0 + TB].rearrange("(k p) t -> p k t", p=128),
                       reads=[s["brT"]], writes=[brb[i]], key="m_ld")
            for i in range(3):
                for cc in range(4):
                    wgb = wg[wi % 2]; wbb = wbr[wi % 2]; bgb = bg[wi % 2]; wi += 1
                    c0 = i * D + cc * 512
                    fw.dma("sp", wgb[:], d["w_gate"].t[l, :, c0:c0 + 512].rearrange("(kt p) n -> p kt n", p=128),
                           reads=[d["w_gate"]], writes=[wgb], key="m_wg%d" % (wi % 2))
                    fw.dma("act", wbb[:], d["w_branch"].t[l, i, :, cc * 512:(cc + 1) * 512].rearrange("(k p) n -> p k n", p=128),
                           reads=[d["w_branch"]], writes=[wbb], key="m_wb%d" % (wi % 2))
                    self.load_bcast("pool", bgb, d["b_gate"], l * 3 * D + c0, 512, "m_bg%d" % (wi % 2))
                    for ti in range(TB // 128):
                        pg = self.bank()
                        for kt in range(KT):
                            fw.op("pe", lambda e, kt=kt: e.matmul(pg[:], hTb[:, kt, ti * 128:(ti + 1) * 128], wgb[:, kt, :], start=(kt == 0), stop=(kt == KT - 1)),
                                  reads=[hTb, wgb], writes=[pg])
                        pbp = self.bank()
                        for k4 in range(4):
                            fw.op("pe", lambda e, k4=k4: e.matmul(pbp[:], brb[i][:, k4, ti * 128:(ti + 1) * 128], wbb[:, k4, :], start=(k4 == 0), stop=(k4 == 3)),
                                  reads=[brb[i], wbb], writes=[pbp])
                        gt = gate[ti % 2]
                        fw.op("dve", lambda e: e.tensor_tensor(out=gt[:], in0=pg[:], in1=bgb[:], op=ALU.add), reads=[pg, bgb], writes=[gt])
                        fw.op("act", lambda e: e.activation(out=gt[:], in_=gt[:], func=AF.Sigmoid), reads=[gt], writes=[gt])
                        if blk == 0 and ti == 0 and i == 0 and cc == 0 and l == 0:
                            self.dump("gate00", gt, gt[:])
                            self.dump("hTb0", hTb, hTb[:, 0, 0:128])
                            self.dump("hTb15", hTb, hTb[:, 15, 0:128])
                            self.dump("wgb0", wgb, wgb[:, 0, :])
                            self.dump("bgb", bgb, bgb[:])
                        mx = mixed[ti]
                        if i == 0:
                            fw.op("dve", lambda e: e.tensor_tensor(out=mx[:, cc * 512:(cc + 1) * 512], in0=pbp[:], in1=gt[:], op=ALU.mult),
                                  reads=[pbp, gt], writes=[mx])
                        else:
                            fw.op("dve", lambda e: e.tensor_tensor(out=gt[:], in0=pbp[:], in1=gt[:], op=ALU.mult), reads=[pbp, gt], writes=[gt])
                            fw.op("pool", lambda e: e.tensor_tensor(out=mx[:, cc * 512:(cc + 1) * 512], in0=mx[:, cc * 512:(cc + 1) * 512], in1=gt[:], op=ALU.add),
                                  reads=[mx, gt], writes=[mx])
            for ti in range(TB // 128):
                mx = mixed[ti]
                if blk == 0 and ti == 0 and l == 0:
                    self.dump("mixed0", mx, mx[:])
                for q4 in range(4):
                    pb = self.bank()
                    for j in range(4):
                        kt = q4 * 4 + j
                        fw.op("pe", lambda e, kt=kt, j=j: e.transpose(out=pb[:, j * 128:(j + 1) * 128], in_=mx[:, kt * 128:(kt + 1) * 128], identity=self.ident[:]),
                              reads=[mx, self.ident], writes=[pb])
                    fw.op("act", lambda e: e.activation(out=mT[:, q4 * 4:(q4 + 1) * 4, :], in_=pb[:].rearrange("p (j t) -> p j t", j=4), func=AF.Copy),
                          reads=[pb], writes=[mT])
                tt0 = t0 + ti * 128
                if isinstance(xin, tuple):
                    srcb = xin[0] if tt0 < L else xin[1]
                    src = srcb.t[tt0:tt0 + 128, :] if tt0 < L else srcb.t[tt0 - L:tt0 - L + 128, :]
                else:
                    srcb = xin; src = xin.t[tt0:tt0 + 128, :]
                fw.dma("sp", xt[:], src, reads=[srcb], writes=[xt], key="m_x")
                for cc in range(4):
                    wgb = wg[wi % 2]; wi += 1
                    fw.dma("sp" if cc % 2 else "act", wgb[:], d["w_out"].t[l, :, cc * 512:(cc + 1) * 512].rearrange("(kt p) n -> p kt n", p=128),
                           reads=[d["w_out"]], writes=[wgb], key="m_wg%d" % (wi % 2))
                    po = self.bank()
                    for kt in range(KT):
                        fw.op("pe", lambda e, kt=kt: e.matmul(po[:], mT[:, kt, :], wgb[:, kt, :], start=(kt == 0), stop=(kt == KT - 1)),
                              reads=[mT, wgb], writes=[po])
                    fw.op("dve", lambda e: e.tensor_tensor(out=ot[:, cc * 512:(cc + 1) * 512], in0=po[:], in1=g1[r][:, cc * 512:(cc + 1) * 512], op=ALU.mult),
                          reads=[po, g1[r]], writes=[ot])
                fw.op("pool", lambda e: e.tensor_tensor(out=ot[:], in0=ot[:], in1=xt[:], op=ALU.add), reads=[ot, xt], writes=[ot])
                fw.dma("sp", s["xa"].t[tt0:tt0 + 128, :], ot[:], reads=[ot], writes=[s["xa"]], key="m_st")

    def p4a_router(self, l, es):
        fw, d, s = self.fw, self.d, self.s
        A = [fw.sb("r_A%d" % r, [128, D]) for r in range(2)]
        Bv = [fw.sb("r_B%d" % r, [128, D]) for r in range(2)]
        tmp = fw.sb("r_tmp", [128, D])
        self.load_bcast("sp", tmp, d["norm2"], l * D, D, "small")
        for r in range(2):
            self.load_bcast("act", A[r], s["modD"], r * 6 * D + 4 * D, D, "small")
            self.load_bcast("pool", Bv[r], s["modD"], r * 6 * D + 3 * D, D, "small")
            fw.op("dve", lambda e, r=r: e.scalar_tensor_tensor(out=A[r][:], in0=A[r][:], scalar=1.0, in1=tmp[:], op0=ALU.add, op1=ALU.mult),
                  reads=[A[r], tmp], writes=[A[r]])
        rt = fw.sb("r_rt", [128, KT, 16])
        fw.dma("sp", rt[:], d["router"].t[l].rearrange("(kt p) n -> p kt n", p=128), reads=[d["router"]], writes=[rt], key="small")
        xt = [fw.sb("r_xt%d" % i, [128, D]) for i in range(2)]
        hh = [fw.sb("r_hh%d" % i, [128, D]) for i in range(2)]
        hT = fw.sb("r_hT", [128, KT, 128])
        st = fw.sb("r_st", [128, 8]); lg = fw.sb("r_lg", [128, 16]); ex = fw.sb("r_ex", [128, 16])
        affT = self.affT
        ntile = NT if l == 0 else 64
        for tt in range(ntile):
            r = 0 if tt < 64 else 1
            xb = xt[tt % 2]; h = hh[tt % 2]
            fw.dma("sp", xb[:], s["xa"].t[tt * 128:(tt + 1) * 128, :], reads=[s["xa"]], writes=[xb], key="r_x%d" % (tt % 2))
            fw.op("act", lambda e: e.activation(out=h[:], in_=xb[:], func=AF.Square, accum_out=st[:, 0:1]), reads=[xb], writes=[h, st])
            fw.op("act", lambda e: e.activation(out=st[:, 1:2], in_=st[:, 0:1], func=AF.Sqrt, scale=1.0 / D, bias=EPS), reads=[st], writes=[st])
            fw.op("dve", lambda e: e.reciprocal(out=st[:, 2:3], in_=st[:, 1:2]), reads=[st], writes=[st])
            fw.op("dve", lambda e: e.scalar_tensor_tensor(out=h[:], in0=xb[:], scalar=st[:, 2:3], in1=A[r][:], op0=ALU.mult, op1=ALU.mult),
                  reads=[xb, st, A[r]], writes=[h])
            fw.op("pool", lambda e: e.tensor_tensor(out=h[:], in0=h[:], in1=Bv[r][:], op=ALU.add), reads=[h, Bv[r]], writes=[h])
            fw.dma("act", s["h2"].t[tt * 128:(tt + 1) * 128, :], h[:], reads=[h], writes=[s["h2"]], key="r_st")
            for q4 in range(4):
                pb = self.bank()
                for j in range(4):
                    kt = q4 * 4 + j
                    fw.op("pe", lambda e, kt=kt, j=j: e.transpose(out=pb[:, j * 128:(j + 1) * 128], in_=h[:, kt * 128:(kt + 1) * 128], identity=self.ident[:]),
                          reads=[h, self.ident], writes=[pb])
                fw.op("act" if q4 % 2 else "dve",
                      (lambda e: e.activation(out=hT[:, q4 * 4:(q4 + 1) * 4, :], in_=pb[:].rearrange("p (j t) -> p j t", j=4), func=AF.Copy)) if q4 % 2 else
                      (lambda e: e.tensor_copy(out=hT[:, q4 * 4:(q4 + 1) * 4, :], in_=pb[:].rearrange("p (j t) -> p j t", j=4))),
                      reads=[pb], writes=[hT])
            pl = self.bank()
            for kt in range(KT):
                fw.op("pe", lambda e, kt=kt: e.matmul(pl[:, 0:16], hT[:, kt, :], rt[:, kt, :], start=(kt == 0), stop=(kt == KT - 1)), reads=[hT, rt], writes=[pl])
            fw.op("dve", lambda e: e.tensor_copy(out=lg[:], in_=pl[:, 0:16]), reads=[pl], writes=[lg])
            fw.op("dve", lambda e: e.tensor_reduce(out=st[:, 3:4], in_=lg[:], axis=AX.X, op=ALU.max), reads=[lg], writes=[st])
            fw.op("dve", lambda e: e.tensor_scalar(out=st[:, 4:5], in0=st[:, 3:4], scalar1=-1.0, scalar2=None, op0=ALU.mult), reads=[st], writes=[st])
            fw.op("act", lambda e: e.activation(out=ex[:], in_=lg[:], func=AF.Exp, bias=st[:, 4:5], scale=1.0, accum_out=st[:, 5:6]), reads=[lg, st], writes=[ex, st])
            fw.op("dve", lambda e: e.reciprocal(out=st[:, 6:7], in_=st[:, 5:6]), reads=[st], writes=[st])
            fw.op("dve", lambda e: e.tensor_scalar(out=ex[:], in0=ex[:], scalar1=st[:, 6:7], scalar2=None, op0=ALU.mult), reads=[ex, st], writes=[ex])
            pt = self.bank()
            fw.op("pe", lambda e: e.transpose(out=pt[0:16, 0:128], in_=ex[:], identity=self.ident[:]), reads=[ex, self.ident], writes=[pt])
            fw.op("act", lambda e: e.activation(out=affT[0:16, tt * 128:(tt + 1) * 128], in_=pt[0:16, 0:128], func=AF.Copy), reads=[pt], writes=[affT])

    def p4b_topk(self, l, es):
        fw = self.fw
        affT = self.affT
        vals, idxu = self.tk_vals, self.tk_idx
        segs = [(0, L, 1024, 0)] + ([(L, AT, 32, 1024)] if l == 0 else [])
        for (a, b, cap, off) in segs:
            for rd in range(cap // 8):
                o = off + rd * 8
                fw.op("dve", lambda e: e.max(out=vals[0:16, o:o + 8], in_=affT[0:16, a:b]), reads=[affT], writes=[vals])
                fw.op("dve", lambda e: e.max_index(out=idxu[0:16, o:o + 8], in_max=vals[0:16, o:o + 8], in_values=affT[0:16, a:b]), reads=[affT, vals], writes=[idxu])
                fw.op("dve", lambda e: e.match_replace(out=affT[0:16, a:b], in_to_replace=vals[0:16, o:o + 8], in_values=affT[0:16, a:b], imm_value=-1.0),
                      reads=[affT, vals], writes=[affT])
        nch = 9 if l == 0 else 8
        idxf = fw.sb("tk_idxf", [16, 1152])
        fw.op("dve", lambda e: e.tensor_copy(out=idxf[:], in_=idxu[:]), reads=[idxu], writes=[idxf])
        if l == 0:
            fw.op("dve", lambda e: e.tensor_scalar(out=idxf[:, 1024:1056], in0=idxf[:, 1024:1056], scalar1=float(L), scalar2=None, op0=ALU.add), reads=[idxf], writes=[idxf])
        for j in range(nch):
            n = 128 if j < 8 else 32
            pt = self.bank()
            fw.op("pe", lambda e: e.transpose(out=pt[0:n, 0:16], in_=idxf[0:16, j * 128:j * 128 + n], identity=self.ident[0:16, 0:16]), reads=[idxf, self.ident], writes=[pt])
            fw.op("dve", lambda e: e.tensor_copy(out=self.idxT[0:n, j, :], in_=pt[0:n, 0:16]), reads=[pt], writes=[self.idxT])
            pt2 = self.bank()
            fw.op("pe", lambda e: e.transpose(out=pt2[0:n, 0:16], in_=vals[0:16, j * 128:j * 128 + n], identity=self.ident[0:16, 0:16]), reads=[vals, self.ident], writes=[pt2])
            fw.op("act", lambda e: e.activation(out=self.gT[0:n, j, :], in_=pt2[0:n, 0:16], func=AF.Copy), reads=[pt2], writes=[self.gT])

    def p4c_experts(self, l, es):
        fw, d, s = self.fw, self.d, self.s
        xs = [fw.sb("e_xs%d" % i, [128, D]) for i in range(2)]
        xsT = fw.sb("e_xsT", [128, KT, 512], BF16)
        zT = fw.sb("e_zT", [128, KT, 512], BF16)
        w1c = [fw.sb("e_w1c%d" % i, [128, KT, 128]) for i in range(2)]
        w3c = [fw.sb("e_w3c%d" % i, [128, KT, 128]) for i in range(2)]
        w1h = [fw.sb("e_w1h%d" % i, [128, KT, 128], BF16) for i in range(2)]
        w3h = [fw.sb("e_w3h%d" % i, [128, KT, 128], BF16) for i in range(2)]
        w2f = fw.sb("e_w2f", [128, KT, 512]); w2h = fw.sb("e_w2h", [128, KT, 512], BF16)
        ysc = [fw.sb("e_ysc%d" % i, [128, D]) for i in range(4)]
        sa = fw.sb("e_sa", [128, 512])
        g2 = [fw.sb("e_g2_%d" % r, [128, D]) for r in range(2)]
        for r in range(2):
            self.load_bcast("sp", g2[r], s["modD"], r * 6 * D + 5 * D, D, "small")
        wi = 0
        groups = []
        for e_ in range(16):
            groups.append((e_, [0, 1, 2, 3], 128, 0))
            groups.append((e_, [4, 5, 6, 7], 128, 0))
        if l == 0:
            for e_ in range(16):
                groups.append((e_, [8], 32, 1))
        for (e_, chunks, n, r) in groups:
            ntok = n * len(chunks)
            for ci, j in enumerate(chunks):
                xb = xs[ci % 2]
                fw.idma(out=xb[0:n, :], out_offset=None, in_=s["h2"].t[:, :],
                        in_offset=bass.IndirectOffsetOnAxis(ap=self.idxT[0:n, j, e_:e_ + 1], axis=0),
                        reads=[s["h2"], self.idxT], writes=[xb], key="e_g%d" % (ci % 2))
                for q4 in range(4):
                    pb = self.bank()
                    for jj in range(4):
                        kt = q4 * 4 + jj
                        fw.op("pe", lambda e, kt=kt, jj=jj: e.transpose(out=pb[:, jj * 128:jj * 128 + n], in_=xb[0:n, kt * 128:(kt + 1) * 128], identity=self.ident[0:n, 0:n]),
                              reads=[xb, self.ident], writes=[pb])
                    src = pb[:].rearrange("p (j t) -> p j t", j=4)[:, :, 0:n]
                    if q4 % 2:
                        fw.op("act", lambda e: e.activation(out=xsT[:, q4 * 4:(q4 + 1) * 4, ci * n:(ci + 1) * n], in_=src, func=AF.Copy), reads=[pb], writes=[xsT])
                    else:
                        fw.op("dve", lambda e: e.tensor_copy(out=xsT[:, q4 * 4:(q4 + 1) * 4, ci * n:(ci + 1) * n], in_=src), reads=[pb], writes=[xsT])
            for ft in range(KT):
                w1f = w1c[wi % 2]; w3f = w3c[wi % 2]; w1b = w1h[wi % 2]; w3b = w3h[wi % 2]; wi += 1
                fw.dma("sp", w1f[:], d["exp_w1"].t[l, e_, :, ft * 128:(ft + 1) * 128].rearrange("(kt p) n -> p kt n", p=128),
                       reads=[d["exp_w1"]], writes=[w1f])
                fw.dma("act", w3f[:], d["exp_w3"].t[l, e_, :, ft * 128:(ft + 1) * 128].rearrange("(kt p) n -> p kt n", p=128),
                       reads=[d["exp_w3"]], writes=[w3f])
                fw.op("pool", lambda e: e.tensor_copy(out=w1b[:], in_=w1f[:]), reads=[w1f], writes=[w1b])
                fw.op("pool", lambda e: e.tensor_copy(out=w3b[:], in_=w3f[:]), reads=[w3f], writes=[w3b])
                pa = self.bank(); pg = self.bank()
                for kt in range(KT):
                    fw.op("pe", lambda e, kt=kt: e.matmul(pa[:, 0:ntok], w1b[:, kt, :], xsT[:, kt, 0:ntok], start=(kt == 0), stop=(kt == KT - 1)), reads=[w1b, xsT], writes=[pa])
                for kt in range(KT):
                    fw.op("pe", lambda e, kt=kt: e.matmul(pg[:, 0:ntok], w3b[:, kt, :], xsT[:, kt, 0:ntok], start=(kt == 0), stop=(kt == KT - 1)), reads=[w3b, xsT], writes=[pg])
                fw.op("act", lambda e: e.activation(out=sa[:, 0:ntok], in_=pa[:, 0:ntok], func=AF.Silu), reads=[pa], writes=[sa])
                fw.op("dve", lambda e: e.tensor_tensor(out=zT[:, ft, 0:ntok], in0=pg[:, 0:ntok], in1=sa[:, 0:ntok], op=ALU.mult), reads=[pg, sa], writes=[zT])
            for dc in range(4):
                fw.dma("sp" if dc % 2 else "act", w2f[:], d["exp_w2"].t[l, e_, :, dc * 512:(dc + 1) * 512].rearrange("(kt p) n -> p kt n", p=128),
                       reads=[d["exp_w2"]], writes=[w2f])
                fw.op("pool", lambda e: e.tensor_copy(out=w2h[:], in_=w2f[:]), reads=[w2f], writes=[w2h])
                for ci, j in enumerate(chunks):
                    py = self.bank()
                    for ft in range(KT):
                        fw.op("pe", lambda e, ft=ft: e.matmul(py[0:n, :], zT[:, ft, ci * n:(ci + 1) * n], w2h[:, ft, :], start=(ft == 0), stop=(ft == KT - 1)), reads=[zT, w2h], writes=[py])
                    fw.op("dve", lambda e: e.scalar_tensor_tensor(out=ysc[ci][0:n, dc * 512:(dc + 1) * 512], in0=py[0:n, :], scalar=self.gT[0:n, j, e_:e_ + 1],
                                                                  in1=g2[r][0:n, dc * 512:(dc + 1) * 512], op0=ALU.mult, op1=ALU.mult),
                          reads=[py, self.gT, g2[r]], writes=[ysc[ci]])
            for ci, j in enumerate(chunks):
                fw.idma(out=s["xa"].t[:, :], out_offset=bass.IndirectOffsetOnAxis(ap=self.idxT[0:n, j, e_:e_ + 1], axis=0), in_=ysc[ci][0:n, :], in_offset=None,
                        reads=[ysc[ci], self.idxT], writes=[s["xa"]], key="e_sc", compute_op=ALU.add)

    def phase(self, fn, *a):
        fw = self.fw
        with ExitStack() as es:
            old = fw.es; fw.es = es
            fn(*a)
            fw.barrier()
            fw.es = old

    def p4_moe(self, l, es):
        fw = self.fw
        self.idxT = fw.sb("idxT", [128, 9, 16], I32); self.gT = fw.sb("gT", [128, 9, 16])
        with ExitStack() as es2:
            old = fw.es; fw.es = es2
            self.affT = fw.sb("affT", [16, AT])
            self.tk_vals = fw.sb("tk_vals", [16, 1152]); self.tk_idx = fw.sb("tk_idx", [16, 1152], U32)
            self.phase(self.p4a_router, l, None)
            self.phase(self.p4b_topk, l, None)
            fw.es = old
        self.phase(self.p4c_experts, l, None)

    def build(self):
        fw = self.fw
        xin = (self.d["x"], self.d["ctx"])
        for l in range(DEPTH):
            if self.test_br and l == 0:
                brin = fw.dram("brT_in", [3 * W, AT], kind="ExternalInput")
                for i in range(12):
                    fw.dma("sp", self.s["brT"].t[i * 128:(i + 1) * 128, :], brin.t[i * 128:(i + 1) * 128, :], reads=[brin], writes=[self.s["brT"]], key="brcp")
            self.phase(self.p0_mod, l, None)
            if self.stop_after == ("p0", l):
                break
            self.phase(self.p1_proj, l, None, xin)
            if self.stop_after == ("p1", l):
                break
            if not self.test_br:
                self.phase(self.p2a_s5, l, None)
                self.phase(self.p2a_glu, l, None)
            if self.stop_after == ("p2a", l):
                break
            if not self.test_br:
                self.phase(self.p2b_na_norm, l, None)
                self.phase(self.p2b_na, l, None)
            if self.stop_after == ("p2b", l):
                break
            if not self.test_br:
                self.phase(self.p2c_hy_short, l, None)
                for seg in ((0, 1) if l == 0 else (0,)):
                    self.phase(self.p2c_hy_filt, l, None, seg)
                    self.phase(self.p2c_hy_spec, l, None, seg)
                    self.phase(self.p2c_hy_conv, l, None, seg)
            if self.stop_after == ("p2c", l):
                break
            self.phase(self.p3_merge, l, None, xin)
            if self.stop_after == ("p3", l):
                break
            self.phase(self.p4_moe, l, None)
            if self.stop_after == ("p4", l):
                break
            xin = self.s["xa"]
        else:
            for i in range(64):
                fw.dma("sp" if i % 2 else "act", self.out.t[i * 128:(i + 1) * 128, :], self.s["xa"].t[i * 128:(i + 1) * 128, :], reads=[self.s["xa"]], writes=[self.out], key="outcp")
        for (oname, name, sl) in self.dbg:
            src = self.s[name]
            r0, r1, c0, c1 = sl
            o = fw.dram("dbg_" + oname, [r1 - r0, c1 - c0], kind="ExternalOutput")
            fw.dma("sp", o.t, src.t[r0:r1, c0:c1], reads=[src], writes=[o], key="dbg")
            self.dbg_out[oname] = o
        fw.finish([self.out] + list(self.dbg_out.values()))


def build_nc(stop_after=None, dbg=None, gather=True, test_br=False, wdepth=DEPTH):
    nc = bass.Bass("TRN2", target_bir_lowering=False)
    es = ExitStack()
    with es:
        k = K(nc, es, stop_after=stop_after, dbg=dbg, gather=gather, test_br=test_br, wdepth=wdepth)
        k.build()
    return nc, k


BIGW = ["w_ada", "w_in", "w_gate", "w_branch", "w_out", "exp_w1", "exp_w3", "exp_w2"]
WNAMES = ["hy_conv_w", "hy_conv_b", "hy_w1", "hy_b1", "hy_w2", "hy_b2", "hy_w3", "hy_b3", "hy_w4", "hy_freq", "hy_bias", "na_q_gain", "na_k_gain", "ssm_lam_re", "ssm_lam_im", "ssm_log_step", "ssm_b_re", "ssm_b_im", "ssm_c_re", "ssm_c_im", "ssm_d", "ssm_w_glu", "w_ada", "b_ada", "norm1", "norm2", "w_in", "w_gate", "b_gate", "w_branch", "w_out", "router", "exp_w1", "exp_w3", "exp_w2"]


def na_table(rpb):
    Lr = rpb.shape[0]
    col = np.arange(64)
    startc = np.clip(col - 8, 0, 48)
    kc = np.arange(64)
    inwin = (kc[None, :] >= startc[:, None]) & (kc[None, :] < startc[:, None] + 16)
    dc = np.clip(kc[None, :] - col[:, None] + 15, 0, 30)
    tab = np.empty((Lr, 8, 8, 8, 64, 64), np.float32)
    for dr0 in range(8):
        for j in range(8):
            v = rpb[:, :, j + dr0, :][:, :, dc]
            v = np.where(inwin[None, None], v, np.float32(-30000.0))
            tab[:, dr0, :, j] = np.transpose(v, (0, 1, 3, 2))
    tab = tab.reshape(Lr, 8, 8, 4, 128, 64)
    return np.ascontiguousarray(np.transpose(tab, (0, 1, 4, 2, 3, 5)))


def hyena_consts():
    n = np.arange(128, dtype=np.float64)
    ang = 2 * np.pi * np.outer(n, n) / 128.0
    F = np.stack([np.cos(ang), -np.sin(ang), np.sin(ang)], axis=1).astype(np.float32)
    angt = 2 * np.pi * np.outer(n, n) / 16384.0
    T = np.stack([np.cos(angt), -np.sin(angt)], axis=1).astype(np.float32)
    out = {"hyc_F": F, "hyc_T": T}
    for nm, length in (("L", L), ("c", CT)):
        t = np.linspace(0.0, 1.0, length, dtype=np.float32)[:, None]
        freqs = np.linspace(1e-4, 15, 16, dtype=np.float32)
        a = (np.float32(2.0 * np.pi / length) * np.arange(length, dtype=np.float32)[:, None] * freqs[None, :]).astype(np.float32)
        z = np.concatenate([t, np.cos(a), -np.sin(a)], axis=-1).astype(np.float32)
        mn, mx = np.log(1e-2) / 1.5, np.log(1e-2) / 0.3
        dec = np.exp(-t * np.abs(np.linspace(mn, mx, W, dtype=np.float32))[None, :]).astype(np.float32)
        out["hyc_z" + nm] = np.ascontiguousarray(z.T)
        out["hyc_d" + nm] = np.ascontiguousarray(dec.T)
    return out


def make_in_maps(inputs, cores, used=None, gather=True):
    maps = []
    shared = {}
    shards = {}
    for n in WNAMES:
        if used is not None and n not in used:
            continue
        a = np.ascontiguousarray(inputs[n])
        if n in BIGW and gather:
            a2 = a.reshape(DEPTH, -1, a.shape[-1])
            R = a2.shape[1]
            shards[n] = [np.ascontiguousarray(a2[:, c * (R // 8):(c + 1) * (R // 8), :]) for c in range(8)]
        else:
            shared[n] = a
    for n_, v_ in hyena_consts().items():
        if used is None or n_ in used:
            shared[n_] = v_
    if used is None or "na_tab" in used:
        shared["na_tab"] = na_table(np.asarray(inputs["na_rpb"], np.float32))
    for c in cores:
        b = c % 4
        m = dict(shared)
        for n in shards:
            m[n + "_sh"] = shards[n][c]
        if used is None or "x" in used:
            m["x"] = np.ascontiguousarray(inputs["x"][b])
        if used is None or "ctx" in used:
            m["ctx"] = np.ascontiguousarray(inputs["ctx"][b])
        m["cvec"] = np.ascontiguousarray(np.stack([inputs["c"][b], inputs["c_ctx"]], axis=0))
        maps.append(m)
    return maps


def kernel(**inputs):
    nc, k = build_nc(gather=False)
    cores = list(range(8))
    res = run_bass_kernel_spmd(nc, make_in_maps(inputs, cores, set(k.d.keys()), gather=False), core_ids=cores)
    out = np.stack([res.results[b]["out"] for b in range(4)], axis=0)
    return out.astype(np.float32)
```
